# Optimizing a Trainium2 kernel written in Bass

```python
import math
import jax
import jax.numpy as jnp
from jax import lax
import numpy as np

D_MODEL = 1024
BATCH = 8
SEQ = 4096
DEPTH = 4

GRID_W = 64
CTX_LEN = 256
N_MIXERS = 4
N_MOD = 6
EPS = 1e-6
FFN_HIDDEN = -(-8 * D_MODEL // (3 * 256)) * 256

GLA_HEADS = 4
GLA_QK = D_MODEL // 2
GLA_VD = D_MODEL
GLA_DK = GLA_QK // GLA_HEADS
GLA_DV = GLA_VD // GLA_HEADS
GLA_RANK = 16
GLA_GATE_NORM = 16.0
GLA_CHUNK = 64
GLA_IN = 2 * GLA_QK + GLA_VD + D_MODEL + 2 * GLA_RANK

HY_SHORT = 3
HY_BANDS = 16
HY_EMB = 1 + 2 * HY_BANDS
HY_FILTER_DIM = 64
HY_DECAY_TARGET = 1e-2
HY_FAST_DECAY = 0.3
HY_SLOW_DECAY = 1.5

ML_HEADS = 4
ML_INNER = 2 * D_MODEL
ML_DH = ML_INNER // ML_HEADS
ML_BLOCK = 4
ML_NBLOCKS = ML_INNER // ML_BLOCK
ML_CONV = 3
ML_CHUNK = 64

S5_GROUP = 16
S5_GROUPS = D_MODEL // S5_GROUP
S5_STATE = 64
S5_DT_MIN = 1e-3
S5_DT_MAX = 1e-1

N_GLA = (DEPTH + 3) // N_MIXERS
N_HY = (DEPTH + 2) // N_MIXERS
N_ML = (DEPTH + 1) // N_MIXERS
N_S5 = DEPTH // N_MIXERS

kernel_name = 'hybrid_gla_hyena_mlstm_s5_trunk'


def rms_norm(x, g):
    x = x.astype(jnp.float32)
    return x * lax.rsqrt(jnp.mean(x * x, axis=-1, keepdims=True) + EPS) * g.astype(jnp.float32)


def head_rms_norm(o, g):
    bsz, length, heads, d = o.shape
    o = o * lax.rsqrt(jnp.mean(o * o, axis=-1, keepdims=True) + EPS) * g
    return o.reshape(bsz, length, heads * d)


def multihead_layer_norm(o, g):
    bsz, length, heads, d = o.shape
    o = o - jnp.mean(o, axis=-1, keepdims=True)
    o = o * lax.rsqrt(jnp.mean(o * o, axis=-1, keepdims=True) + EPS)
    return o.reshape(bsz, length, heads * d) * g


def modulate(h, shift, scale):
    return h * (1.0 + scale) + shift


def flip_seq(t):
    return jnp.flip(t, axis=1)


def depthwise_conv_centred(x, w, b):
    k = w.shape[0]
    y = lax.conv_general_dilated(x, w.astype(x.dtype)[:, None, :], window_strides=(1,),
                                 padding=[(k // 2, k // 2)], dimension_numbers=('NWC', 'WIO', 'NWC'),
                                 feature_group_count=x.shape[-1])
    return y + b


def to_col_major(x, rows):
    bsz, length, ch = x.shape
    return x.reshape(bsz, rows, GRID_W, ch).transpose(0, 2, 1, 3).reshape(bsz, length, ch)


def from_col_major(x, rows):
    bsz, length, ch = x.shape
    return x.reshape(bsz, GRID_W, rows, ch).transpose(0, 2, 1, 3).reshape(bsz, length, ch)


def swiglu(h, w_in, w_out):
    gate, up = jnp.split(h @ w_in, 2, axis=-1)
    return (jax.nn.silu(gate) * up) @ w_out


def gla_chunk_scan(q, k, v, g, s0):
    bsz, length, heads, _ = q.shape
    dv = v.shape[-1]
    n_chunks = length // GLA_CHUNK

    def chunked(t):
        return t.reshape(bsz, n_chunks, GLA_CHUNK, heads, t.shape[-1]).swapaxes(0, 1)

    causal = jnp.tril(jnp.ones((GLA_CHUNK, GLA_CHUNK), dtype=bool))

    def step(s, inp):
        qc, kc, vc, gc = inp
        b = jnp.cumsum(gc, axis=1)
        b_end = b[:, -1]
        q_dec = qc * jnp.exp(b)
        scores = jnp.einsum('bihd,bjhd->bhij', q_dec, kc * jnp.exp(-b))
        scores = jnp.where(causal, scores, 0.0)
        o = jnp.einsum('bhij,bjhv->bihv', scores, vc) + jnp.einsum('bihd,bhdv->bihv', q_dec, s)
        k_end = kc * jnp.exp(b_end[:, None] - b)
        s = jnp.exp(b_end)[..., None] * s + jnp.einsum('bjhd,bjhv->bhdv', k_end, vc)
        return s, o

    s_end, o = lax.scan(step, s0, (chunked(q), chunked(k), chunked(v), chunked(g)))
    return o.swapaxes(0, 1).reshape(bsz, length, heads, dv), s_end


def gla_mixer(h_lat, h_ctx, w_in, w_gate, b_gate, norm_g, w_out):
    def project(h):
        bsz, length, _ = h.shape
        z = h.astype(jnp.float32) @ w_in
        q, k, v, r, glr = jnp.split(z, [GLA_QK, 2 * GLA_QK, 2 * GLA_QK + GLA_VD,
                                       2 * GLA_QK + GLA_VD + D_MODEL], axis=-1)
        glr = glr.reshape(bsz, length, 2, GLA_RANK)
        g = jax.nn.log_sigmoid(jnp.einsum('blzr,zrk->blzk', glr, w_gate) + b_gate) / GLA_GATE_NORM
        g = g.reshape(bsz, length, 2, GLA_HEADS, GLA_DK)
        q = q.reshape(bsz, length, GLA_HEADS, GLA_DK) * GLA_DK ** -0.5
        k = k.reshape(bsz, length, GLA_HEADS, GLA_DK)
        v = v.reshape(bsz, length, GLA_HEADS, GLA_DV)
        return q, k, v, r, g[:, :, 0], g[:, :, 1]

    def bidirectional(h, s_f0, s_b0):
        q, k, v, r, g_f, g_b = project(h)
        o_f, s_f = gla_chunk_scan(q, k, v, g_f, s_f0)
        o_b, s_b = gla_chunk_scan(flip_seq(q), flip_seq(k), flip_seq(v), flip_seq(g_b), s_b0)
        o = head_rms_norm(o_f + flip_seq(o_b), norm_g)
        return (o * jax.nn.silu(r)) @ w_out, s_f, s_b

    s0 = jnp.zeros((h_ctx.shape[0], GLA_HEADS, GLA_DK, GLA_DV), jnp.float32)
    y_ctx, s_f, s_b = bidirectional(h_ctx, s0, s0)
    y_lat, _, _ = bidirectional(h_lat, s_f, s_b)
    return y_lat, y_ctx


def hyena_filter(length, w1, b1, w2, b2, w3, freq):
    pos = jnp.arange(length, dtype=jnp.float32)
    t = pos / length
    bands = jnp.linspace(1e-4, HY_BANDS - 1, HY_BANDS, dtype=jnp.float32)
    ang = 2.0 * math.pi * t[:, None] * bands[None]
    feats = jnp.concatenate([t[:, None], jnp.cos(ang), -jnp.sin(ang)], axis=-1)
    z = jnp.sin(freq * (feats @ w1 + b1))
    z = jnp.sin(freq * (z @ w2 + b2))
    h = (z @ w3).reshape(length, 2, D_MODEL)
    log_target = math.log(HY_DECAY_TARGET)
    deltas = jnp.abs(jnp.linspace(log_target / HY_SLOW_DECAY, log_target / HY_FAST_DECAY, D_MODEL,
                                  dtype=jnp.float32))
    decay = jnp.exp(-t[:, None] * deltas)
    h = h * decay[:, None]
    return h[:, 0], h[:, 1]


def fft_long_conv(u, h_f, h_b, bias):
    length = u.shape[1]
    h2 = jnp.concatenate([h_f[:1] + h_b[:1], h_f[1:], jnp.zeros_like(h_f[:1]),
                          jnp.flip(h_b[1:], axis=0)], axis=0)
    u_hat = jnp.fft.rfft(u, n=2 * length, axis=1)
    h_hat = jnp.fft.rfft(h2, axis=0)
    y = jnp.fft.irfft(u_hat * h_hat[None], n=2 * length, axis=1)[:, :length]
    return y + u * bias


def hyena_mixer(h_lat, h_ctx, w_in, conv_w, conv_b, f_w1, f_b1, f_w2, f_b2, f_w3, f_freq, f_bias, w_out):
    def run(h):
        length = h.shape[1]
        u = depthwise_conv_centred(h.astype(jnp.float32) @ w_in, conv_w, conv_b)
        x0, x1, v = jnp.split(u, 3, axis=-1)
        h_f, h_b = hyena_filter(length, f_w1, f_b1, f_w2, f_b2, f_w3, f_freq)
        y = x0 * fft_long_conv(v * x1, h_f, h_b, f_bias)
        return y @ w_out

    return run(h_lat), run(h_ctx)


def mlstm_chunk_scan(q, k, v, i_pre, log_f, state):
    bsz, length, heads, _ = q.shape
    n_chunks = length // ML_CHUNK

    def chunked(t):
        return t.reshape((bsz, n_chunks, ML_CHUNK) + t.shape[2:]).swapaxes(0, 1)

    causal = jnp.tril(jnp.ones((ML_CHUNK, ML_CHUNK), dtype=bool))[None, :, :, None]

    def step(carry, inp):
        c_mat, n_vec, m = carry
        qc, kc, vc, ic, fc = inp
        b = jnp.cumsum(fc, axis=1)
        b_end = b[:, -1]
        d_intra = jnp.where(causal, b[:, :, None] - b[:, None] + ic[:, None], -jnp.inf)
        d_inter = b + m[:, None]
        m_tok = jnp.maximum(jnp.max(d_intra, axis=2), d_inter)
        w_inter = jnp.exp(d_inter - m_tok)
        s = jnp.einsum('bihd,bjhd->bijh', qc, kc) * jnp.exp(d_intra - m_tok[:, :, None])
        num = jnp.einsum('bijh,bjhv->bihv', s, vc) + w_inter[..., None] * jnp.einsum('bihd,bhdv->bihv', qc, c_mat)
        den = jnp.sum(s, axis=2) + w_inter * jnp.einsum('bihd,bhd->bih', qc, n_vec)
        h = num / jnp.maximum(jnp.abs(den), jnp.exp(-m_tok))[..., None]
        d_state = b_end[:, None] - b + ic
        m_new = jnp.maximum(b_end + m, jnp.max(d_state, axis=1))
        w_prev = jnp.exp(b_end + m - m_new)
        w_tok = jnp.exp(d_state - m_new[:, None])
        c_mat = w_prev[..., None, None] * c_mat + jnp.einsum('bjh,bjhd,bjhv->bhdv', w_tok, kc, vc)
        n_vec = w_prev[..., None] * n_vec + jnp.einsum('bjh,bjhd->bhd', w_tok, kc)
        return (c_mat, n_vec, m_new), h

    state, h = lax.scan(step, state, (chunked(q), chunked(k), chunked(v), chunked(i_pre), chunked(log_f)))
    return h.swapaxes(0, 1).reshape(bsz, length, heads, v.shape[-1]), state


def mlstm_mixer(h_lat, h_ctx, w_in, conv_w, conv_b, w_q, w_k, w_v, w_gates, b_gates, norm_g, skip, w_out):
    def project(h):
        bsz, length, _ = h.shape
        x_m, z = jnp.split(h.astype(jnp.float32) @ w_in, 2, axis=-1)
        x_c = jax.nn.silu(depthwise_conv_centred(x_m, conv_w, conv_b))

        def block_diag(t, w):
            t = t.reshape(bsz, length, ML_NBLOCKS, ML_BLOCK)
            return jnp.einsum('blnc,ncd->blnd', t, w).reshape(bsz, length, ML_INNER)

        q, k, v = block_diag(x_c, w_q), block_diag(x_c, w_k), block_diag(x_m, w_v)
        gates = jnp.einsum('blc,zcg->blzg', jnp.concatenate([q, k, v], axis=-1), w_gates) + b_gates
        i_pre = gates[..., :ML_HEADS]
        log_f = jax.nn.log_sigmoid(gates[..., ML_HEADS:])
        heads = lambda t: t.reshape(bsz, length, ML_HEADS, ML_DH)
        return heads(q), heads(k) * ML_DH ** -0.5, heads(v), i_pre, log_f, x_c, z

    def bidirectional(h, state_f, state_b):
        q, k, v, i_pre, log_f, x_c, z = project(h)
        h_f, state_f = mlstm_chunk_scan(q, k, v, i_pre[:, :, 0], log_f[:, :, 0], state_f)
        h_b, state_b = mlstm_chunk_scan(flip_seq(q), flip_seq(k), flip_seq(v), flip_seq(i_pre[:, :, 1]),
                                        flip_seq(log_f[:, :, 1]), state_b)
        h_n = multihead_layer_norm(h_f + flip_seq(h_b), norm_g)
        return ((h_n + skip * x_c) * jax.nn.silu(z)) @ w_out, state_f, state_b

    bsz = h_ctx.shape[0]
    state0 = (jnp.zeros((bsz, ML_HEADS, ML_DH, ML_DH), jnp.float32),
              jnp.zeros((bsz, ML_HEADS, ML_DH), jnp.float32),
              jnp.full((bsz, ML_HEADS), -jnp.inf, jnp.float32))
    y_ctx, st_f, st_b = bidirectional(h_ctx, state0, state0)
    y_lat, _, _ = bidirectional(h_lat, st_f, st_b)
    return y_lat, y_ctx


def _linear_recurrence_combine(left, right):
    a_l, b_l = left
    a_r, b_r = right
    return a_l * a_r, a_r * b_l + b_r


def s5_mixer(h_lat, h_ctx, lam_re, lam_im, log_dt, b_re, b_im, c_re, c_im, d_skip, w_glu):
    f32 = jnp.float32
    b_mat = lax.complex(b_re.astype(f32), b_im.astype(f32))
    c_mat = lax.complex(c_re.astype(f32), c_im.astype(f32))

    def discretise(direction):
        lam = lax.complex(lam_re[direction].astype(f32), lam_im[direction].astype(f32))
        lam_bar = jnp.exp(lam * jnp.exp(log_dt[direction].astype(f32))[:, None])
        return lam_bar, ((lam_bar - 1.0) / lam)[..., None] * b_mat

    disc = (discretise(0), discretise(1))

    def scan_readout(u_seq, lam_bar, b_bar, x0):
        bu = jnp.einsum('gpc,lbgc->lbgp', b_bar, u_seq)
        a = jnp.broadcast_to(lam_bar, (u_seq.shape[0], 1) + lam_bar.shape)
        a_cum, xs = lax.associative_scan(_linear_recurrence_combine, (a, bu), axis=0)
        xs = xs + a_cum * x0
        return jnp.real(jnp.einsum('gcp,lbgp->lbgc', c_mat, xs)), xs[-1]

    def bidirectional(h, x0_f, x0_b):
        bsz, length, _ = h.shape
        u = h.astype(f32)
        u_seq = u.swapaxes(0, 1).reshape(length, bsz, S5_GROUPS, S5_GROUP).astype(jnp.complex64)
        y_f, x_f = scan_readout(u_seq, disc[0][0], disc[0][1], x0_f)
        y_b, x_b = scan_readout(jnp.flip(u_seq, axis=0), disc[1][0], disc[1][1], x0_b)
        y = (y_f + jnp.flip(y_b, axis=0)).reshape(length, bsz, D_MODEL).swapaxes(0, 1) + d_skip * u
        val, gate = jnp.split(jax.nn.gelu(y) @ w_glu, 2, axis=-1)
        return val * jax.nn.sigmoid(gate), x_f, x_b

    x0 = jnp.zeros((h_ctx.shape[0], S5_GROUPS, S5_STATE), jnp.complex64)
    y_ctx, x_f, x_b = bidirectional(h_ctx, x0, x0)
    y_lat, _, _ = bidirectional(h_lat, x_f, x_b)
    return y_lat, y_ctx


def setup_inputs(seed: int = 0) -> dict:
    key = jax.random.key(seed)
    keys = iter(jax.random.split(key, 96))
    f32 = jnp.float32

    def normal(shape, scale=1.0):
        return scale * jax.random.normal(next(keys), shape, f32)

    def gain(shape):
        return 1.0 + normal(shape, 0.02)

    D, F = D_MODEL, FFN_HIDDEN
    ml_b_i = normal((N_ML, 2, ML_HEADS), 0.1)
    ml_b_f = jnp.linspace(3.0, 6.0, ML_HEADS, dtype=f32) + normal((N_ML, 2, ML_HEADS), 0.1)
    lam_im_init = math.pi * jnp.arange(S5_STATE, dtype=f32)
    return {
        'x': normal((BATCH, SEQ, D)),
        'c': normal((BATCH, D)),
        'ctx': normal((BATCH, CTX_LEN, D)),
        'c_ctx': normal((D,)),
        'mod_w': normal((DEPTH, D, N_MOD * D), D ** -0.5),
        'mod_b': normal((DEPTH, N_MOD * D), 0.01),
        'norm_g': gain((DEPTH, 4, D)),
        'ffn_w_in': normal((DEPTH, D, 2 * F), D ** -0.5),
        'ffn_w_out': normal((DEPTH, F, D), F ** -0.5),
        'gla_w_in': normal((N_GLA, D, GLA_IN), D ** -0.5),
        'gla_w_gate': normal((N_GLA, 2, GLA_RANK, GLA_QK), GLA_RANK ** -0.5),
        'gla_b_gate': normal((N_GLA, 2, GLA_QK), 0.1),
        'gla_norm_g': gain((N_GLA, GLA_DV)),
        'gla_w_out': normal((N_GLA, GLA_VD, D), GLA_VD ** -0.5),
        'hy_w_in': normal((N_HY, D, 3 * D), D ** -0.5),
        'hy_conv_w': normal((N_HY, HY_SHORT, 3 * D), HY_SHORT ** -0.5),
        'hy_conv_b': normal((N_HY, 3 * D), 0.01),
        'hy_f_w1': normal((N_HY, HY_EMB, HY_FILTER_DIM), HY_EMB ** -0.5),
        'hy_f_b1': normal((N_HY, HY_FILTER_DIM), 0.1),
        'hy_f_w2': normal((N_HY, HY_FILTER_DIM, HY_FILTER_DIM), HY_FILTER_DIM ** -0.5),
        'hy_f_b2': normal((N_HY, HY_FILTER_DIM), 0.1),
        'hy_f_w3': normal((N_HY, HY_FILTER_DIM, 2 * D), HY_FILTER_DIM ** -0.5),
        'hy_f_freq': gain((N_HY, HY_FILTER_DIM)),
        'hy_f_bias': normal((N_HY, D), 0.1),
        'hy_w_out': normal((N_HY, D, D), D ** -0.5),
        'ml_w_in': normal((N_ML, D, 2 * ML_INNER), D ** -0.5),
        'ml_conv_w': normal((N_ML, ML_CONV, ML_INNER), ML_CONV ** -0.5),
        'ml_conv_b': normal((N_ML, ML_INNER), 0.01),
        'ml_w_q': normal((N_ML, ML_NBLOCKS, ML_BLOCK, ML_BLOCK), ML_BLOCK ** -0.5),
        'ml_w_k': normal((N_ML, ML_NBLOCKS, ML_BLOCK, ML_BLOCK), ML_BLOCK ** -0.5),
        'ml_w_v': normal((N_ML, ML_NBLOCKS, ML_BLOCK, ML_BLOCK), ML_BLOCK ** -0.5),
        'ml_w_gates': normal((N_ML, 2, 3 * ML_INNER, 2 * ML_HEADS), (3 * ML_INNER) ** -0.5),
        'ml_b_gates': jnp.concatenate([ml_b_i, ml_b_f], axis=-1),
        'ml_norm_g': gain((N_ML, ML_INNER)),
        'ml_skip': gain((N_ML, ML_INNER)),
        'ml_w_out': normal((N_ML, ML_INNER, D), ML_INNER ** -0.5),
        's5_lam_re': -0.5 + normal((N_S5, 2, S5_GROUPS, S5_STATE), 0.01),
        's5_lam_im': lam_im_init + normal((N_S5, 2, S5_GROUPS, S5_STATE), 0.01),
        's5_log_dt': jax.random.uniform(next(keys), (N_S5, 2, S5_GROUPS), f32,
                                        minval=math.log(S5_DT_MIN), maxval=math.log(S5_DT_MAX)),
        's5_b_re': normal((N_S5, S5_GROUPS, S5_STATE, S5_GROUP), (2 * S5_GROUP) ** -0.5),
        's5_b_im': normal((N_S5, S5_GROUPS, S5_STATE, S5_GROUP), (2 * S5_GROUP) ** -0.5),
        's5_c_re': normal((N_S5, S5_GROUPS, S5_GROUP, S5_STATE), (2 * S5_STATE) ** -0.5),
        's5_c_im': normal((N_S5, S5_GROUPS, S5_GROUP, S5_STATE), (2 * S5_STATE) ** -0.5),
        's5_d': normal((N_S5, D)),
        's5_w_glu': normal((N_S5, D, 2 * D), D ** -0.5),
    }


def reference(x, c, ctx, c_ctx, mod_w, mod_b, norm_g, ffn_w_in, ffn_w_out,
              gla_w_in, gla_w_gate, gla_b_gate, gla_norm_g, gla_w_out,
              hy_w_in, hy_conv_w, hy_conv_b, hy_f_w1, hy_f_b1, hy_f_w2, hy_f_b2, hy_f_w3, hy_f_freq, hy_f_bias,
              hy_w_out,
              ml_w_in, ml_conv_w, ml_conv_b, ml_w_q, ml_w_k, ml_w_v, ml_w_gates, ml_b_gates, ml_norm_g, ml_skip,
              ml_w_out,
              s5_lam_re, s5_lam_im, s5_log_dt, s5_b_re, s5_b_im, s5_c_re, s5_c_im, s5_d, s5_w_glu):
    bsz = x.shape[0]
    rows = x.shape[1] // GRID_W
    lat, cx = x, ctx
    silu_c = jax.nn.silu(c.astype(jnp.float32))
    silu_cc = jax.nn.silu(c_ctx.astype(jnp.float32))
    for i in range(DEPTH):
        kind, j = i % N_MIXERS, i // N_MIXERS
        last = i == DEPTH - 1
        mod_l = (silu_c @ mod_w[i] + mod_b[i]).reshape(bsz, N_MOD, 1, D_MODEL)
        mod_c = (silu_cc @ mod_w[i] + mod_b[i]).reshape(N_MOD, 1, D_MODEL)
        h_lat = modulate(rms_norm(lat, norm_g[i, 0]), mod_l[:, 0], mod_l[:, 1])
        h_ctx = modulate(rms_norm(cx, norm_g[i, 0]), mod_c[0], mod_c[1])
        if kind == 0:
            y_lat, y_ctx = gla_mixer(h_lat, h_ctx, gla_w_in[j], gla_w_gate[j], gla_b_gate[j], gla_norm_g[j],
                                     gla_w_out[j])
        elif kind == 1:
            y_lat, y_ctx = hyena_mixer(h_lat, h_ctx, hy_w_in[j], hy_conv_w[j], hy_conv_b[j], hy_f_w1[j],
                                       hy_f_b1[j], hy_f_w2[j], hy_f_b2[j], hy_f_w3[j], hy_f_freq[j],
                                       hy_f_bias[j], hy_w_out[j])
        elif kind == 2:
            y_lat, y_ctx = mlstm_mixer(to_col_major(h_lat, rows), h_ctx, ml_w_in[j], ml_conv_w[j], ml_conv_b[j],
                                       ml_w_q[j], ml_w_k[j], ml_w_v[j], ml_w_gates[j], ml_b_gates[j],
                                       ml_norm_g[j], ml_skip[j], ml_w_out[j])
            y_lat = from_col_major(y_lat, rows)
        else:
            y_lat, y_ctx = s5_mixer(to_col_major(h_lat, rows), h_ctx, s5_lam_re[j], s5_lam_im[j], s5_log_dt[j],
                                    s5_b_re[j], s5_b_im[j], s5_c_re[j], s5_c_im[j], s5_d[j], s5_w_glu[j])
            y_lat = from_col_major(y_lat, rows)
        lat = lat + (mod_l[:, 2] * rms_norm(y_lat, norm_g[i, 1])).astype(lat.dtype)
        h = modulate(rms_norm(lat, norm_g[i, 2]), mod_l[:, 3], mod_l[:, 4])
        lat = lat + (mod_l[:, 5] * rms_norm(swiglu(h, ffn_w_in[i], ffn_w_out[i]), norm_g[i, 3])).astype(lat.dtype)
        if not last:
            cx = cx + (mod_c[2] * rms_norm(y_ctx, norm_g[i, 1])).astype(cx.dtype)
            hc = modulate(rms_norm(cx, norm_g[i, 2]), mod_c[3], mod_c[4])
            cx = cx + (mod_c[5] * rms_norm(swiglu(hc, ffn_w_in[i], ffn_w_out[i]), norm_g[i, 3])).astype(cx.dtype)
    return lat
```

```python
import ml_dtypes


import numpy as np
import math
import concourse.bass as bass
import concourse.mybir as mybir
from concourse.bass_utils import run_bass_kernel_spmd

F32 = mybir.dt.float32
BF16 = mybir.dt.bfloat16
AF = mybir.ActivationFunctionType
ALU = mybir.AluOpType
AX = mybir.AxisListType
ENGS = ('tensor', 'vector', 'scalar', 'gpsimd', 'sync')
SEM_ROT = 30000
STORE_Q = 'gpsimd'


class Dep:
    __slots__ = ('w', 'r', 'const', 'excl')

    def __init__(self, const=False, excl=False):
        self.w = None
        self.r = {}
        self.const = const
        self.excl = excl


def PDep():
    return Dep(excl=True)


class KB:
    def __init__(self, nc, n_dma_sems=24):
        self.nc = nc
        self.prog = {e: [] for e in ENGS}
        self.cnt = {e: 0 for e in ENGS}
        self.sem = {e: nc.alloc_semaphore(name=f"s_{e}_0") for e in ENGS}
        self.semgen = {e: 0 for e in ENGS}
        self.seen = {e: {} for e in ENGS}
        self.dsem = [nc.alloc_semaphore(name=f"s_dma_{i}") for i in range(n_dma_sems)]
        self.dcnt = [0] * n_dma_sems
        self.drr = 0
        self.ninst = 0
        self.last_ev = {}

    def _wait(self, eng, ev):
        sem, val = ev
        key = id(sem)
        if self.seen[eng].get(key, 0) >= val:
            return
        self.seen[eng][key] = val
        self.prog[eng].append(lambda e, sem=sem, val=val: e.wait_ge(sem, val))

    def _deps(self, eng, R, W):
        evs = []
        for d in R:
            if d.w is not None:
                evs.append(d.w)
        for d in W:
            if d.w is not None:
                evs.append(d.w)
            evs.extend(d.r.values())
        for ev in evs:
            if eng == 'tensor' and ev[2] == 'tensor':
                continue
            self._wait(eng, ev[:2])

    def _record(self, ev, R, W):
        for d in W:
            d.w = ev
            d.r = {}
        for d in R:
            if not d.const:
                d.r[id(ev[0])] = ev

    def op(self, eng, fn, R=(), W=()):
        ex = [d for d in R if d.excl]
        if ex:
            R = [d for d in R if not d.excl]
            W = list(W) + ex
        self._deps(eng, R, W)
        if self.cnt[eng] >= SEM_ROT:
            self.semgen[eng] += 1
            self.sem[eng] = self.nc.alloc_semaphore(name=f"s_{eng}_{self.semgen[eng]}")
            self.cnt[eng] = 0
        self.cnt[eng] += 1
        sem, val = self.sem[eng], self.cnt[eng]
        self.prog[eng].append(lambda e, fn=fn, sem=sem: fn(e).then_inc(sem, 1))
        self._record((sem, val, eng), R, W)
        self.last_ev[eng] = (sem, val)
        self.ninst += 1

    def I(self, eng, name, *args, R=(), W=(), **kw):
        self.op(eng, lambda e, name=name, args=args, kw=kw: getattr(e, name)(*args, **kw), R=R, W=W)

    def dma(self, out, in_, R=(), W=(), eng=None, **kw):
        if eng is None:
            eng = STORE_Q if (str(out.space) == 'DRAM' and str(in_.space) != 'DRAM') else 'sync'
        self._deps(eng, R, W)
        i = self.drr
        self.drr = (self.drr + 1) % len(self.dsem)
        sem = self.dsem[i]
        if self.dcnt[i] > 0:
            self._wait(eng, (sem, self.dcnt[i]))
        self.dcnt[i] += 16
        val = self.dcnt[i]
        self.prog[eng].append(lambda e, sem=sem, out=out, in_=in_, kw=kw: e.dma_start(out=out, in_=in_, **kw).then_inc(sem, 16))
        self._record((sem, val, 'dma'), R, W)
        self.ninst += 1

    def barrier(self):
        for e in ENGS:
            for e2 in ENGS:
                if e2 != e and e2 in self.last_ev:
                    self._wait(e, self.last_ev[e2])
            for i, s in enumerate(self.dsem):
                if self.dcnt[i] > 0:
                    self._wait(e, (s, self.dcnt[i]))

    def finish(self):
        for e2 in ENGS:
            if e2 != 'sync' and e2 in self.last_ev:
                self._wait('sync', self.last_ev[e2])
        for i, s in enumerate(self.dsem):
            if self.dcnt[i] > 0:
                self._wait('sync', (s, self.dcnt[i]))
        with self.nc.Block() as block:
            for e in ENGS:
                lst = self.prog[e]
                if not lst:
                    continue
                getattr(block, e)(lambda engobj, lst=lst: [f(engobj) for f in lst])


from contextlib import ExitStack

D = 1024
LAT = 4096
CTXL = 256
FH = 2816
EPS = 1e-6


class Env:
    def __init__(self, name="k"):
        self.nc = bass.Bass("TRN2", target_bir_lowering=False)
        self.kb = KB(self.nc)
        self.es = ExitStack()
        self.n = 0

    def sb(self, shape, dt=F32, name="t", es=None):
        self.n += 1
        return (es or self.es).enter_context(self.nc.sbuf_tensor(f"{name}_{self.n}", list(shape), dt))

    def ps(self, shape, dt=F32, name="p", es=None):
        self.n += 1
        return (es or self.es).enter_context(self.nc.psum_tensor(f"{name}_{self.n}", list(shape), dt))

    def dram(self, name, shape, dt=F32, kind="Internal"):
        return self.nc.dram_tensor(name, list(shape), dt, kind=kind).ap()


def setup_consts(E, ident_dram):
    kb = E.kb
    E.idf = E.sb([128, 128], F32, "idf"); E.d_idf = Dep(const=True)
    E.idb = E.sb([128, 128], BF16, "idb"); E.d_idb = Dep(const=True)
    E.ones = E.sb([128, 512], F32, "ones"); E.d_ones = Dep(const=True)
    E.onesb = E.sb([128, 512], BF16, "onesb"); E.d_onesb = Dep(const=True)
    E.epsc = E.sb([128, 1], F32, "epsc"); E.d_eps = Dep(const=True)
    kb.dma(E.idf[:], ident_dram, W=[E.d_idf])
    kb.I('vector', 'tensor_copy', out=E.idb[:], in_=E.idf[:], R=[E.d_idf], W=[E.d_idb])
    kb.I('vector', 'memset', E.ones[:], 1.0, W=[E.d_ones])
    kb.I('vector', 'memset', E.onesb[:], 1.0, W=[E.d_onesb])
    kb.I('vector', 'memset', E.epsc[:], EPS, W=[E.d_eps])


def alloc_mod(E):
    P = {}
    for nm, shp in (("sc", [128, 8]), ("gcol", [128, 8]), ("bcol", [128, 2, 8]), ("Gcol", [128, 8]), ("Scol", [128, 8]),
                    ("GG", [128, D])):
        P[nm] = E.sb(shp, F32, nm)
    return P


def compute_mod(E, P, cvec, mod_w, mod_b, norm_g, h, es, pcol, d_pcol, pm, d_pm):
    kb, nc = E.kb, E.nc
    sc = P["sc"]; d_sc = Dep()
    kb.dma(sc[:], cvec.rearrange("(k p) -> p k", p=128), W=[d_sc], allow_slow_non_contiguous=True)
    kb.I('scalar', 'activation', out=sc[:], in_=sc[:], func=AF.Silu, R=[d_sc], W=[d_sc])
    gcol = P["gcol"]; d_gcol = Dep()
    bcol = P["bcol"]; d_bcol = Dep()
    kb.dma(gcol[:], norm_g[2 * h].rearrange("(k p) -> p k", p=128), W=[d_gcol], allow_slow_non_contiguous=True)
    kb.dma(bcol[:], mod_b[3 * h * D:(3 * h + 2) * D].rearrange("(j k p) -> p j k", p=128, k=8), W=[d_bcol],
           allow_slow_non_contiguous=True)
    Gcol = P["Gcol"]; Scol = P["Scol"]; d_G = Dep(); d_S = Dep()
    mwv = mod_w.rearrange("(k p) c -> p k c", p=128)
    wt = [E.sb([128, 8, 256], F32, "mwt", es=es) for _ in range(2)]
    d_wt = [Dep(), Dep()]
    nld = 0
    for j in range(2):
        for half in range(4):
            c0 = (3 * h + j) * D + half * 256
            b = nld % 2; nld += 1
            kb.dma(wt[b][:], mwv[:, :, c0:c0 + 256], W=[d_wt[b]])
            for oc in range(2):
                col = j * 8 + half * 2 + oc
                for k in range(8):
                    kb.I('tensor', 'matmul', out=pcol[:, col:col + 1], lhsT=wt[b][:, k, oc * 128:(oc + 1) * 128], rhs=sc[:, k:k + 1],
                        start=(k == 0), stop=(k == 7), R=[d_wt[b], d_sc], W=[d_pcol])
    kb.I('vector', 'tensor_tensor', out=Scol[:], in0=pcol[:, 0:8], in1=bcol[:, 0, :], op=ALU.add, R=[d_pcol, d_bcol], W=[d_S])
    kb.I('vector', 'scalar_tensor_tensor', out=Gcol[:], in0=pcol[:, 8:16], scalar=1.0, in1=bcol[:, 1, :],
                                                     op0=ALU.add, op1=ALU.add, R=[d_pcol, d_bcol], W=[d_G])
    kb.I('vector', 'tensor_tensor', out=Gcol[:], in0=Gcol[:], in1=gcol[:], op=ALU.mult, R=[d_G, d_gcol], W=[d_G])
    rep = E.sb([128, 8, 128], F32, "rep", es=es); d_rep = Dep()
    for k in range(8):
        kb.I('vector', 'tensor_scalar_mul', out=rep[:, k, :], in0=E.ones[:, 0:128], scalar1=sc[:, k:k + 1], R=[d_sc, E.d_ones], W=[d_rep])
    rows = E.sb([1, 2, D], F32, "rows", es=es); d_rows = Dep()
    kb.dma(rows[0:1, 0, :], mod_b[(3 * h + 2) * D:(3 * h + 3) * D].rearrange("(o c) -> o c", o=1), W=[d_rows])
    kb.dma(rows[0:1, 1, :], norm_g[2 * h + 1].rearrange("(o c) -> o c", o=1), W=[d_rows])
    GG = P["GG"]; d_GG = Dep()
    gb = E.sb([128, D], F32, "gbbc", es=es); d_gb = Dep()
    for half in range(2):
        kb.I('tensor', 'matmul', out=pm[:], lhsT=E.ones[0:1, 0:128],
                                                      rhs=rows[0:1, 1, half * 512:(half + 1) * 512], start=True, stop=True, R=[d_rows, E.d_ones], W=[d_pm])
        kb.I('vector', 'tensor_copy', out=gb[:, half * 512:(half + 1) * 512], in_=pm[:], R=[d_pm], W=[d_gb])
    for q in range(4):
        c0 = (3 * h + 2) * D + q * 256
        b = nld % 2; nld += 1
        kb.dma(wt[b][:], mwv[:, :, c0:c0 + 256], W=[d_wt[b]])
        for k in range(8):
            kb.I('tensor', 'matmul', out=pm[:, 0:256], lhsT=rep[:, k, :], rhs=wt[b][:, k, :],
                                                         start=(k == 0), stop=False, R=[d_wt[b], d_rep], W=[d_pm])
        kb.I('tensor', 'matmul', out=pm[:, 0:256], lhsT=E.ones[0:1, 0:128],
                                                rhs=rows[0:1, 0, q * 256:(q + 1) * 256], start=False, stop=True, R=[d_rows, E.d_ones], W=[d_pm])
        kb.I('vector', 'tensor_tensor', out=GG[:, q * 256:(q + 1) * 256], in0=pm[:, 0:256],
                                                       in1=gb[:, q * 256:(q + 1) * 256], op=ALU.mult, R=[d_pm, d_gb], W=[d_GG])
    return Gcol, Scol, GG, (d_G, d_S, d_GG)


class NormT:
    def __init__(self, E, nbuf=2):
        self.E = E
        self.sq = [E.sb([128, D], F32, "sqj") for _ in range(nbuf)]
        self.ss = [E.sb([128, 2], F32, "ss") for _ in range(nbuf)]
        self.xb = [E.sb([128, D], BF16, "xb") for _ in range(nbuf)]
        self.pT = [E.ps([128, 8, 128], BF16, "pT") for _ in range(1)]
        self.d_sq = [Dep() for _ in range(nbuf)]
        self.d_ss = [Dep() for _ in range(nbuf)]
        self.d_xb = [Dep() for _ in range(nbuf)]
        self.d_pT = [PDep()]
        self.i = 0

    def run(self, xt, d_xt, Gcol, Scol, dGS, out_fn, d_out):
        E, kb = self.E, self.E.kb
        b = self.i % len(self.sq); self.i += 1
        sq, ss, xb, pT = self.sq[b], self.ss[b], self.xb[b], self.pT[0]
        kb.I('scalar', 'activation', out=sq[:], in_=xt, func=AF.Square, accum_out=ss[:, 0:1], R=[d_xt], W=[self.d_sq[b], self.d_ss[b]])
        kb.I('scalar', 'activation', out=ss[:, 1:2], in_=ss[:, 0:1], func=AF.Sqrt, bias=E.epsc[:, 0:1], scale=1.0 / D, R=[self.d_ss[b], E.d_eps], W=[self.d_ss[b]])
        kb.I('vector', 'reciprocal', out=ss[:, 1:2], in_=ss[:, 1:2], R=[self.d_ss[b]], W=[self.d_ss[b]])
        kb.I('vector', 'tensor_scalar_mul', out=xb[:], in0=xt, scalar1=ss[:, 1:2], R=[d_xt, self.d_ss[b]], W=[self.d_xb[b]])
        for k in range(8):
            kb.I('tensor', 'transpose', out=pT[:, k, :], in_=xb[:, k * 128:(k + 1) * 128], identity=E.idb[:], R=[self.d_xb[b], E.d_idb], W=[self.d_pT[0]])
        for k in range(8):
            kb.I('scalar', 'activation', out=out_fn(k), in_=pT[:, k, :], func=AF.Identity,
                                                        bias=Scol[:, k:k + 1], scale=Gcol[:, k:k + 1], R=[self.d_pT[0]] + list(dGS), W=[d_out])


class PostRes:
    def __init__(self, E, nbuf=2):
        self.E = E
        self.sq = [E.sb([128, D], F32, "psq") for _ in range(nbuf)]
        self.ss = [E.sb([128, 2], F32, "pss") for _ in range(nbuf)]
        self.d_sq = [Dep() for _ in range(nbuf)]
        self.d_ss = [Dep() for _ in range(nbuf)]
        self.i = 0

    def run(self, y, d_y, xt, d_xt, GG, d_GG):
        E, kb = self.E, self.E.kb
        b = self.i % len(self.sq); self.i += 1
        sq, ss = self.sq[b], self.ss[b]
        kb.I('scalar', 'activation', out=sq[:], in_=y, func=AF.Square, accum_out=ss[:, 0:1], R=[d_y], W=[self.d_sq[b], self.d_ss[b]])
        kb.I('scalar', 'activation', out=ss[:, 1:2], in_=ss[:, 0:1], func=AF.Sqrt, bias=E.epsc[:, 0:1], scale=1.0 / D, R=[self.d_ss[b], E.d_eps], W=[self.d_ss[b]])
        kb.I('vector', 'reciprocal', out=ss[:, 1:2], in_=ss[:, 1:2], R=[self.d_ss[b]], W=[self.d_ss[b]])
        kb.I('vector', 'scalar_tensor_tensor', out=sq[:], in0=y, scalar=ss[:, 1:2], in1=GG[:],
                                                         op0=ALU.mult, op1=ALU.mult, R=[d_y, self.d_ss[b], d_GG], W=[self.d_sq[b]])
        kb.I('gpsimd', 'tensor_tensor', out=xt, in0=xt, in1=sq[:], op=ALU.add, R=[self.d_sq[b], d_xt], W=[d_xt])


class Ring:
    def __init__(self, E, shape, dt, n=1, name="r", psum=False, es=None):
        mk = E.ps if psum else E.sb
        self.t = [mk(shape, dt, name, es=es) for _ in range(n)]
        self.d = [Dep() for _ in range(n)]
        self.i = -1

    def next(self):
        self.i = (self.i + 1) % len(self.t)
        return self.t[self.i], self.d[self.i]

    def cur(self):
        return self.t[self.i], self.d[self.i]


def compute_mod2(E, Pl, Pc, cl, cc, mod_w, mod_b, norm_g, h, es, pcol, d_pcol, pm, d_pm):
    kb = E.kb
    PP = (Pl, Pc)
    sc2 = E.sb([128, 8, 2], F32, "sc2", es=es); d_sc = Dep()
    kb.dma(sc2[:, :, 0], cl.rearrange("(k p) -> p k", p=128), W=[d_sc], allow_slow_non_contiguous=True)
    kb.dma(sc2[:, :, 1], cc.rearrange("(k p) -> p k", p=128), W=[d_sc], allow_slow_non_contiguous=True)
    kb.I('scalar', 'activation', out=sc2[:], in_=sc2[:], func=AF.Silu, R=[d_sc], W=[d_sc])
    gcol = Pl["gcol"]; d_gcol = Dep(); bcol = Pl["bcol"]; d_bcol = Dep()
    kb.dma(gcol[:], norm_g[2 * h].rearrange("(k p) -> p k", p=128), W=[d_gcol], allow_slow_non_contiguous=True)
    kb.dma(bcol[:], mod_b[3 * h * D:(3 * h + 2) * D].rearrange("(j k p) -> p j k", p=128, k=8), W=[d_bcol],
           allow_slow_non_contiguous=True)
    d_G = [Dep(), Dep()]; d_S = [Dep(), Dep()]; d_GG = [Dep(), Dep()]
    mwv = mod_w.rearrange("(k p) c -> p k c", p=128)
    wt = [E.sb([128, 8, 256], F32, "mwt", es=es) for _ in range(2)]
    d_wt = [Dep(), Dep()]
    nld = 0
    for j in range(2):
        for half in range(4):
            c0 = (3 * h + j) * D + half * 256
            b = nld % 2; nld += 1
            kb.dma(wt[b][:], mwv[:, :, c0:c0 + 256], W=[d_wt[b]])
            for oc in range(2):
                col = j * 8 + half * 2 + oc
                for k in range(8):
                    kb.I('tensor', 'matmul', out=pcol[:, 2 * col:2 * col + 2], lhsT=wt[b][:, k, oc * 128:(oc + 1) * 128], rhs=sc2[:, k, :],
                         start=(k == 0), stop=(k == 7), R=[d_wt[b], d_sc], W=[d_pcol])
    pc3 = pcol[:, 0:32].rearrange("p (c x) -> p c x", x=2)
    for x in range(2):
        P = PP[x]
        kb.I('vector', 'tensor_tensor', out=P["Scol"][:], in0=pc3[:, 0:8, x], in1=bcol[:, 0, :], op=ALU.add, R=[d_pcol, d_bcol], W=[d_S[x]])
        kb.I('vector', 'scalar_tensor_tensor', out=P["Gcol"][:], in0=pc3[:, 8:16, x], scalar=1.0, in1=bcol[:, 1, :], op0=ALU.add, op1=ALU.add,
             R=[d_pcol, d_bcol], W=[d_G[x]])
        kb.I('vector', 'tensor_tensor', out=P["Gcol"][:], in0=P["Gcol"][:], in1=gcol[:], op=ALU.mult, R=[d_G[x], d_gcol], W=[d_G[x]])
    rep = [E.sb([128, 8, 128], F32, "rep", es=es) for _ in range(2)]; d_rep = Dep()
    for x in range(2):
        for k in range(8):
            kb.I('vector', 'tensor_scalar_mul', out=rep[x][:, k, :], in0=E.ones[:, 0:128], scalar1=sc2[:, k, x:x + 1], R=[d_sc, E.d_ones], W=[d_rep])
    rows = E.sb([1, 2, D], F32, "rows", es=es); d_rows = Dep()
    kb.dma(rows[0:1, 0, :], mod_b[(3 * h + 2) * D:(3 * h + 3) * D].rearrange("(o c) -> o c", o=1), W=[d_rows])
    kb.dma(rows[0:1, 1, :], norm_g[2 * h + 1].rearrange("(o c) -> o c", o=1), W=[d_rows])
    gb = E.sb([128, D], F32, "gbbc", es=es); d_gb = Dep()
    for half in range(2):
        kb.I('tensor', 'matmul', out=pm[:], lhsT=E.ones[0:1, 0:128], rhs=rows[0:1, 1, half * 512:(half + 1) * 512], start=True, stop=True,
             R=[d_rows, E.d_ones], W=[d_pm])
        kb.I('vector', 'tensor_copy', out=gb[:, half * 512:(half + 1) * 512], in_=pm[:], R=[d_pm], W=[d_gb])
    pmx = (pm, pcol); d_pmx = (d_pm, d_pcol)
    for q in range(4):
        c0 = (3 * h + 2) * D + q * 256
        b = nld % 2; nld += 1
        kb.dma(wt[b][:], mwv[:, :, c0:c0 + 256], W=[d_wt[b]])
        for x in range(2):
            for k in range(8):
                kb.I('tensor', 'matmul', out=pmx[x][:, 0:256], lhsT=rep[x][:, k, :], rhs=wt[b][:, k, :], start=(k == 0), stop=False,
                     R=[d_wt[b], d_rep], W=[d_pmx[x]])
            kb.I('tensor', 'matmul', out=pmx[x][:, 0:256], lhsT=E.ones[0:1, 0:128], rhs=rows[0:1, 0, q * 256:(q + 1) * 256], start=False, stop=True,
                 R=[d_rows, E.d_ones], W=[d_pmx[x]])
            kb.I('vector', 'tensor_tensor', out=PP[x]["GG"][:, q * 256:(q + 1) * 256], in0=pmx[x][:, 0:256], in1=gb[:, q * 256:(q + 1) * 256],
                 op=ALU.mult, R=[d_pmx[x], d_gb], W=[d_GG[x]])
    return ((Pl["Gcol"], Pl["Scol"], Pl["GG"], (d_G[0], d_S[0], d_GG[0])), (Pc["Gcol"], Pc["Scol"], Pc["GG"], (d_G[1], d_S[1], d_GG[1])))


def ffn_weights_prep(E, wo, w_in, w_out, es):
    kb = E.kb
    E.n += 1
    wsc = E.dram(f"wsc_{E.n}", [11, 128, 8 * 2 * 256], BF16)
    d_wo = Dep()
    stg = [E.sb([128, 2048], F32, "stg", es=es) for _ in range(2)]; d_stg = [Dep(), Dep()]
    stb = [E.sb([128, 8, 2, 256], BF16, "stb", es=es) for _ in range(2)]; d_stb = [Dep(), Dep()]
    wov = w_out.rearrange("(f p) c -> p f c", p=128)
    n = 0
    for f2 in range(11):
        b = n % 2; n += 1
        kb.dma(stg[b][:].rearrange("p (f c) -> p f c", f=2), wov[:, 2 * f2:2 * f2 + 2, :], W=[d_stg[b]])
        kb.I('gpsimd', 'tensor_copy', out=wo[:, 2 * f2:2 * f2 + 2, :],
                                                           in_=stg[b][:].rearrange("p (f c) -> p f c", f=2), R=[d_stg[b]], W=[d_wo])
    wiv = w_in.rearrange("(k p) c -> p k c", p=128)
    d_wsc = Dep()
    for s in range(11):
        bb = s % 2
        for g in range(2):
            b = n % 2; n += 1
            c0 = g * FH + s * 256
            kb.dma(stg[b][:].rearrange("p (k c) -> p k c", k=8), wiv[:, :, c0:c0 + 256], W=[d_stg[b]])
            kb.I('gpsimd' if g == 0 else 'vector', 'tensor_copy', out=stb[bb][:, :, g, :], in_=stg[b][:].rearrange("p (k c) -> p k c", k=8), R=[d_stg[b]], W=[d_stb[bb]])
        kb.dma(wsc[s], stb[bb][:].rearrange("p k g c -> p (k g c)"), R=[d_stb[bb]], W=[d_wsc])
    return wsc, d_wsc, wo, d_wo


def ffn_run(E, groups, wsc, d_wsc, wo, d_wo, nt, post):
    kb = E.kb
    NX = 8
    xs = [E.sb([128, D], F32, "x") for _ in range(NX)]; d_xs = [Dep() for _ in range(NX)]
    hT = [E.sb([128, 8, 512], BF16, "hT") for _ in range(2)]; d_hT = [Dep(), Dep()]
    wbuf = [E.sb([128, 8, 2, 256], BF16, "wi") for _ in range(4)]; d_wb = [Dep() for _ in range(4)]
    act = E.sb([128, 22, 512], BF16, "act"); d_act = Dep()
    sg = [E.sb([128, 512], F32, "sg") for _ in range(2)]; d_sg = [Dep(), Dep()]
    pg = [E.ps([128, 512], F32, "pg") for _ in range(2)]; d_pg = [PDep(), PDep()]
    pu = [E.ps([128, 512], F32, "pu") for _ in range(2)]; d_pu = [PDep(), PDep()]
    py = E.ps([128, D], F32, "py"); d_py = PDep()
    xi = 0; wi = 0
    for gi, g in enumerate(groups):
        tiles = g['tiles']
        ntk = len(tiles) * 128
        hb = gi % 2
        slots = []
        for ti, (src, dst) in enumerate(tiles):
            s = xi % NX; xi += 1
            slots.append(s)
            kb.dma(xs[s][:], src, W=[d_xs[s]])
            nt.run(xs[s][:], d_xs[s], g['G'], g['S'], g['dGS'],
                   lambda k, ti=ti, hb=hb: hT[hb][:, k, ti * 128:(ti + 1) * 128], d_hT[hb])
        for s in range(11):
            wb = wi % 4; wi += 1
            kb.dma(wbuf[wb][:].rearrange("p k g c -> p (k g c)"), wsc[s], R=[d_wsc], W=[d_wb[wb]])
            for fo in range(2):
                f = 2 * s + fo
                pb = f % 2
                for k in range(8):
                    kb.I('tensor', 'matmul', out=pg[pb][:, 0:ntk], lhsT=wbuf[wb][:, k, 0, fo * 128:(fo + 1) * 128], rhs=hT[hb][:, k, 0:ntk],
                        start=(k == 0), stop=(k == 7), R=[d_wb[wb], d_hT[hb]], W=[d_pg[pb]])
                for k in range(8):
                    kb.I('tensor', 'matmul', out=pu[pb][:, 0:ntk], lhsT=wbuf[wb][:, k, 1, fo * 128:(fo + 1) * 128], rhs=hT[hb][:, k, 0:ntk],
                        start=(k == 0), stop=(k == 7), R=[d_wb[wb], d_hT[hb]], W=[d_pu[pb]])
                kb.I('scalar', 'activation', out=sg[pb][:, 0:ntk], in_=pg[pb][:, 0:ntk], func=AF.Silu, R=[d_pg[pb]], W=[d_sg[pb]])
                kb.I('vector', 'tensor_tensor', out=act[:, f, 0:ntk], in0=pu[pb][:, 0:ntk],
                                                                       in1=sg[pb][:, 0:ntk], op=ALU.mult, R=[d_pu[pb], d_sg[pb]], W=[d_act])
        for ti, (src, dst) in enumerate(tiles):
            s = slots[ti]
            for half in range(2):
                for f in range(22):
                    kb.I('tensor', 'matmul', out=py[:, half * 512:(half + 1) * 512], lhsT=act[:, f, ti * 128:(ti + 1) * 128],
                        rhs=wo[:, f, half * 512:(half + 1) * 512], start=(f == 0), stop=(f == 21), R=[d_act, d_wo], W=[d_py])
            post.run(py[:], d_py, xs[s][:], d_xs[s], g['GG'], g['dGG'])
            kb.dma(dst, xs[s][:], R=[d_xs[s]])


class Caster:
    def __init__(self, E, es):
        self.E = E
        self.stg = Ring(E, [128, 2048], F32, 2, "cstg", es=es)
        self.n = 0

    def cast3(self, dst, d_dst, src, c0, C, scale=None, nk_total=8):
        E, kb = self.E, self.E.kb
        sv = src.rearrange("(k p) c -> p k c", p=128)
        nk = max(1, min(nk_total, 2048 // C))
        for k0 in range(0, nk_total, nk):
            kk = min(nk, nk_total - k0)
            st, d_st = self.stg.next()
            v = st[:, 0:kk * C].rearrange("p (k c) -> p k c", k=kk)
            kb.dma(v, sv[:, k0:k0 + kk, c0:c0 + C], W=[d_st])
            if scale is not None:
                kb.I('scalar', 'mul', out=dst[:, k0:k0 + kk, :], in_=v, mul=scale, R=[d_st], W=[d_dst])
            else:
                eng = ('vector', 'gpsimd')[self.n % 2]; self.n += 1
                kb.I(eng, 'tensor_copy', out=dst[:, k0:k0 + kk, :], in_=v, R=[d_st], W=[d_dst])


def gla_layer(E, A):
    kb = E.kb
    DK = 128
    Pl = alloc_mod(E); Pc = alloc_mod(E)
    Wq = E.sb([128, 8, 512], BF16, "Wq"); Wk = E.sb([128, 8, 512], BF16, "Wk")
    Wv = E.sb([128, 8, 1024], BF16, "Wv"); Wr = E.sb([128, 8, 1024], BF16, "Wr")
    Wg = E.sb([128, 8, 32], BF16, "Wg"); Wo = E.sb([128, 8, 1024], BF16, "Wo")
    d_W = Dep()
    wgate = E.sb([16, 2, 512], F32, "wgate"); bgate = E.sb([1, 2, 512], F32, "bgate"); d_gate = Dep()
    cm = E.sb([128, 6, 128], F32, "cmats"); d_cm = Dep()
    gng = E.sb([128, 1024], F32, "gng"); d_gng = Dep()
    onec = E.sb([128, 1], F32, "onec"); d_onec = Dep()
    S32 = E.sb([128, 1024], F32, "S32"); d_S32 = Dep()
    Sbf = E.sb([128, 1024], BF16, "Sbf"); d_Sbf = Dep()
    nt = NormT(E, nbuf=1); post = PostRes(E, nbuf=1)
    xs = Ring(E, [128, D], F32, 2, "x")
    hT = Ring(E, [128, 8, 128], BF16, 2, "hT")
    glr = Ring(E, [16, 128], F32, 2, "glr")
    e1 = Ring(E, [128, 512], F32, 2, "e1"); lsp = Ring(E, [128, 512], F32, 2, "lsp")
    EbT = Ring(E, [128, 512], F32, 2, "EbT"); EnbT = Ring(E, [128, 512], F32, 2, "EnbT"); Eend = Ring(E, [128, 512], F32, 2, "Eend")
    qd = Ring(E, [128, 512], BF16, 2, "qd"); ki = Ring(E, [128, 512], BF16, 2, "ki"); ke = Ring(E, [128, 512], BF16, 2, "ke")
    vsb = Ring(E, [128, 1024], BF16, 1, "vsb"); sT = Ring(E, [128, 512], BF16, 2, "sT")
    osb = Ring(E, [128, 1024], F32, 2, "osb")
    ofl = Ring(E, [128, 1024], F32, 1, "ofl")
    sr = Ring(E, [128, 1024], F32, 1, "sr")
    onb = Ring(E, [128, 1024], BF16, 1, "onb"); onT = Ring(E, [128, 8, 128], BF16, 1, "onT")
    ssh = Ring(E, [128, 8], F32, 2, "ssh")
    pA = E.ps([128, 512], F32, "pA"); d_pA = PDep()
    pB = E.ps([128, 512], F32, "pB"); d_pB = PDep()
    p2a = E.ps([128, 1024], F32, "p2a"); d_p2a = PDep()
    p2b = E.ps([128, 1024], F32, "p2b"); d_p2b = PDep()
    E.n += 1
    ofd = E.dram(f"ofd_{E.n}", [34, 128, 1024], F32)
    d_ofd = [Dep() for _ in range(34)]
    with ExitStack() as es:
        (Gl, Sl, GGl, dl), (Gc, Sc, GGc, dc) = compute_mod2(E, Pl, Pc, A['c'], A['cc'], A['mod_w'], A['mod_b'], A['norm_g'], 0, es, pA, d_pA, pB, d_pB)
        kb.barrier()
    with ExitStack() as es:
        cs = Caster(E, es)
        w = A['gla_w_in']
        cs.cast3(Wq, d_W, w, 0, 512, scale=DK ** -0.5)
        cs.cast3(Wk, d_W, w, 512, 512)
        cs.cast3(Wv, d_W, w, 1024, 1024)
        cs.cast3(Wr, d_W, w, 2048, 1024)
        cs.cast3(Wg, d_W, w, 3072, 32)
        cs.cast3(Wo, d_W, A['gla_w_out'], 0, 1024)
        kb.dma(wgate[:], A['gla_w_gate'].rearrange("z r k -> r z k"), W=[d_gate])
        kb.dma(bgate[:], A['gla_b_gate'].rearrange("(o z) k -> o z k", o=1), W=[d_gate])
        kb.dma(cm[:], A['gla_cm'].rearrange("m p c -> p m c"), W=[d_cm])
        kb.I('vector', 'memset', onec[:], 1.0, W=[d_onec])
        kb.I('vector', 'memset', S32[:], 0.0, W=[d_S32])
        row = E.sb([1, 256], F32, "gnrow", es=es); d_row = Dep()
        kb.dma(row[:], A['gla_norm_g'].rearrange("(o c) -> o c", o=1), W=[d_row])
        kb.I('tensor', 'matmul', out=pA[:, 0:256], lhsT=E.ones[0:1, 0:128], rhs=row[0:1, :], start=True, stop=True,
             R=[d_row, E.d_ones], W=[d_pA])
        for h in range(4):
            kb.I('vector', 'tensor_copy', out=gng[:, h * 256:(h + 1) * 256], in_=pA[:, 0:256], R=[d_pA], W=[d_gng])
        kb.barrier()
    d_onec.const = True; d_cm.const = True; d_W.const = True; d_gate.const = True; d_gng.const = True
    for dd in list(dl) + list(dc):
        dd.const = True
    cxv = A['cx'].rearrange("(n p) d -> n p d", p=128); cxo = A['cx_o'].rearrange("(n p) d -> n p d", p=128)
    ltv = A['lat'].rearrange("(n p) d -> n p d", p=128); lto = A['lat_o'].rearrange("(n p) d -> n p d", p=128)
    seq = [(cxv[j], cxo[j], 1) for j in range(2)] + [(ltv[j], lto[j], 0) for j in range(32)]
    for direction in (0, 1):
        order = list(range(34)) if direction == 0 else [1, 0] + list(range(33, 1, -1))
        U = cm[:, 0 + 2 * direction, :]; L = cm[:, 1 + 2 * direction, :]; mask = cm[:, 4 + direction, :]
        ecol = 127 if direction == 0 else 0
        kb.I('vector', 'memset', S32[:], 0.0, W=[d_S32])
        kb.I('vector', 'memset', Sbf[:], 0.0, W=[d_Sbf])
        for ti in order:
            src, dst, isc = seq[ti]
            G, S_, GG, dm = (Gc, Sc, GGc, dc) if isc else (Gl, Sl, GGl, dl)
            x, d_x = xs.next()
            kb.dma(x[:], src, W=[d_x])
            h_, d_h = hT.next()
            nt.run(x[:], d_x, G, S_, dm[:2], lambda k: h_[:, k, :], d_h)
            g_, d_g = glr.next()
            for k in range(8):
                kb.I('tensor', 'matmul', out=pA[0:16, 0:128], lhsT=Wg[:, k, direction * 16:(direction + 1) * 16], rhs=h_[:, k, :],
                     start=(k == 0), stop=(k == 7), R=[d_W, d_h], W=[d_pA])
            kb.I('vector', 'tensor_copy', out=g_[:], in_=pA[0:16, 0:128], R=[d_pA], W=[d_g])
            kb.I('tensor', 'matmul', out=pB[:], lhsT=g_[:], rhs=wgate[:, direction, :], start=True, stop=False,
                 R=[d_g, d_gate], W=[d_pB])
            kb.I('tensor', 'matmul', out=pB[:], lhsT=E.ones[0:1, 0:128], rhs=bgate[0:1, direction, :], start=False, stop=True,
                 R=[d_gate, E.d_ones], W=[d_pB])
            e_, d_e = e1.next(); l_, d_l = lsp.next()
            kb.I('scalar', 'activation', out=e_[:], in_=pB[:], func=AF.Exp, scale=-1.0, R=[d_pB], W=[d_e])
            kb.I('scalar', 'activation', out=l_[:], in_=e_[:], func=AF.Ln, bias=onec[:, 0:1], scale=1.0, R=[d_e, d_onec], W=[d_l])
            for h in range(4):
                kb.I('tensor', 'matmul', out=pA[:, h * 128:(h + 1) * 128], lhsT=l_[:, h * 128:(h + 1) * 128], rhs=U,
                     start=True, stop=True, R=[d_l, d_cm], W=[d_pA])
            eb, d_eb = EbT.next(); enb, d_enb = EnbT.next(); een, d_een = Eend.next()
            kb.I('scalar', 'activation', out=eb[:], in_=pA[:], func=AF.Exp, R=[d_pA], W=[d_eb])
            kb.I('scalar', 'activation', out=enb[:], in_=pA[:], func=AF.Exp, scale=-1.0, R=[d_pA], W=[d_enb])
            kb.I('tensor', 'matmul', out=pB[:], lhsT=L, rhs=l_[:], start=True, stop=True, R=[d_l, d_cm], W=[d_pB])
            kb.I('scalar', 'activation', out=een[:], in_=pB[:], func=AF.Exp, R=[d_pB], W=[d_een])
            for h in range(4):
                for k in range(8):
                    kb.I('tensor', 'matmul', out=pA[:, h * 128:(h + 1) * 128], lhsT=Wq[:, k, h * 128:(h + 1) * 128], rhs=h_[:, k, :],
                         start=(k == 0), stop=(k == 7), R=[d_W, d_h], W=[d_pA])
            q_, d_q = qd.next()
            kb.I('vector', 'tensor_tensor', out=q_[:], in0=pA[:], in1=eb[:], op=ALU.mult, R=[d_pA, d_eb], W=[d_q])
            for h in range(4):
                for k in range(8):
                    kb.I('tensor', 'matmul', out=pB[:, h * 128:(h + 1) * 128], lhsT=Wk[:, k, h * 128:(h + 1) * 128], rhs=h_[:, k, :],
                         start=(k == 0), stop=(k == 7), R=[d_W, d_h], W=[d_pB])
            k_, d_k = ki.next()
            kb.I('vector', 'tensor_tensor', out=k_[:], in0=pB[:], in1=enb[:], op=ALU.mult, R=[d_pB, d_enb], W=[d_k])
            for k in range(8):
                kb.I('tensor', 'matmul', out=pA[:], lhsT=h_[:, k, :], rhs=Wk[:, k, :], start=(k == 0), stop=(k == 7),
                     R=[d_W, d_h], W=[d_pA])
            ke_, d_ke = ke.next()
            kb.I('vector', 'tensor_tensor', out=ke_[:], in0=pA[:], in1=een[:], op=ALU.mult, R=[d_pA, d_een], W=[d_ke])
            for half in range(2):
                for k in range(8):
                    kb.I('tensor', 'matmul', out=p2a[:, half * 512:(half + 1) * 512], lhsT=h_[:, k, :],
                         rhs=Wv[:, k, half * 512:(half + 1) * 512], start=(k == 0), stop=(k == 7), R=[d_W, d_h], W=[d_p2a])
            v_, d_v = vsb.next()
            kb.I('scalar', 'copy', out=v_[:], in_=p2a[:], R=[d_p2a], W=[d_v])
            for h in range(4):
                kb.I('tensor', 'matmul', out=pB[:, h * 128:(h + 1) * 128], lhsT=k_[:, h * 128:(h + 1) * 128],
                     rhs=q_[:, h * 128:(h + 1) * 128], start=True, stop=True, R=[d_k, d_q], W=[d_pB])
            s_, d_s = sT.next()
            for h in range(4):
                kb.I('vector', 'tensor_tensor', out=s_[:, h * 128:(h + 1) * 128], in0=pB[:, h * 128:(h + 1) * 128], in1=mask,
                     op=ALU.mult, R=[d_pB, d_cm], W=[d_s])
            for h in range(4):
                kb.I('tensor', 'matmul', out=p2b[:, h * 256:(h + 1) * 256], lhsT=s_[:, h * 128:(h + 1) * 128],
                     rhs=v_[:, h * 256:(h + 1) * 256], start=True, stop=False, R=[d_s, d_v], W=[d_p2b])
                kb.I('tensor', 'matmul', out=p2b[:, h * 256:(h + 1) * 256], lhsT=q_[:, h * 128:(h + 1) * 128],
                     rhs=Sbf[:, h * 256:(h + 1) * 256], start=False, stop=True, R=[d_q, d_Sbf], W=[d_p2b])
            for h in range(4):
                kb.I('tensor', 'matmul', out=p2a[:, h * 256:(h + 1) * 256], lhsT=ke_[:, h * 128:(h + 1) * 128],
                     rhs=v_[:, h * 256:(h + 1) * 256], start=True, stop=True, R=[d_ke, d_v], W=[d_p2a])
            for h in range(4):
                kb.I('vector', 'scalar_tensor_tensor', out=S32[:, h * 256:(h + 1) * 256], in0=S32[:, h * 256:(h + 1) * 256],
                     scalar=eb[:, h * 128 + ecol:h * 128 + ecol + 1], in1=p2a[:, h * 256:(h + 1) * 256],
                     op0=ALU.mult, op1=ALU.add, R=[d_S32, d_eb, d_p2a], W=[d_S32])
            kb.I('scalar', 'copy', out=Sbf[:], in_=S32[:], R=[d_S32], W=[d_Sbf])
            o_, d_o = osb.next()
            if direction == 0:
                kb.I('scalar', 'copy', out=o_[:], in_=p2b[:], R=[d_p2b], W=[d_o])
                kb.dma(ofd[ti], o_[:], R=[d_o], W=[d_ofd[ti]])
                continue
            f_, d_f = ofl.next()
            kb.dma(f_[:], ofd[ti], R=[d_ofd[ti]], W=[d_f])
            kb.I('vector', 'tensor_tensor', out=o_[:], in0=p2b[:], in1=f_[:], op=ALU.add, R=[d_p2b, d_f], W=[d_o])
            ss_, d_ss = ssh.next()
            for h in range(4):
                kb.I('scalar', 'activation', out=f_[:, h * 256:(h + 1) * 256], in_=o_[:, h * 256:(h + 1) * 256], func=AF.Square,
                     accum_out=ss_[:, h:h + 1], R=[d_o], W=[d_f, d_ss])
            kb.I('scalar', 'activation', out=ss_[:, 4:8], in_=ss_[:, 0:4], func=AF.Sqrt, bias=E.epsc[:, 0:1], scale=1.0 / 256,
                 R=[d_ss, E.d_eps], W=[d_ss])
            kb.I('vector', 'reciprocal', out=ss_[:, 4:8], in_=ss_[:, 4:8], R=[d_ss], W=[d_ss])
            for half in range(2):
                for k in range(8):
                    kb.I('tensor', 'matmul', out=p2a[:, half * 512:(half + 1) * 512], lhsT=h_[:, k, :],
                         rhs=Wr[:, k, half * 512:(half + 1) * 512], start=(k == 0), stop=(k == 7), R=[d_W, d_h], W=[d_p2a])
            r_, d_r = sr.next()
            kb.I('scalar', 'activation', out=r_[:], in_=p2a[:], func=AF.Silu, R=[d_p2a], W=[d_r])
            kb.I('gpsimd', 'tensor_tensor', out=r_[:], in0=r_[:], in1=gng[:], op=ALU.mult, R=[d_r, d_gng], W=[d_r])
            ob_, d_ob = onb.next()
            for h in range(4):
                kb.I('vector', 'scalar_tensor_tensor', out=ob_[:, h * 256:(h + 1) * 256], in0=o_[:, h * 256:(h + 1) * 256],
                     scalar=ss_[:, 4 + h:5 + h], in1=r_[:, h * 256:(h + 1) * 256], op0=ALU.mult, op1=ALU.mult,
                     R=[d_o, d_ss, d_r], W=[d_ob])
            oT_, d_oT = onT.next()
            for k in range(8):
                kb.I('tensor', 'transpose', out=nt.pT[0][:, k, :], in_=ob_[:, k * 128:(k + 1) * 128], identity=E.idb[:],
                     R=[d_ob, E.d_idb], W=[nt.d_pT[0]])
            kb.I('scalar', 'copy', out=oT_[:], in_=nt.pT[0][:], R=[nt.d_pT[0]], W=[d_oT])
            for half in range(2):
                for k in range(8):
                    kb.I('tensor', 'matmul', out=p2b[:, half * 512:(half + 1) * 512], lhsT=oT_[:, k, :],
                         rhs=Wo[:, k, half * 512:(half + 1) * 512], start=(k == 0), stop=(k == 7), R=[d_W, d_oT], W=[d_p2b])
            post.run(p2b[:], d_p2b, x[:], d_x, GG, dm[2])
            kb.dma(dst, x[:], R=[d_x])


TWO_PI = 6.283185307179586


def bcast_rows(E, dst, d_dst, src_row_ap, ncols, pm, d_pm, stg, d_stg):
    kb = E.kb
    for c0 in range(0, ncols, 512):
        cc = min(512, ncols - c0)
        kb.dma(stg[0:1, 0:cc], src_row_ap[c0:c0 + cc].rearrange("(o c) -> o c", o=1), W=[d_stg])
        kb.I('tensor', 'matmul', out=pm[:, 0:cc], lhsT=E.ones[0:1, 0:128], rhs=stg[0:1, 0:cc], start=True, stop=True,
             R=[d_stg, E.d_ones], W=[d_pm])
        kb.I('vector', 'tensor_copy', out=dst[:, c0:c0 + cc], in_=pm[:, 0:cc], R=[d_pm], W=[d_dst])


def hyena_seq(E, A, L, tag, src, dst, G, S_, GG, dm, nt, post, PS):
    kb = E.kb
    T = L // 128
    NFQ = T + 1
    TB = min(512, L); NTB = L // TB
    pA, d_pA, pB, d_pB, p2a, d_p2a, p2b, d_p2b = PS
    E.n += 1
    u = E.n
    hs_d = E.dram(f"hs_{u}", [T, 128, D], BF16); hd_d = E.dram(f"hd_{u}", [T, 128, D], BF16)
    w_d = E.dram(f"w_{u}", [T, 128, D], BF16); x0_d = E.dram(f"x0_{u}", [8, 128, L], BF16)
    pq_d = E.dram(f"pq_{u}", [NFQ, 128, 2 * D], BF16); yh_d = E.dram(f"yh_{u}", [NFQ, 128, 2 * D], BF16)
    yx_d = E.dram(f"yx_{u}", [8, 128, L], BF16)
    d_hs = Dep(); d_hd = Dep(); d_w = Dep(); d_x0 = Dep(); d_pq = Dep(); d_yh = Dep(); d_yx = Dep()
    srcv = src.rearrange("(n p) d -> n p d", p=128); dstv = dst.rearrange("(n p) d -> n p d", p=128)
    T1c, T1s, T2c, T2s = A[f'T1c_{tag}'], A[f'T1s_{tag}'], A[f'T2c_{tag}'], A[f'T2s_{tag}']

    with ExitStack() as es:
        fe = E.sb([33, L], F32, "fe", es=es); d_fe = Dep()
        w1 = E.sb([33, 64], F32, "w1", es=es); w2 = E.sb([64, 64], F32, "w2", es=es); w3 = E.sb([64, 2 * D], F32, "w3", es=es)
        cols = E.sb([64, 8], F32, "fcols", es=es); d_fw = Dep()
        z1 = E.sb([64, L], F32, "z1", es=es); z2 = E.sb([64, L], F32, "z2", es=es); d_z1 = Dep(); d_z2 = Dep()
        dbc = E.sb([128, D], F32, "dbc", es=es); d_dbc = Dep()
        tn = E.sb([128, T], F32, "tn", es=es); d_tn = Dep()
        dec = Ring(E, [128, D], F32, 2, "dec", es=es)
        hf = Ring(E, [128, D], F32, 2, "hf", es=es); hb = Ring(E, [128, D], F32, 2, "hb", es=es)
        hso = Ring(E, [128, D], BF16, 2, "hso", es=es); hdo = Ring(E, [128, D], BF16, 2, "hdo", es=es)
        kb.dma(fe[:], A[f'feT_{tag}'], W=[d_fe])
        kb.dma(w1[:], A['hy_f_w1'], W=[d_fw]); kb.dma(w2[:], A['hy_f_w2'], W=[d_fw]); kb.dma(w3[:], A['hy_f_w3'], W=[d_fw])
        kb.dma(cols[:, 0:1], A['hy_f_freq'].rearrange("(p o) -> p o", o=1), W=[d_fw])
        kb.dma(cols[:, 1:2], A['hy_f_b1'].rearrange("(p o) -> p o", o=1), W=[d_fw])
        kb.dma(cols[:, 2:3], A['hy_f_b2'].rearrange("(p o) -> p o", o=1), W=[d_fw])
        kb.dma(dbc[:], A['hy_delta_bc'], W=[d_dbc])
        kb.dma(tn[:], A[f'tn_{tag}'], W=[d_tn])
        three = E.sb([64, 512], F32, "three", es=es); d_three = Dep(const=True)
        sq_ = E.sb([64, 512], F32, "sq_", es=es); d_sq_ = Dep()
        s3 = E.sb([64, 512], F32, "s3", es=es); d_s3 = Dep()
        kb.I('vector', 'memset', three[:], 3.0, W=[d_three])
        kb.I('scalar', 'mul', out=cols[:, 3:4], in_=cols[:, 0:1], mul=1.0 / 3.0, R=[d_fw], W=[d_fw])
        kb.I('vector', 'tensor_tensor', out=cols[:, 4:5], in0=cols[:, 1:2], in1=cols[:, 3:4], op=ALU.mult, R=[d_fw], W=[d_fw])
        kb.I('vector', 'tensor_tensor', out=cols[:, 5:6], in0=cols[:, 2:3], in1=cols[:, 3:4], op=ALU.mult, R=[d_fw], W=[d_fw])
        for (zin, d_zin, wm, zo, d_zo, bc, kk) in ((fe, d_fe, w1, z1, d_z1, 4, 33), (z1, d_z1, w2, z2, d_z2, 5, 64)):
            for c0 in range(0, L, 512):
                cw = min(512, L - c0)
                kb.I('tensor', 'matmul', out=pA[0:64, 0:cw], lhsT=wm[0:kk, :], rhs=zin[0:kk, c0:c0 + cw], start=True, stop=True,
                     R=[d_zin, d_fw], W=[d_pA])
                kb.I('scalar', 'activation', out=s3[:, 0:cw], in_=pA[0:64, 0:cw], func=AF.Sin, scale=cols[:, 3:4],
                     bias=cols[:, bc:bc + 1], R=[d_pA, d_fw], W=[d_s3])
                kb.I('vector', 'tensor_tensor', out=sq_[:, 0:cw], in0=s3[:, 0:cw], in1=s3[:, 0:cw], op=ALU.mult, R=[d_s3], W=[d_sq_])
                kb.I('vector', 'scalar_tensor_tensor', out=sq_[:, 0:cw], in0=sq_[:, 0:cw], scalar=-4.0, in1=three[:, 0:cw],
                     op0=ALU.mult, op1=ALU.add, R=[d_sq_, d_three], W=[d_sq_])
                kb.I('vector', 'tensor_tensor', out=zo[:, c0:c0 + cw], in0=sq_[:, 0:cw], in1=s3[:, 0:cw], op=ALU.mult,
                     R=[d_sq_, d_s3], W=[d_zo])
        for t in range(T):
            for (pp, d_pp, c0) in ((p2a, d_p2a, 0), (p2b, d_p2b, D)):
                for half in range(2):
                    kb.I('tensor', 'matmul', out=pp[:, half * 512:(half + 1) * 512], lhsT=z2[:, t * 128:(t + 1) * 128],
                         rhs=w3[:, c0 + half * 512:c0 + (half + 1) * 512], start=True, stop=True, R=[d_z2, d_fw], W=[d_pp])
            de, d_de = dec.next()
            kb.I('scalar', 'activation', out=de[:], in_=dbc[:], func=AF.Exp, scale=tn[:, t:t + 1], R=[d_dbc, d_tn], W=[d_de])
            f_, d_f = hf.next(); b_, d_b = hb.next()
            kb.I('vector', 'tensor_tensor', out=f_[:], in0=p2a[:], in1=de[:], op=ALU.mult, R=[d_p2a, d_de], W=[d_f])
            kb.I('vector', 'tensor_tensor', out=b_[:], in0=p2b[:], in1=de[:], op=ALU.mult, R=[d_p2b, d_de], W=[d_b])
            so, d_so = hso.next(); do_, d_do = hdo.next()
            kb.I('gpsimd', 'tensor_tensor', out=so[:], in0=f_[:], in1=b_[:], op=ALU.add, R=[d_f, d_b], W=[d_so])
            kb.I('gpsimd', 'tensor_tensor', out=do_[:], in0=f_[:], in1=b_[:], op=ALU.subtract, R=[d_f, d_b], W=[d_do])
            kb.dma(hs_d[t], so[:], R=[d_so], W=[d_hs])
            kb.dma(hd_d[t], do_[:], R=[d_do], W=[d_hd])
        kb.barrier()

    with ExitStack() as esA:
        hTa = E.sb([128, 8, L + 2], BF16, "hTa", es=esA); d_hTa = Dep()
        xs = Ring(E, [128, D], F32, 2, "hx", es=esA)
        kb.I('vector', 'memset', hTa[:, :, 0:1], 0.0, W=[d_hTa])
        kb.I('vector', 'memset', hTa[:, :, L + 1:L + 2], 0.0, W=[d_hTa])
        for t in range(T):
            x, d_x = xs.next()
            kb.dma(x[:], srcv[t], W=[d_x])
            nt.run(x[:], d_x, G, S_, dm[:2], lambda k, t=t: hTa[:, k, 1 + t * 128:1 + (t + 1) * 128], d_hTa)
        wv = A['hy_w_in'].rearrange("(k p) c -> p k c", p=128)
        for sub in (0, 1, 2, 3):
            with ExitStack() as es:
                parts = (1, 2) if sub < 2 else (0,)
                half = sub % 2
                Wt = {pp: [E.sb([128, 8, 512], BF16, "Wt", es=es) for _ in range(3)] for pp in parts}
                d_Wt = Dep()
                cwb = E.sb([128, 3, 512], F32, "cwb", es=es); d_cwb = Dep()
                stg = Ring(E, [128, 512], F32, 2, "wstg", es=es)
                rstg = E.sb([1, 512], F32, "rstg", es=es); d_rstg = Dep()
                for pp in parts:
                    c0 = pp * D + half * 512
                    for j in range(3):
                        bcast_rows(E, cwb[:, j, :], d_cwb, A['hy_conv_w'][j, c0:c0 + 512], 512, pA, d_pA, rstg, d_rstg)
                    for k in range(8):
                        st, d_st = stg.next()
                        kb.dma(st[:], wv[:, k, c0:c0 + 512], W=[d_st])
                        for j in range(3):
                            kb.I(('vector', 'gpsimd', 'vector')[j], 'tensor_tensor', out=Wt[pp][j][:, k, :], in0=st[:], in1=cwb[:, j, :],
                                 op=ALU.mult, R=[d_st, d_cwb], W=[d_Wt])
                if sub < 2:
                    brow = E.sb([1, 2, 512], F32, "brow", es=es); d_brow = Dep()
                    for pp in (1, 2):
                        kb.dma(brow[0:1, pp - 1, :], A['hy_conv_b'][pp * D + half * 512:pp * D + half * 512 + 512].rearrange("(o c) -> o c", o=1),
                               W=[d_brow])
                    x1s = Ring(E, [128, 512], F32, 2, "x1s", es=es); wo_ = Ring(E, [128, 512], BF16, 2, "wo_", es=es)
                    for t in range(T):
                        for (pp, pt, d_pt) in ((1, pA, d_pA), (2, pB, d_pB)):
                            for j in range(3):
                                for k in range(8):
                                    kb.I('tensor', 'matmul', out=pt[:], lhsT=hTa[:, k, t * 128 + j:t * 128 + j + 128], rhs=Wt[pp][j][:, k, :],
                                         start=(j == 0 and k == 0), stop=False, R=[d_hTa, d_Wt], W=[d_pt])
                            kb.I('tensor', 'matmul', out=pt[:], lhsT=E.ones[0:1, 0:128], rhs=brow[0:1, pp - 1, :], start=False, stop=True,
                                 R=[d_brow, E.d_ones], W=[d_pt])
                        x1, d_x1 = x1s.next()
                        kb.I('scalar', 'copy', out=x1[:], in_=pA[:], R=[d_pA], W=[d_x1])
                        w_, d_w_ = wo_.next()
                        kb.I('vector', 'tensor_tensor', out=w_[:], in0=pB[:], in1=x1[:], op=ALU.mult, R=[d_pB, d_x1], W=[d_w_])
                        kb.dma(w_d[t][:, half * 512:(half + 1) * 512], w_[:], R=[d_w_], W=[d_w])
                else:
                    bcol = E.sb([128, 4], F32, "bcol0", es=es); d_bcol = Dep()
                    kb.dma(bcol[:], A['hy_conv_b'][half * 512:(half + 1) * 512].rearrange("(k p) -> p k", p=128), W=[d_bcol],
                           allow_slow_non_contiguous=True)
                    xo = Ring(E, [128, TB], BF16, 2, "xo", es=es)
                    pr = [(pA, d_pA), (pB, d_pB)]
                    n = 0
                    for ci in range(4):
                        cc = half * 4 + ci
                        for tb in range(NTB):
                            pt, d_pt = pr[n % 2]; n += 1
                            for j in range(3):
                                for k in range(8):
                                    kb.I('tensor', 'matmul', out=pt[:, 0:TB], lhsT=Wt[0][j][:, k, ci * 128:(ci + 1) * 128],
                                         rhs=hTa[:, k, tb * TB + j:tb * TB + j + TB], start=(j == 0 and k == 0), stop=(j == 2 and k == 7),
                                         R=[d_hTa, d_Wt], W=[d_pt])
                            o_, d_o = xo.next()
                            kb.I('scalar', 'activation', out=o_[:], in_=pt[:, 0:TB], func=AF.Identity, bias=bcol[:, ci:ci + 1], scale=1.0,
                                 R=[d_pt, d_bcol], W=[d_o])
                            kb.dma(x0_d[cc][:, tb * TB:(tb + 1) * TB], o_[:], R=[d_o], W=[d_x0])
                kb.barrier()

    for sub in (0, 1):
        with ExitStack() as es:
            nres = 2 if sub == 0 else 1
            res = [E.sb([128, T, D], BF16, "res", es=es) for _ in range(nres)]; d_res = Dep()
            t1 = Ring(E, [128, 2, T, 128], BF16, 2, "t1", es=es)
            ob = Ring(E, [128, 2 * D], BF16, 2, "ob", es=es)
            srcs = (hs_d, hd_d) if sub == 0 else (w_d,)
            dsrc = (d_hs, d_hd) if sub == 0 else (d_w,)
            for i in range(nres):
                for t in range(T):
                    kb.dma(res[i][:, t, :], srcs[i][t], R=[dsrc[i]], W=[d_res])
            if sub == 0:
                fb = E.sb([128, D], F32, "fb", es=es); d_fb = Dep()
                rstg = E.sb([1, 512], F32, "rstg2", es=es); d_rstg = Dep()
                bcast_rows(E, fb, d_fb, A['hy_f_bias'], D, pA, d_pA, rstg, d_rstg)
            else:
                pq = Ring(E, [128, 2 * D], BF16, 2, "pq", es=es)
                tmp = Ring(E, [128, 512], F32, 2, "ytmp", es=es); tmp2 = Ring(E, [128, 512], F32, 2, "ytmp2", es=es)
            for fq in range(NFQ):
                tt, d_tt = t1.next()
                kb.dma(tt[:, 0], T1c[fq], W=[d_tt])
                kb.dma(tt[:, 1], T1s[fq], W=[d_tt])
                o_, d_o = ob.next()
                if sub == 1:
                    q_, d_q = pq.next()
                    kb.dma(q_[:], pq_d[fq], R=[d_pq], W=[d_q])
                for half in range(2):
                    hsl = slice(half * 512, (half + 1) * 512)
                    pA, d_pA, pB, d_pB = (PS[0], PS[1], PS[2], PS[3]) if half == 0 else (PS[4][:, 0:512], PS[5], PS[6][:, 0:512], PS[7])
                    for tr, (pt, d_pt) in enumerate(((pA, d_pA), (pB, d_pB))):
                        r = res[tr] if sub == 0 else res[0]
                        for tc in range(T):
                            kb.I('tensor', 'matmul', out=pt[:], lhsT=tt[:, tr, tc, :], rhs=r[:, tc, hsl], start=(tc == 0), stop=(tc == T - 1),
                                 R=[d_tt, d_res], W=[d_pt])
                    if sub == 0:
                        kb.I('vector', 'tensor_tensor', out=o_[:, hsl], in0=pA[:], in1=fb[:, hsl], op=ALU.add, R=[d_pA, d_fb], W=[d_o])
                        kb.I('scalar', 'copy', out=o_[:, D + half * 512:D + (half + 1) * 512], in_=pB[:], R=[d_pB], W=[d_o])
                    else:
                        P_ = q_[:, hsl]; Q_ = q_[:, D + half * 512:D + (half + 1) * 512]
                        a_, d_a = tmp.next(); b_, d_b = tmp2.next()
                        kb.I('vector', 'tensor_tensor', out=a_[:], in0=pA[:], in1=P_, op=ALU.mult, R=[d_pA, d_q], W=[d_a])
                        kb.I('vector', 'tensor_tensor', out=b_[:], in0=pB[:], in1=Q_, op=ALU.mult, R=[d_pB, d_q], W=[d_b])
                        kb.I('gpsimd', 'tensor_tensor', out=o_[:, hsl], in0=a_[:], in1=b_[:], op=ALU.subtract, R=[d_a, d_b], W=[d_o])
                        a_, d_a = tmp.next(); b_, d_b = tmp2.next()
                        kb.I('vector', 'tensor_tensor', out=a_[:], in0=pA[:], in1=Q_, op=ALU.mult, R=[d_pA, d_q], W=[d_a])
                        kb.I('vector', 'tensor_tensor', out=b_[:], in0=pB[:], in1=P_, op=ALU.mult, R=[d_pB, d_q], W=[d_b])
                        kb.I('gpsimd', 'tensor_tensor', out=o_[:, D + half * 512:D + (half + 1) * 512], in0=a_[:], in1=b_[:], op=ALU.add,
                             R=[d_a, d_b], W=[d_o])
                if sub == 0:
                    kb.dma(pq_d[fq], o_[:], R=[d_o], W=[d_pq])
                else:
                    kb.dma(yh_d[fq], o_[:], R=[d_o], W=[d_yh])
            kb.barrier()

    pA, d_pA, pB, d_pB, p2a, d_p2a, p2b, d_p2b = PS
    with ExitStack() as es:
        yh = E.sb([128, NFQ, 2 * D], BF16, "yh", es=es); d_yhs = Dep()
        for fq in range(NFQ):
            kb.dma(yh[:, fq, :], yh_d[fq], R=[d_yh], W=[d_yhs])
        t2 = Ring(E, [128, 2, TB], BF16, 4, "t2", es=es)
        x0t = Ring(E, [128, TB], BF16, 2, "x0t", es=es); yo = Ring(E, [128, TB], BF16, 2, "yo", es=es)
        acc = [(pA, d_pA), (pB, d_pB), (p2a, d_p2a), (p2b, d_p2b)]
        for ccg in range(2):
            for tb in range(NTB):
                for fq in range(NFQ):
                    tt, d_tt = t2.next()
                    kb.dma(tt[:, 0, :], T2c[fq][:, tb * TB:(tb + 1) * TB], W=[d_tt])
                    kb.dma(tt[:, 1, :], T2s[fq][:, tb * TB:(tb + 1) * TB], W=[d_tt])
                    for ci in range(4):
                        cc = ccg * 4 + ci
                        pt, d_pt = acc[ci]
                        kb.I('tensor', 'matmul', out=pt[:, 0:TB], lhsT=yh[:, fq, cc * 128:(cc + 1) * 128], rhs=tt[:, 0, :],
                             start=(fq == 0), stop=False, R=[d_yhs, d_tt], W=[d_pt])
                        kb.I('tensor', 'matmul', out=pt[:, 0:TB], lhsT=yh[:, fq, D + cc * 128:D + (cc + 1) * 128], rhs=tt[:, 1, :],
                             start=False, stop=(fq == NFQ - 1), R=[d_yhs, d_tt], W=[d_pt])
                for ci in range(4):
                    cc = ccg * 4 + ci
                    pt, d_pt = acc[ci]
                    x_, d_x_ = x0t.next()
                    kb.dma(x_[:], x0_d[cc][:, tb * TB:(tb + 1) * TB], R=[d_x0], W=[d_x_])
                    o_, d_o = yo.next()
                    kb.I('vector', 'tensor_tensor', out=o_[:], in0=pt[:, 0:TB], in1=x_[:], op=ALU.mult, R=[d_pt, d_x_], W=[d_o])
                    kb.dma(yx_d[cc][:, tb * TB:(tb + 1) * TB], o_[:], R=[d_o], W=[d_yx])
        kb.barrier()

    with ExitStack() as es:
        Wo = E.sb([128, 8, D], BF16, "hWo", es=es); d_Wo = Dep()
        cs = Caster(E, es)
        cs.cast3(Wo, d_Wo, A['hy_w_out'], 0, D)
        yT = Ring(E, [128, 8, 128], BF16, 2, "yT", es=es)
        xs = Ring(E, [128, D], F32, 2, "dx", es=es)
        for t in range(T):
            y_, d_y = yT.next()
            kb.dma(y_[:], yx_d.rearrange("c p l -> p c l")[:, :, t * 128:(t + 1) * 128], R=[d_yx], W=[d_y])
            x, d_x = xs.next()
            kb.dma(x[:], srcv[t], W=[d_x])
            for half in range(2):
                for k in range(8):
                    kb.I('tensor', 'matmul', out=p2a[:, half * 512:(half + 1) * 512], lhsT=y_[:, k, :],
                         rhs=Wo[:, k, half * 512:(half + 1) * 512], start=(k == 0), stop=(k == 7), R=[d_y, d_Wo], W=[d_p2a])
            post.run(p2a[:], d_p2a, x[:], d_x, GG, dm[2])
            kb.dma(dstv[t], x[:], R=[d_x])
        kb.barrier()


def hyena_layer(E, A):
    kb = E.kb
    Pl = alloc_mod(E); Pc = alloc_mod(E)
    nt = NormT(E, nbuf=2); post = PostRes(E, nbuf=2)
    pA = E.ps([128, 512], F32, "pA"); d_pA = PDep()
    pB = E.ps([128, 512], F32, "pB"); d_pB = PDep()
    p2a = E.ps([128, 1024], F32, "p2a"); d_p2a = PDep()
    p2b = E.ps([128, 1024], F32, "p2b"); d_p2b = PDep()
    PS = (pA, d_pA, pB, d_pB, p2a, d_p2a, p2b, d_p2b)
    with ExitStack() as es:
        (Gl, Sl, GGl, dl), (Gc, Sc, GGc, dc) = compute_mod2(E, Pl, Pc, A['c'], A['cc'], A['mod_w'], A['mod_b'], A['norm_g'], 0, es, pA, d_pA, pB, d_pB)
        kb.barrier()
    hyena_seq(E, A, CTXL, "c", A['cx'], A['cx_o'], Gc, Sc, GGc, dc, nt, post, PS)
    hyena_seq(E, A, LAT, "l", A['lat'], A['lat_o'], Gl, Sl, GGl, dl, nt, post, PS)


def col_major_rows(ap2d, n):
    v = ap2d.rearrange("(r w) d -> w r d", w=64)
    return v[2 * n], v[2 * n + 1]


def load_tile(E, x, d_x, A, seg, n):
    kb = E.kb
    if seg == 0:
        kb.dma(x[:], A['cx'].rearrange("(n p) d -> n p d", p=128)[n], W=[d_x])
    else:
        a, b = col_major_rows(A['lat'], n)
        kb.dma(x[0:64, :], a, W=[d_x])
        kb.dma(x[64:128, :], b, W=[d_x])


def store_tile(E, x, d_x, A, seg, n):
    kb = E.kb
    if seg == 0:
        kb.dma(A['cx_o'].rearrange("(n p) d -> n p d", p=128)[n], x[:], R=[d_x])
    else:
        a, b = col_major_rows(A['lat_o'], n)
        kb.dma(a, x[0:64, :], R=[d_x])
        kb.dma(b, x[64:128, :], R=[d_x])


def mlstm_layer(E, A, passes=(1, 2)):
    kb = E.kb
    LT = CTXL + LAT
    NTT = LT // 128
    Pl = alloc_mod(E); Pc = alloc_mod(E)
    nt = NormT(E, nbuf=1); post = PostRes(E, nbuf=1)
    pA = E.ps([128, 512], F32, "pA"); d_pA = PDep()
    pB = E.ps([128, 512], F32, "pB"); d_pB = PDep()
    pC = E.ps([128, 512], F32, "pC"); d_pC = PDep()
    p4 = E.ps([128, 2048], F32, "p4"); d_p4 = PDep()
    gates = E.sb([16, LT], F32, "gates"); d_gates = Dep()
    E.n += 1
    u = E.n
    qT_d = E.dram(f"mqT_{u}", [16, 128, LT], BF16); kT_d = E.dram(f"mkT_{u}", [16, 128, LT], BF16)
    xcT_d = E.dram(f"mxc_{u}", [16, 128, LT], BF16); szT_d = E.dram(f"msz_{u}", [16, 128, LT], BF16)
    ktok_d = E.dram(f"mkt_{u}", [NTT, 128, 2048], BF16); vtok_d = E.dram(f"mvt_{u}", [NTT, 128, 2048], BF16)
    hf_d = E.dram(f"mhf_{u}", [NTT, 128, 2048], F32)
    d_qT = Dep(); d_kT = Dep(); d_xcT = Dep(); d_szT = Dep(); d_ktok = Dep(); d_vtok = Dep()
    d_hf = [Dep() for _ in range(NTT)]
    with ExitStack() as es:
        (Gl, Sl, GGl, dl), (Gc, Sc, GGc, dc) = compute_mod2(E, Pl, Pc, A['c'], A['cc'], A['mod_w'], A['mod_b'], A['norm_g'], 0, es, pA, d_pA, pB, d_pB)
        kb.barrier()
    for dd in list(dl) + list(dc):
        dd.const = True

    with ExitStack() as es:
        hTa = E.sb([128, 8, LAT + 2], BF16, "mhTa", es=es); d_hTa = Dep()
        bd = E.sb([128, 3, 16, 128], BF16, "bd", es=es); d_bd = Dep()
        wg = E.sb([128, 48, 16], BF16, "wg", es=es); d_wg = Dep()
        cwc = E.sb([128, 4, 16], F32, "cwc", es=es); d_cwc = Dep()
        xs = Ring(E, [128, D], F32, 2, "mx", es=es)
        stg = Ring(E, [128, 2048], F32, 2, "mstg", es=es)
        wxb = Ring(E, [128, 8, 128], BF16, 2, "wxb", es=es); wzb = Ring(E, [128, 8, 128], BF16, 2, "wzb", es=es)
        xm32 = E.sb([128, LAT + 2], F32, "xm32", es=es); d_xm32 = Dep()
        tcv = E.sb([128, LAT], F32, "tcv", es=es); d_tcv = Dep()
        xmb = E.sb([128, LAT], BF16, "xmb", es=es); d_xmb = Dep()
        xcb = E.sb([128, LAT], BF16, "xcb", es=es); d_xcb = Dep()
        ob = Ring(E, [128, 512], BF16, 4, "mob", es=es)
        for j in range(3):
            for c0 in range(0, 16, 8):
                st, d_st = stg.next()
                kb.dma(st[:, 0:1024].rearrange("p (f c) -> p f c", f=8), A['ml_bd'][j, c0:c0 + 8].rearrange("f p c -> p f c"), W=[d_st])
                kb.I('vector', 'tensor_copy', out=bd[:, j, c0:c0 + 8, :], in_=st[:, 0:1024].rearrange("p (f c) -> p f c", f=8),
                     R=[d_st], W=[d_bd])
        st, d_st = stg.next()
        for z in range(2):
            for c0 in range(0, 48, 8):
                kb.dma(st[:, 0:768].rearrange("p (c z g) -> p c z g", c=48, z=2)[:, c0:c0 + 8, z, :],
                       A['ml_w_gates'][z].rearrange("(c p) g -> p c g", p=128)[:, c0:c0 + 8, :], W=[d_st], allow_slow_non_contiguous=True)
        kb.I('vector', 'tensor_copy', out=wg[:], in_=st[:, 0:768].rearrange("p (c zg) -> p c zg", c=48), R=[d_st], W=[d_wg])
        st, d_st = stg.next()
        kb.dma(st[0:48, 0:128], A['ml_conv_w'].rearrange("j (f p) -> (j f) p", p=128), W=[d_st])
        kb.dma(st[48:64, 0:128], A['ml_conv_b'].rearrange("(f p) -> f p", p=128), W=[d_st])
        kb.I('tensor', 'matmul', out=pA[:, 0:64], lhsT=st[0:64, 0:128], rhs=E.idf[0:64, 0:64], start=True, stop=True, R=[d_st, E.d_idf], W=[d_pA])
        kb.I('vector', 'tensor_copy', out=cwc[:].rearrange("p j f -> p (j f)"), in_=pA[:, 0:64], R=[d_pA], W=[d_cwc])
        wv = A['ml_w_in'].rearrange("(k p) c -> p k c", p=128)
        STOP = 99
        if STOP == 0:
            kb.barrier(); return
        for seg, L, off, tb0 in ((0, CTXL, 0, 0), (1, LAT, CTXL, 2)):
            T = L // 128; B = min(512, L); NB = L // B
            G, S_, dm = (Gc, Sc, dc) if seg == 0 else (Gl, Sl, dl)
            kb.I('vector', 'memset', hTa[:, :, 0:1], 0.0, W=[d_hTa])
            kb.I('vector', 'memset', hTa[:, :, L + 1:L + 2], 0.0, W=[d_hTa])
            for t in range(T):
                x, d_x = xs.next()
                load_tile(E, x, d_x, A, seg, t)
                nt.run(x[:], d_x, G, S_, dm[:2], lambda k, t=t: hTa[:, k, 1 + t * 128:1 + (t + 1) * 128], d_hTa)
            if STOP == 1:
                kb.barrier(); return
            kb.I('vector', 'memset', xm32[:, 0:1], 0.0, W=[d_xm32])
            kb.I('vector', 'memset', xm32[:, L + 1:L + 2], 0.0, W=[d_xm32])
            for fc in range(16):
                st, d_st = stg.next()
                wx_, d_wx = wxb.next(); wz_, d_wz = wzb.next()
                kb.dma(st[:, 0:1024].rearrange("p (k c) -> p k c", k=8), wv[:, :, fc * 128:(fc + 1) * 128], W=[d_st])
                kb.dma(st[:, 1024:2048].rearrange("p (k c) -> p k c", k=8), wv[:, :, 2048 + fc * 128:2048 + (fc + 1) * 128], W=[d_st])
                kb.I('vector', 'tensor_copy', out=wx_[:], in_=st[:, 0:1024].rearrange("p (k c) -> p k c", k=8), R=[d_st], W=[d_wx])
                kb.I('gpsimd', 'tensor_copy', out=wz_[:], in_=st[:, 1024:2048].rearrange("p (k c) -> p k c", k=8), R=[d_st], W=[d_wz])
                for blk in range(NB):
                    bs = slice(blk * B, (blk + 1) * B)
                    for k in range(8):
                        kb.I('tensor', 'matmul', out=pA[:, 0:B], lhsT=wx_[:, k, :], rhs=hTa[:, k, 1 + blk * B:1 + (blk + 1) * B],
                             start=(k == 0), stop=(k == 7), R=[d_wx, d_hTa], W=[d_pA])
                    kb.I('scalar', 'copy', out=xm32[:, 1 + blk * B:1 + (blk + 1) * B], in_=pA[:, 0:B], R=[d_pA], W=[d_xm32])
                    kb.I('vector', 'tensor_copy', out=xmb[:, bs], in_=pA[:, 0:B], R=[d_pA], W=[d_xmb])
                    for k in range(8):
                        kb.I('tensor', 'matmul', out=pB[:, 0:B], lhsT=wz_[:, k, :], rhs=hTa[:, k, 1 + blk * B:1 + (blk + 1) * B],
                             start=(k == 0), stop=(k == 7), R=[d_wz, d_hTa], W=[d_pB])
                    o_, d_o = ob.next()
                    kb.I('scalar', 'activation', out=o_[:, 0:B], in_=pB[:, 0:B], func=AF.Silu, R=[d_pB], W=[d_o])
                    kb.dma(szT_d[fc][:, off + blk * B:off + (blk + 1) * B], o_[:, 0:B], R=[d_o], W=[d_szT])
                if STOP == 2:
                    kb.barrier(); return
                kb.I('vector', 'tensor_scalar_mul', out=tcv[:, 0:L], in0=xm32[:, 1:L + 1], scalar1=cwc[:, 1, fc:fc + 1],
                     R=[d_xm32, d_cwc], W=[d_tcv])
                kb.I('vector', 'scalar_tensor_tensor', out=tcv[:, 0:L], in0=xm32[:, 0:L], scalar=cwc[:, 0, fc:fc + 1], in1=tcv[:, 0:L],
                     op0=ALU.mult, op1=ALU.add, R=[d_xm32, d_cwc, d_tcv], W=[d_tcv])
                kb.I('vector', 'scalar_tensor_tensor', out=tcv[:, 0:L], in0=xm32[:, 2:L + 2], scalar=cwc[:, 2, fc:fc + 1], in1=tcv[:, 0:L],
                     op0=ALU.mult, op1=ALU.add, R=[d_xm32, d_cwc, d_tcv], W=[d_tcv])
                kb.I('scalar', 'activation', out=xcb[:, 0:L], in_=tcv[:, 0:L], func=AF.Silu, bias=cwc[:, 3, fc:fc + 1], scale=1.0,
                     R=[d_tcv, d_cwc], W=[d_xcb])
                kb.dma(xcT_d[fc][:, off:off + L], xcb[:, 0:L], R=[d_xcb], W=[d_xcT])
                if STOP == 3:
                    kb.barrier(); return
                for blk in range(NB):
                    bs = slice(blk * B, (blk + 1) * B)
                    outs = []
                    for j, (srcb, d_src, dd, d_dd) in enumerate(((xcb, d_xcb, qT_d, d_qT), (xcb, d_xcb, kT_d, d_kT), (xmb, d_xmb, None, None))):
                        pt, d_pt = (pA, d_pA) if j % 2 == 0 else (pB, d_pB)
                        kb.I('tensor', 'matmul', out=pt[:, 0:B], lhsT=bd[:, j, fc, :], rhs=srcb[:, bs], start=True, stop=True,
                             R=[d_bd, d_src], W=[d_pt])
                        o_, d_o = ob.next()
                        kb.I(('vector', 'scalar', 'vector')[j], 'tensor_copy' if j != 1 else 'copy', out=o_[:, 0:B], in_=pt[:, 0:B],
                             R=[d_pt], W=[d_o])
                        if dd is not None:
                            kb.dma(dd[fc][:, off + blk * B:off + (blk + 1) * B], o_[:, 0:B], R=[d_o], W=[d_dd])
                        outs.append((o_, d_o))
                    for j, (o_, d_o) in enumerate(outs):
                        kb.I('tensor', 'matmul', out=pC[0:16, 0:B], lhsT=wg[:, 16 * j + fc, :], rhs=o_[:, 0:B], start=(j == 0), stop=(j == 2),
                             R=[d_wg, d_o], W=[d_pC])
                    gsl = gates[:, off + blk * B:off + (blk + 1) * B]
                    if fc == 0:
                        kb.I('vector', 'tensor_copy', out=gsl, in_=pC[0:16, 0:B], R=[d_pC], W=[d_gates])
                    else:
                        kb.I('vector', 'tensor_tensor', out=gsl, in0=pC[0:16, 0:B], in1=gsl, op=ALU.add, R=[d_pC, d_gates], W=[d_gates])
                if STOP == 4:
                    kb.barrier(); return
                for t0 in range(0, T, 4):
                    nn = min(4, T - t0)
                    for (srcb, d_src, j, dd, d_dd, pt, d_pt) in ((xcb, d_xcb, 1, ktok_d, d_ktok, pA, d_pA), (xmb, d_xmb, 2, vtok_d, d_vtok, pB, d_pB)):
                        for q in range(nn):
                            kb.I('tensor', 'matmul', out=pt[:, q * 128:(q + 1) * 128], lhsT=srcb[:, (t0 + q) * 128:(t0 + q + 1) * 128],
                                 rhs=bd[:, j, fc, :], start=True, stop=True, R=[d_src, d_bd], W=[d_pt])
                        o_, d_o = ob.next()
                        kb.I('vector' if j == 1 else 'scalar', 'tensor_copy' if j == 1 else 'copy', out=o_[:, 0:nn * 128], in_=pt[:, 0:nn * 128],
                             R=[d_pt], W=[d_o])
                        kb.dma(dd[tb0 + t0:tb0 + t0 + nn].rearrange("t p c -> p t c")[:, :, fc * 128:(fc + 1) * 128],
                               o_[:, 0:nn * 128].rearrange("p (t c) -> p t c", t=nn), R=[d_o], W=[d_dd])
            if STOP == 5:
                kb.barrier(); return
        kb.barrier()

    if 2 not in passes:
        return
    with ExitStack() as es:
        cm = E.sb([128, 7, 128], F32, "mcm", es=es); d_cm = Dep(const=True)
        kb.dma(cm[:], A['ml_cm'].rearrange("m p c -> p m c"), W=[d_cm])
        bgr = E.sb([1, 16], F32, "bgr", es=es); d_bgr = Dep(const=True)
        kb.dma(bgr[:], A['ml_b_gates'].rearrange("(o z) g -> o (z g)", o=1), W=[d_bgr])
        onec = E.sb([128, 1], F32, "monec", es=es); d_onec = Dep(const=True)
        kb.I('vector', 'memset', onec[:], 1.0, W=[d_onec])
        c22 = E.sb([128, 4], F32, "c22", es=es); d_c22 = Dep(const=True)
        kb.I('vector', 'memset', c22[:], 512.0 ** 0.5, W=[d_c22])
        Wo = E.sb([128, 16, D], BF16, "mWo", es=es); d_Wo = Dep()
        ngsk = E.sb([128, 2, 16], F32, "ngsk", es=es); d_ngsk = Dep()
        NGB = E.sb([128, 16, 128], F32, "NGB", es=es); SKB = E.sb([128, 16, 128], F32, "SKB", es=es); d_NS = Dep()
        nstg = E.sb([32, 128], F32, "nstg", es=es); d_nstg = Dep()
        kb.dma(nstg[0:16, :], A['ml_norm_g'].rearrange("(f p) -> f p", p=128), W=[d_nstg])
        kb.dma(nstg[16:32, :], A['ml_skip'].rearrange("(f p) -> f p", p=128), W=[d_nstg])
        kb.I('tensor', 'matmul', out=pA[:, 0:32], lhsT=nstg[0:32, :], rhs=E.idf[0:32, 0:32], start=True, stop=True, R=[d_nstg, E.d_idf], W=[d_pA])
        kb.I('vector', 'tensor_copy', out=ngsk[:].rearrange("p j f -> p (j f)"), in_=pA[:, 0:32], R=[d_pA], W=[d_ngsk])
        for f in range(16):
            kb.I('vector', 'tensor_scalar_mul', out=NGB[:, f, :], in0=E.ones[:, 0:128], scalar1=ngsk[:, 0, f:f + 1], R=[d_ngsk, E.d_ones], W=[d_NS])
            kb.I('vector', 'tensor_scalar_mul', out=SKB[:, f, :], in0=E.ones[:, 0:128], scalar1=ngsk[:, 1, f:f + 1], R=[d_ngsk, E.d_ones], W=[d_NS])
        with ExitStack() as es2:
            cs = Caster(E, es2)
            cs.cast3(Wo, d_Wo, A['ml_w_out'], 0, D, nk_total=16)
            kb.barrier()
        d_Wo.const = True; d_NS.const = True
        C32 = E.sb([128, 16, 512], F32, "C32", es=es); d_C32 = Dep()
        Cbf = E.sb([128, 16, 512], BF16, "Cbf", es=es); d_Cbf = Dep()
        n32 = E.sb([128, 16], F32, "n32", es=es); nbf = E.sb([128, 16], BF16, "nbf", es=es); d_n32 = Dep(); d_nbf = Dep()
        qT = Ring(E, [128, 16, 128], BF16, 1, "mqT", es=es); kT = Ring(E, [128, 16, 128], BF16, 1, "mkT", es=es)
        ktk = Ring(E, [128, 2048], BF16, 1, "mktk", es=es); vtk = Ring(E, [128, 2048], BF16, 1, "mvtk", es=es)
        qd = Ring(E, [128, 16, 128], BF16, 1, "mqd", es=es); kw = Ring(E, [128, 2048], BF16, 1, "mkw", es=es)
        sm = Ring(E, [128, 64], F32, 1, "msm", es=es)
        R1 = Ring(E, [128, 512], F32, 1, "mR1", es=es); EB = Ring(E, [128, 512], F32, 1, "mEB", es=es); Dm = Ring(E, [128, 512], F32, 1, "mDm", es=es)
        sT = Ring(E, [128, 512], BF16, 1, "msT", es=es)
        hdir = Ring(E, [128, 2048], F32, 1, "mhdir", es=es); hfl = Ring(E, [128, 2048], F32, 1, "mhfl", es=es)
        hnb = Ring(E, [128, 2048], BF16, 1, "mhnb", es=es); fin = Ring(E, [128, 16, 128], BF16, 1, "mfin", es=es)
        xcl = Ring(E, [128, 16, 128], BF16, 1, "mxcl", es=es); szl = Ring(E, [128, 16, 128], BF16, 1, "mszl", es=es)
        xs = Ring(E, [128, D], F32, 1, "mx2", es=es)
        seqt = [(0, 0), (0, 1)] + [(1, n) for n in range(32)]
        for direction in (0, 1):
            order = list(range(NTT)) if direction == 0 else [1, 0] + list(range(NTT - 1, 1, -1))
            U = cm[:, 0 + 2 * direction, :]; Lm = cm[:, 1 + 2 * direction, :]; mask = cm[:, 4 + direction, :]; negones = cm[:, 6, :]
            kb.I('vector', 'memset', C32[:], 0.0, W=[d_C32]); kb.I('vector', 'memset', Cbf[:], 0.0, W=[d_Cbf])
            kb.I('vector', 'memset', n32[:], 0.0, W=[d_n32]); kb.I('vector', 'memset', nbf[:], 0.0, W=[d_nbf])
            zb = direction * 8
            for ti in order:
                seg, n = seqt[ti]
                tsl = slice(ti * 128, (ti + 1) * 128)
                q_, d_q = qT.next(); k_, d_k = kT.next(); kt_, d_kt = ktk.next(); vt_, d_vt = vtk.next()
                for f0 in (0, 8):
                    kb.dma(q_[:, f0:f0 + 8, :], qT_d.rearrange("f p l -> p f l")[:, f0:f0 + 8, tsl], R=[d_qT], W=[d_q])
                for f0 in (0, 8):
                    kb.dma(k_[:, f0:f0 + 8, :], kT_d.rearrange("f p l -> p f l")[:, f0:f0 + 8, tsl], R=[d_kT], W=[d_k])
                kb.dma(kt_[:], ktok_d[ti], R=[d_ktok], W=[d_kt])
                kb.dma(vt_[:], vtok_d[ti], R=[d_vtok], W=[d_vt])
                s_, d_s = sm.next()
                kb.I('tensor', 'matmul', out=pC[:, 0:16], lhsT=gates[0:16, tsl], rhs=E.idf[0:16, 0:16], start=True, stop=False,
                     R=[d_gates, E.d_idf], W=[d_pC])
                kb.I('tensor', 'matmul', out=pC[:, 0:16], lhsT=E.ones[0:1, 0:128], rhs=bgr[0:1, :], start=False, stop=True,
                     R=[d_bgr, E.d_ones], W=[d_pC])
                kb.I('vector', 'tensor_copy', out=s_[:, 0:4], in_=pC[:, zb:zb + 4], R=[d_pC], W=[d_s])
                kb.I('scalar', 'activation', out=s_[:, 4:8], in_=pC[:, zb + 4:zb + 8], func=AF.Exp, scale=-1.0, R=[d_pC], W=[d_s])
                kb.I('scalar', 'activation', out=s_[:, 8:12], in_=s_[:, 4:8], func=AF.Ln, bias=onec[:, 0:1], scale=1.0, R=[d_s, d_onec], W=[d_s])
                lsp = s_[:, 8:12]
                kb.I('tensor', 'matmul', out=pC[:, 16:20], lhsT=U, rhs=lsp, start=True, stop=True, R=[d_s, d_cm], W=[d_pC])
                kb.I('tensor', 'matmul', out=pC[:, 20:24], lhsT=Lm, rhs=lsp, start=True, stop=True, R=[d_s, d_cm], W=[d_pC])
                kb.I('tensor', 'matmul', out=pC[:, 24:28], lhsT=negones, rhs=lsp, start=True, stop=True, R=[d_s, d_cm], W=[d_pC])
                kb.I('vector', 'tensor_tensor', out=s_[:, 12:16], in0=s_[:, 0:4], in1=pC[:, 16:20], op=ALU.subtract, R=[d_s, d_pC], W=[d_s])
                kb.I('vector', 'tensor_tensor', out=s_[:, 16:20], in0=pC[:, 20:24], in1=s_[:, 0:4], op=ALU.add, R=[d_s, d_pC], W=[d_s])
                kb.I('scalar', 'activation', out=s_[:, 16:20], in_=s_[:, 16:20], func=AF.Exp, R=[d_s], W=[d_s])
                kb.I('scalar', 'activation', out=s_[:, 20:24], in_=pC[:, 24:28], func=AF.Exp, R=[d_pC], W=[d_s])
                r1, d_r1 = R1.next()
                for h in range(4):
                    kb.I('vector', 'tensor_scalar_mul', out=r1[:, h * 128:(h + 1) * 128], in0=U, scalar1=s_[:, 8 + h:9 + h], R=[d_s, d_cm], W=[d_r1])
                kb.I('tensor', 'matmul', out=pB[:], lhsT=E.ones[:, 0:128], rhs=r1[:], start=True, stop=True, R=[d_r1, E.d_ones], W=[d_pB])
                eb, d_eb = EB.next(); dmt, d_dm = Dm.next()
                kb.I('scalar', 'activation', out=eb[:], in_=pB[:], func=AF.Exp, R=[d_pB], W=[d_eb])
                for h in range(4):
                    kb.I('scalar', 'activation', out=dmt[:, h * 128:(h + 1) * 128], in_=pB[:, h * 128:(h + 1) * 128], func=AF.Exp,
                         bias=s_[:, 12 + h:13 + h], scale=1.0, R=[d_pB, d_s], W=[d_dm])
                    kb.I('gpsimd', 'tensor_tensor', out=dmt[:, h * 128:(h + 1) * 128], in0=dmt[:, h * 128:(h + 1) * 128], in1=mask, op=ALU.mult,
                         R=[d_dm, d_cm], W=[d_dm])
                for h in range(4):
                    for c in range(4):
                        kb.I('tensor', 'matmul', out=pA[:, h * 128:(h + 1) * 128], lhsT=k_[:, 4 * h + c, :], rhs=q_[:, 4 * h + c, :],
                             start=(c == 0), stop=(c == 3), R=[d_k, d_q], W=[d_pA])
                st_, d_st_ = sT.next()
                kb.I('vector', 'tensor_tensor', out=st_[:], in0=pA[:], in1=dmt[:], op=ALU.mult, R=[d_pA, d_dm], W=[d_st_])
                qd_, d_qd = qd.next()
                for f in range(16):
                    h = f // 4
                    kb.I('vector' if f % 2 == 0 else 'gpsimd', 'tensor_tensor', out=qd_[:, f, :], in0=q_[:, f, :], in1=eb[:, h * 128:(h + 1) * 128],
                         op=ALU.mult, R=[d_q, d_eb], W=[d_qd])
                kw_, d_kw = kw.next()
                for h in range(4):
                    kb.I('vector', 'tensor_scalar_mul', out=kw_[:, h * 512:(h + 1) * 512], in0=kt_[:, h * 512:(h + 1) * 512],
                         scalar1=s_[:, 16 + h:17 + h], R=[d_kt, d_s], W=[d_kw])
                for h in range(4):
                    kb.I('tensor', 'matmul', out=p4[:, h * 512:(h + 1) * 512], lhsT=st_[:, h * 128:(h + 1) * 128], rhs=vt_[:, h * 512:(h + 1) * 512],
                         start=True, stop=False, R=[d_st_, d_vt], W=[d_p4])
                    for c in range(4):
                        kb.I('tensor', 'matmul', out=p4[:, h * 512:(h + 1) * 512], lhsT=qd_[:, 4 * h + c, :], rhs=Cbf[:, 4 * h + c, :],
                             start=False, stop=(c == 3), R=[d_qd, d_Cbf], W=[d_p4])
                    kb.I('tensor', 'matmul', out=pC[:, 32 + h:33 + h], lhsT=st_[:, h * 128:(h + 1) * 128], rhs=E.onesb[:, 0:1],
                         start=True, stop=False, R=[d_st_, E.d_onesb], W=[d_pC])
                    for c in range(4):
                        kb.I('tensor', 'matmul', out=pC[:, 32 + h:33 + h], lhsT=qd_[:, 4 * h + c, :], rhs=nbf[:, 4 * h + c:4 * h + c + 1],
                             start=False, stop=(c == 3), R=[d_qd, d_nbf], W=[d_pC])
                kb.I('scalar', 'activation', out=s_[:, 24:28], in_=pC[:, 32:36], func=AF.Abs, R=[d_pC], W=[d_s])
                kb.I('vector', 'tensor_tensor', out=s_[:, 24:28], in0=s_[:, 24:28], in1=c22[:], op=ALU.max, R=[d_s, d_c22], W=[d_s])
                kb.I('vector', 'reciprocal', out=s_[:, 24:28], in_=s_[:, 24:28], R=[d_s], W=[d_s])
                hd_, d_hd = hdir.next()
                for h in range(4):
                    if h % 2 == 0:
                        kb.I('vector', 'tensor_scalar_mul', out=hd_[:, h * 512:(h + 1) * 512], in0=p4[:, h * 512:(h + 1) * 512],
                             scalar1=s_[:, 24 + h:25 + h], R=[d_p4, d_s], W=[d_hd])
                    else:
                        kb.I('scalar', 'mul', out=hd_[:, h * 512:(h + 1) * 512], in_=p4[:, h * 512:(h + 1) * 512], mul=s_[:, 24 + h:25 + h],
                             R=[d_p4, d_s], W=[d_hd])
                for h in range(4):
                    for c in range(4):
                        f = 4 * h + c
                        kb.I('tensor', 'matmul', out=p4[:, c * 512:(c + 1) * 512], lhsT=kw_[:, f * 128:(f + 1) * 128], rhs=vt_[:, h * 512:(h + 1) * 512],
                             start=True, stop=True, R=[d_kw, d_vt], W=[d_p4])
                        kb.I('tensor', 'matmul', out=pC[:, 48 + f:49 + f], lhsT=kw_[:, f * 128:(f + 1) * 128], rhs=E.onesb[:, 0:1],
                             start=True, stop=True, R=[d_kw, E.d_onesb], W=[d_pC])
                    for c in range(4):
                        f = 4 * h + c
                        kb.I('vector', 'scalar_tensor_tensor', out=C32[:, f, :], in0=C32[:, f, :], scalar=s_[:, 20 + h:21 + h],
                             in1=p4[:, c * 512:(c + 1) * 512], op0=ALU.mult, op1=ALU.add, R=[d_C32, d_s, d_p4], W=[d_C32])
                    kb.I('scalar', 'copy', out=Cbf[:, 4 * h:4 * h + 4, :], in_=C32[:, 4 * h:4 * h + 4, :], R=[d_C32], W=[d_Cbf])
                    kb.I('vector', 'scalar_tensor_tensor', out=n32[:, 4 * h:4 * h + 4], in0=n32[:, 4 * h:4 * h + 4], scalar=s_[:, 20 + h:21 + h],
                         in1=pC[:, 48 + 4 * h:52 + 4 * h], op0=ALU.mult, op1=ALU.add, R=[d_n32, d_s, d_pC], W=[d_n32])
                kb.I('vector', 'tensor_copy', out=nbf[:], in_=n32[:], R=[d_n32], W=[d_nbf])
                if direction == 0:
                    kb.dma(hf_d[ti], hd_[:], R=[d_hd], W=[d_hf[ti]])
                    continue
                hf_, d_hf_ = hfl.next()
                kb.dma(hf_[:], hf_d[ti], R=[d_hf[ti]], W=[d_hf_])
                kb.I('gpsimd', 'tensor_tensor', out=hd_[:], in0=hd_[:], in1=hf_[:], op=ALU.add, R=[d_hd, d_hf_], W=[d_hd])
                hn_, d_hn = hnb.next()
                for h in range(4):
                    hs = slice(h * 512, (h + 1) * 512)
                    kb.I('scalar', 'activation', out=hf_[:, hs], in_=hd_[:, hs], func=AF.Identity, accum_out=s_[:, 28 + h:29 + h],
                         R=[d_hd], W=[d_hf_, d_s])
                kb.I('scalar', 'mul', out=s_[:, 28:32], in_=s_[:, 28:32], mul=-1.0 / 512, R=[d_s], W=[d_s])
                for h in range(4):
                    hs = slice(h * 512, (h + 1) * 512)
                    kb.I('scalar', 'activation', out=hd_[:, hs], in_=hd_[:, hs], func=AF.Identity, bias=s_[:, 28 + h:29 + h], scale=1.0,
                         R=[d_hd, d_s], W=[d_hd])
                    kb.I('scalar', 'activation', out=hf_[:, hs], in_=hd_[:, hs], func=AF.Square, accum_out=s_[:, 32 + h:33 + h],
                         R=[d_hd], W=[d_hf_, d_s])
                kb.I('scalar', 'activation', out=s_[:, 36:40], in_=s_[:, 32:36], func=AF.Sqrt, bias=E.epsc[:, 0:1], scale=1.0 / 512,
                     R=[d_s, E.d_eps], W=[d_s])
                kb.I('vector', 'reciprocal', out=s_[:, 36:40], in_=s_[:, 36:40], R=[d_s], W=[d_s])
                for h in range(4):
                    hs = slice(h * 512, (h + 1) * 512)
                    kb.I('vector', 'tensor_scalar_mul', out=hn_[:, hs], in0=hd_[:, hs], scalar1=s_[:, 36 + h:37 + h], R=[d_hd, d_s], W=[d_hn])
                xc_, d_xc = xcl.next(); sz_, d_sz = szl.next()
                for f0 in (0, 8):
                    kb.dma(xc_[:, f0:f0 + 8, :], xcT_d.rearrange("f p l -> p f l")[:, f0:f0 + 8, tsl], R=[d_xcT], W=[d_xc])
                for f0 in (0, 8):
                    kb.dma(sz_[:, f0:f0 + 8, :], szT_d.rearrange("f p l -> p f l")[:, f0:f0 + 8, tsl], R=[d_szT], W=[d_sz])
                ff_ = hf_[:].rearrange("p (f c) -> p f c", f=16); d_ff = d_hf_; fi_, d_fi = fin.next()
                for rnd in range(2):
                    for k in range(8):
                        f = rnd * 8 + k
                        kb.I('tensor', 'transpose', out=nt.pT[0][:, k, :], in_=hn_[:, f * 128:(f + 1) * 128], identity=E.idb[:],
                             R=[d_hn, E.d_idb], W=[nt.d_pT[0]])
                    kb.I('vector', 'tensor_tensor', out=ff_[:, rnd * 8:(rnd + 1) * 8, :], in0=nt.pT[0][:], in1=NGB[:, rnd * 8:(rnd + 1) * 8, :],
                         op=ALU.mult, R=[nt.d_pT[0], d_NS], W=[d_ff])
                kb.I('gpsimd', 'tensor_tensor', out=fi_[:], in0=xc_[:], in1=SKB[:], op=ALU.mult, R=[d_xc, d_NS], W=[d_fi])
                kb.I('vector', 'tensor_tensor', out=ff_[:], in0=ff_[:], in1=fi_[:], op=ALU.add, R=[d_ff, d_fi], W=[d_ff])
                kb.I('vector', 'tensor_tensor', out=fi_[:], in0=ff_[:], in1=sz_[:], op=ALU.mult, R=[d_ff, d_sz], W=[d_fi])
                x, d_x = xs.next()
                load_tile(E, x, d_x, A, seg, n)
                for half in range(2):
                    for f in range(16):
                        kb.I('tensor', 'matmul', out=p4[:, half * 512:(half + 1) * 512], lhsT=fi_[:, f, :], rhs=Wo[:, f, half * 512:(half + 1) * 512],
                             start=(f == 0), stop=(f == 15), R=[d_fi, d_Wo], W=[d_p4])
                post.run(p4[:, 0:1024], d_p4, x[:], d_x, GGc if seg == 0 else GGl, (dc if seg == 0 else dl)[2])
                store_tile(E, x, d_x, A, seg, n)
        kb.barrier()


def cmul(kb, outr, outi, ar, ai, br, bi, t1, t2, R, d_out, d_t):
    kb.I('vector', 'tensor_tensor', out=t1, in0=ar, in1=br, op=ALU.mult, R=R, W=[d_t])
    kb.I('vector', 'tensor_tensor', out=t2, in0=ai, in1=bi, op=ALU.mult, R=R, W=[d_t])
    kb.I('gpsimd', 'tensor_tensor', out=outr, in0=t1, in1=t2, op=ALU.subtract, R=[d_t], W=[d_out])
    kb.I('vector', 'tensor_tensor', out=t1, in0=ar, in1=bi, op=ALU.mult, R=R, W=[d_t])
    kb.I('vector', 'tensor_tensor', out=t2, in0=ai, in1=br, op=ALU.mult, R=R, W=[d_t])
    kb.I('gpsimd', 'tensor_tensor', out=outi, in0=t1, in1=t2, op=ALU.add, R=[d_t], W=[d_out])


def s5_layer(E, A):
    kb = E.kb
    NTT = 34
    PI = math.pi
    Pl = alloc_mod(E); Pc = alloc_mod(E)
    post = PostRes(E, nbuf=1)
    pU = E.ps([128, 8, 128], F32, "pU"); d_pU = PDep()
    pBr = E.ps([128, 512], F32, "pBr"); d_pBr = PDep()
    pBi = E.ps([128, 512], F32, "pBi"); d_pBi = PDep()
    pBr2 = E.ps([128, 512], F32, "pBr2"); d_pBr2 = PDep()
    pBi2 = E.ps([128, 512], F32, "pBi2"); d_pBi2 = PDep()
    pY = E.ps([128, 1024], F32, "pY"); d_pY = PDep()
    E.n += 1
    u = E.n
    yf_d = E.dram(f"syf_{u}", [NTT, 128, 1024], F32); gT_d = E.dram(f"sgT_{u}", [NTT, 128, 1024], BF16)
    d_yf = [Dep() for _ in range(NTT)]; d_gT = [Dep() for _ in range(NTT)]
    with ExitStack() as es:
        (Gl, Sl, GGl, dl), (Gc, Sc, GGc, dc) = compute_mod2(E, Pl, Pc, A['c'], A['cc'], A['mod_w'], A['mod_b'], A['norm_g'], 0, es, pBr, d_pBr, pBi, d_pBi)
        kb.barrier()
    for dd in list(dl) + list(dc):
        dd.const = True
    seqt = [(0, 0), (0, 1)] + [(1, n) for n in range(32)]

    with ExitStack() as es:
        Bb = E.sb([128, 2, 32, 128], BF16, "sBb", es=es); d_Bb = Dep()
        Cc = E.sb([128, 2, 32, 128], BF16, "sCc", es=es); d_Cc = Dep()
        RJ = E.sb([128, 2, 128], F32, "sRJ", es=es); RJb = E.sb([128, 2, 128], BF16, "sRJb", es=es); d_RJ = Dep()
        dcol = E.sb([128, 8], F32, "sdcol", es=es); d_dcol = Dep()
        cst = E.sb([128, 4], F32, "scst", es=es); d_cst = Dep()
        on128 = E.ones[:, 0:128]
        kb.dma(RJ[:, 0, :], A['ident'], W=[d_RJ]); kb.dma(RJ[:, 1, :], A['s5_J'], W=[d_RJ])
        kb.I('vector', 'tensor_copy', out=RJb[:], in_=RJ[:], R=[d_RJ], W=[d_RJ])
        kb.dma(dcol[:], A['s5_dcol'], W=[d_dcol])
        kb.I('vector', 'memset', cst[:, 0:1], 1.0, W=[d_cst]); kb.I('vector', 'memset', cst[:, 1:2], PI / 2, W=[d_cst])
        with ExitStack() as es2:
            stg = Ring(E, [128, 2048], F32, 2, "sstg", es=es2)
            for ri, nm in enumerate(('s5_Bre', 's5_Bim')):
                for q0 in range(0, 32, 16):
                    st, d_st = stg.next()
                    kb.dma(st[:].rearrange("p (q c) -> p q c", q=16), A[nm][q0:q0 + 16].rearrange("q p c -> p q c"), W=[d_st])
                    kb.I('vector', 'tensor_copy', out=Bb[:, ri, q0:q0 + 16, :], in_=st[:].rearrange("p (q c) -> p q c", q=16), R=[d_st], W=[d_Bb])
            for ri, nm in enumerate(('s5_Cre', 's5_Cim')):
                for q0 in range(0, 32, 16):
                    st, d_st = stg.next()
                    kb.dma(st[:].rearrange("p (q c) -> p q c", q=16), A[nm][q0:q0 + 16].rearrange("q p c -> p q c"), W=[d_st])
                    kb.I('scalar', 'mul', out=Cc[:, ri, q0:q0 + 16, :], in_=st[:].rearrange("p (q c) -> p q c", q=16), mul=(1.0 if ri == 0 else -1.0),
                         R=[d_st], W=[d_Cc])
            kb.barrier()
        d_Bb.const = True; d_Cc.const = True; d_RJ.const = True; d_dcol.const = True; d_cst.const = True
        LP = E.sb([128, 2, 32, 128], F32, "sLP", es=es); LN = E.sb([128, 2, 32, 128], F32, "sLN", es=es); d_LP = Dep(); d_LN = Dep()
        X = E.sb([128, 2, 32, 128], F32, "sX", es=es); d_X = [[Dep() for _ in range(8)] for _ in range(2)]
        pr = E.sb([128, 24, 32], F32, "spr", es=es); d_pr = Dep()
        SET = []
        for _s in range(2):
            SET.append(dict(
                Br=E.sb([128, 512], BF16, "sBr", es=es), Bi=E.sb([128, 512], BF16, "sBi", es=es),
                A1=E.sb([128, 512], F32, "sA1", es=es), A2=E.sb([128, 512], F32, "sA2", es=es),
                P1=E.sb([128, 512], F32, "sP1", es=es), P2=E.sb([128, 512], F32, "sP2", es=es),
                Zr=E.sb([128, 512], F32, "sZr", es=es), Zi=E.sb([128, 512], F32, "sZi", es=es),
                d=dict((k, Dep()) for k in ('Br', 'Bi', 'A1', 'A2', 'P1', 'P2', 'Zr', 'Zi'))))
        wk = [SET[0]['A1'], SET[0]['A2']]; d_wk = [SET[0]['d']['A1'], SET[0]['d']['A2']]
        Xb = Ring(E, [128, 2, 4, 128], BF16, 2, "sXb", es=es)
        rmask = E.sb([128, 512], F32, "srmask", es=es); d_rmask = Dep()
        kb.I('vector', 'memset', rmask[:], 1.0, W=[d_rmask])
        kb.I('vector', 'memset', rmask[:].rearrange("p (q t) -> p q t", q=4)[:, :, 0:1], 0.0, W=[d_rmask])
        d_rmask.const = True
        xs = Ring(E, [128, D], F32, 1, "sx", es=es)
        ss = E.sb([128, 2], F32, "sss", es=es); d_ss = Dep()
        xb = E.sb([128, D], BF16, "sxb", es=es); d_xb = Dep()
        uT = E.sb([128, 8, 128], BF16, "suT", es=es); d_uT = Dep()
        ysb = E.sb([128, D], F32, "sysb", es=es); d_ysb = Dep()
        yfs = E.sb([128, 8, 128], F32, "syfs", es=es); d_yfs = Dep()
        yfl = E.sb([128, 8, 128], F32, "syfl", es=es); d_yfl = Dep()
        g1 = E.sb([128, D], F32, "sg1", es=es); d_g1 = Dep()
        sq = g1; d_sq = d_g1
        gTo = xb; d_gTo = d_xb

        def P(i):
            return pr[:, i, :]

        for direction in (0, 1):
            kb.dma(P(0), A['s5_lreT'][direction], W=[d_pr]); kb.dma(P(1), A['s5_limT'][direction], W=[d_pr])
            kb.dma(P(2), A['s5_ldtT'][direction], W=[d_pr])
            I = lambda eng, name, **kw: kb.I(eng, name, R=[d_pr, d_cst], W=[d_pr], **kw)
            I('scalar', 'activation', out=P(2), in_=P(2), func=AF.Exp)
            I('vector', 'tensor_tensor', out=P(3), in0=P(0), in1=P(2), op=ALU.mult)
            I('vector', 'tensor_tensor', out=P(4), in0=P(1), in1=P(2), op=ALU.mult)
            I('scalar', 'activation', out=P(5), in_=P(3), func=AF.Exp)
            I('scalar', 'activation', out=P(6), in_=P(3), func=AF.Exp, scale=-1.0)
            I('vector', 'tensor_scalar', out=P(7), in0=P(4), scalar1=PI, scalar2=1.0, op0=ALU.is_ge, op1=ALU.mult)
            for m in (3, 5, 7):
                I('vector', 'tensor_scalar', out=P(8), in0=P(4), scalar1=m * PI, scalar2=1.0, op0=ALU.is_ge, op1=ALU.mult)
                I('vector', 'tensor_tensor', out=P(7), in0=P(7), in1=P(8), op=ALU.add)
            I('vector', 'scalar_tensor_tensor', out=P(8), in0=P(7), scalar=-6.28125, in1=P(4), op0=ALU.mult, op1=ALU.add)
            I('vector', 'scalar_tensor_tensor', out=P(8), in0=P(7), scalar=-(2 * PI - 6.28125), in1=P(8), op0=ALU.mult, op1=ALU.add)
            I('scalar', 'activation', out=P(9), in_=P(8), func=AF.Sin)
            I('scalar', 'activation', out=P(10), in_=P(8), func=AF.Abs)
            I('scalar', 'activation', out=P(10), in_=P(10), func=AF.Sin, scale=-1.0, bias=cst[:, 1:2])
            I('vector', 'tensor_tensor', out=P(11), in0=P(5), in1=P(10), op=ALU.mult)
            I('vector', 'tensor_tensor', out=P(12), in0=P(5), in1=P(9), op=ALU.mult)
            I('vector', 'tensor_tensor', out=P(13), in0=P(6), in1=P(10), op=ALU.mult)
            I('vector', 'scalar_tensor_tensor', out=P(14), in0=P(6), scalar=-1.0, in1=P(9), op0=ALU.mult, op1=ALU.mult)
            I('vector', 'tensor_scalar', out=P(15), in0=P(11), scalar1=-1.0, scalar2=1.0, op0=ALU.add, op1=ALU.mult)
            I('vector', 'tensor_tensor', out=P(16), in0=P(0), in1=P(0), op=ALU.mult)
            I('vector', 'tensor_tensor', out=P(17), in0=P(1), in1=P(1), op=ALU.mult)
            I('vector', 'tensor_tensor', out=P(16), in0=P(16), in1=P(17), op=ALU.add)
            I('vector', 'reciprocal', out=P(16), in_=P(16))
            I('vector', 'tensor_tensor', out=P(17), in0=P(15), in1=P(0), op=ALU.mult)
            I('vector', 'tensor_tensor', out=P(18), in0=P(12), in1=P(1), op=ALU.mult)
            I('vector', 'tensor_tensor', out=P(17), in0=P(17), in1=P(18), op=ALU.add)
            I('vector', 'tensor_tensor', out=P(17), in0=P(17), in1=P(16), op=ALU.mult)
            I('vector', 'tensor_tensor', out=P(18), in0=P(12), in1=P(0), op=ALU.mult)
            I('vector', 'tensor_tensor', out=P(19), in0=P(15), in1=P(1), op=ALU.mult)
            I('vector', 'tensor_tensor', out=P(18), in0=P(18), in1=P(19), op=ALU.subtract)
            I('vector', 'tensor_tensor', out=P(18), in0=P(18), in1=P(16), op=ALU.mult)
            cmul(kb, P(19), P(20), P(17), P(18), P(13), P(14), P(21), P(22), [d_pr], d_pr, d_pr)
            for (tbl, d_tbl, b_r, b_i, m_r, m_i) in ((LP, d_LP, 11, 12, 11, 12), (LN, d_LN, 19, 20, 13, 14)):
                kb.I('vector', 'tensor_copy', out=tbl[:, 0, :, 0], in_=P(b_r), R=[d_pr], W=[d_tbl])
                kb.I('vector', 'tensor_copy', out=tbl[:, 1, :, 0], in_=P(b_i), R=[d_pr], W=[d_tbl])
                kb.I('vector', 'tensor_copy', out=P(15), in_=P(m_r), R=[d_pr], W=[d_pr])
                kb.I('vector', 'tensor_copy', out=P(16), in_=P(m_i), R=[d_pr], W=[d_pr])
                n = 1
                while n < 128:
                    shp = [128, 32, n]
                    t1 = wk[0][:, 0:32 * n].rearrange("p (q t) -> p q t", q=32) if 32 * n <= 512 else g1[:, 0:32 * n // 4].rearrange("p (q t) -> p q t", q=32) if False else None
                    t1 = sq[:, 0:1024].rearrange("p (q t) -> p q t", q=32)[:, :, 0:min(n, 32)] if n <= 32 else None
                    for c0 in range(0, n, 32):
                        cn = min(32, n - c0)
                        ta = sq[:].rearrange("p (q t) -> p q t", q=32)[:, :, 0:cn]
                        tb_ = ysb[:].rearrange("p (q t) -> p q t", q=32)[:, :, 0:cn]
                        src_r = tbl[:, 0, :, c0:c0 + cn]; src_i = tbl[:, 1, :, c0:c0 + cn]
                        pwr = pr[:, 15, :].unsqueeze(2).to_broadcast([128, 32, cn]); pwi = pr[:, 16, :].unsqueeze(2).to_broadcast([128, 32, cn])
                        kb.I('vector', 'tensor_tensor', out=ta, in0=src_r, in1=pwr, op=ALU.mult, R=[d_tbl, d_pr], W=[d_sq])
                        kb.I('vector', 'tensor_tensor', out=tb_, in0=src_i, in1=pwi, op=ALU.mult, R=[d_tbl, d_pr], W=[d_ysb])
                        kb.I('gpsimd', 'tensor_tensor', out=tbl[:, 0, :, n + c0:n + c0 + cn], in0=ta, in1=tb_, op=ALU.subtract, R=[d_sq, d_ysb], W=[d_tbl])
                        kb.I('vector', 'tensor_tensor', out=ta, in0=src_r, in1=pwi, op=ALU.mult, R=[d_tbl, d_pr], W=[d_sq])
                        kb.I('vector', 'tensor_tensor', out=tb_, in0=src_i, in1=pwr, op=ALU.mult, R=[d_tbl, d_pr], W=[d_ysb])
                        kb.I('gpsimd', 'tensor_tensor', out=tbl[:, 1, :, n + c0:n + c0 + cn], in0=ta, in1=tb_, op=ALU.add, R=[d_sq, d_ysb], W=[d_tbl])
                    cmul(kb, P(21), P(22), P(15), P(16), P(15), P(16), P(23), P(8), [d_pr], d_pr, d_pr)
                    kb.I('vector', 'tensor_copy', out=P(15), in_=P(21), R=[d_pr], W=[d_pr])
                    kb.I('vector', 'tensor_copy', out=P(16), in_=P(22), R=[d_pr], W=[d_pr])
                    n *= 2
            for f in range(8):
                kb.I('vector', 'memset', X[:, :, 4 * f:4 * f + 4, 127:128], 0.0, W=[d_X[0][f], d_X[1][f]])
            order = list(range(NTT)) if direction == 0 else [1, 0] + list(range(NTT - 1, 1, -1))
            for ti in order:
                seg, n_ = seqt[ti]
                G, S_, dm = (Gc, Sc, dc) if seg == 0 else (Gl, Sl, dl)
                x, d_x = xs.next()
                load_tile(E, x, d_x, A, seg, n_)
                kb.I('scalar', 'activation', out=sq[:], in_=x[:], func=AF.Square, accum_out=ss[:, 0:1], R=[d_x], W=[d_sq, d_ss])
                kb.I('scalar', 'activation', out=ss[:, 1:2], in_=ss[:, 0:1], func=AF.Sqrt, bias=E.epsc[:, 0:1], scale=1.0 / D, R=[d_ss, E.d_eps], W=[d_ss])
                kb.I('vector', 'reciprocal', out=ss[:, 1:2], in_=ss[:, 1:2], R=[d_ss], W=[d_ss])
                kb.I('vector', 'tensor_scalar_mul', out=xb[:], in0=x[:], scalar1=ss[:, 1:2], R=[d_x, d_ss], W=[d_xb])
                for k in range(8):
                    kb.I('tensor', 'matmul', out=pU[:, k, :], lhsT=xb[:, k * 128:(k + 1) * 128], rhs=RJb[:, direction, :], start=True, stop=True,
                         R=[d_xb, d_RJ], W=[d_pU])
                for k in range(8):
                    kb.I('scalar', 'activation', out=uT[:, k, :], in_=pU[:, k, :], func=AF.Identity, bias=S_[:, k:k + 1], scale=G[:, k:k + 1],
                         R=[d_pU] + list(dm[:2]), W=[d_uT])
                v4 = lambda t: t[:].rearrange("p (q t) -> p q t", q=4)
                PSS = ((pBr, d_pBr, pBi, d_pBi), (pBr2, d_pBr2, pBi2, d_pBi2))
                for j in range(4):
                    for s_ in range(2):
                        fc = 2 * j + s_; S = SET[s_]; dS = S['d']; pr_, d_pr_, pi_, d_pi_ = PSS[s_]
                        for pi in range(4):
                            q = 4 * fc + pi
                            kb.I('tensor', 'matmul', out=pr_[:, pi * 128:(pi + 1) * 128], lhsT=Bb[:, 0, q, :], rhs=uT[:, fc, :], start=True, stop=True,
                                 R=[d_Bb, d_uT], W=[d_pr_])
                            kb.I('tensor', 'matmul', out=pi_[:, pi * 128:(pi + 1) * 128], lhsT=Bb[:, 1, q, :], rhs=uT[:, fc, :], start=True, stop=True,
                                 R=[d_Bb, d_uT], W=[d_pi_])
                        kb.I('scalar', 'copy', out=S['Br'][:], in_=pr_[:], R=[d_pr_], W=[dS['Br']])
                        kb.I('scalar', 'copy', out=S['Bi'][:], in_=pi_[:], R=[d_pi_], W=[dS['Bi']])
                    for s_ in range(2):
                        fc = 2 * j + s_; S = SET[s_]; dS = S['d']
                        lnr = LN[:, 0, 4 * fc:4 * fc + 4, :]; lni = LN[:, 1, 4 * fc:4 * fc + 4, :]
                        kb.I('vector', 'tensor_tensor', out=v4(S['A1']), in0=v4(S['Br']), in1=lnr, op=ALU.mult, R=[dS['Br'], d_LN], W=[dS['A1']])
                        kb.I('vector', 'tensor_tensor', out=v4(S['A2']), in0=v4(S['Bi']), in1=lni, op=ALU.mult, R=[dS['Bi'], d_LN], W=[dS['A2']])
                        kb.I('vector', 'tensor_tensor', out=S['A1'][:], in0=S['A1'][:], in1=S['A2'][:], op=ALU.subtract, R=[dS['A2']], W=[dS['A1']])
                        kb.I('gpsimd', 'tensor_tensor', out=v4(S['P1']), in0=v4(S['Br']), in1=lni, op=ALU.mult, R=[dS['Br'], d_LN], W=[dS['P1']])
                        kb.I('gpsimd', 'tensor_tensor', out=v4(S['P2']), in0=v4(S['Bi']), in1=lnr, op=ALU.mult, R=[dS['Bi'], d_LN], W=[dS['P2']])
                        kb.I('gpsimd', 'tensor_tensor', out=S['P1'][:], in0=S['P1'][:], in1=S['P2'][:], op=ALU.add, R=[dS['P2']], W=[dS['P1']])
                    for s_ in range(2):
                        fc = 2 * j + s_; S = SET[s_]; dS = S['d']
                        kb.I('vector', 'tensor_tensor', out=v4(S['A1'])[:, :, 0], in0=v4(S['A1'])[:, :, 0], in1=X[:, 0, 4 * fc:4 * fc + 4, 127], op=ALU.add,
                             R=[d_X[0][fc]], W=[dS['A1']])
                        kb.I('gpsimd', 'tensor_tensor', out=v4(S['P1'])[:, :, 0], in0=v4(S['P1'])[:, :, 0], in1=X[:, 1, 4 * fc:4 * fc + 4, 127], op=ALU.add,
                             R=[d_X[1][fc]], W=[dS['P1']])
                    for s_ in range(2):
                        S = SET[s_]; dS = S['d']
                        kb.I('vector', 'tensor_tensor_scan', out=S['Zr'][:], data0=rmask[:], data1=S['A1'][:], initial=0.0, op0=ALU.mult, op1=ALU.add,
                             R=[dS['A1'], d_rmask], W=[dS['Zr']])
                        kb.I('vector', 'tensor_tensor_scan', out=S['Zi'][:], data0=rmask[:], data1=S['P1'][:], initial=0.0, op0=ALU.mult, op1=ALU.add,
                             R=[dS['P1'], d_rmask], W=[dS['Zi']])
                    for s_ in range(2):
                        fc = 2 * j + s_; S = SET[s_]; dS = S['d']
                        lpr = LP[:, 0, 4 * fc:4 * fc + 4, :]; lpi = LP[:, 1, 4 * fc:4 * fc + 4, :]
                        kb.I('vector', 'tensor_tensor', out=v4(S['A1']), in0=v4(S['Zr']), in1=lpr, op=ALU.mult, R=[dS['Zr'], d_LP], W=[dS['A1']])
                        kb.I('vector', 'tensor_tensor', out=v4(S['A2']), in0=v4(S['Zi']), in1=lpi, op=ALU.mult, R=[dS['Zi'], d_LP], W=[dS['A2']])
                        kb.I('vector', 'tensor_tensor', out=X[:, 0, 4 * fc:4 * fc + 4, :], in0=v4(S['A1']), in1=v4(S['A2']), op=ALU.subtract,
                             R=[dS['A1'], dS['A2']], W=[d_X[0][fc]])
                        kb.I('gpsimd', 'tensor_tensor', out=v4(S['P1']), in0=v4(S['Zr']), in1=lpi, op=ALU.mult, R=[dS['Zr'], d_LP], W=[dS['P1']])
                        kb.I('gpsimd', 'tensor_tensor', out=v4(S['P2']), in0=v4(S['Zi']), in1=lpr, op=ALU.mult, R=[dS['Zi'], d_LP], W=[dS['P2']])
                        kb.I('gpsimd', 'tensor_tensor', out=X[:, 1, 4 * fc:4 * fc + 4, :], in0=v4(S['P1']), in1=v4(S['P2']), op=ALU.add,
                             R=[dS['P1'], dS['P2']], W=[d_X[1][fc]])
                    if seg == 1:
                        for s_ in range(2):
                            fc = 2 * j + s_
                            xb_, d_xb_ = Xb.next()
                            for ri in range(2):
                                kb.I('scalar', 'copy', out=xb_[:, ri], in_=X[:, ri, 4 * fc:4 * fc + 4, :], R=[d_X[ri][fc]], W=[d_xb_])
                            for pi in range(4):
                                q = 4 * fc + pi
                                for ri in range(2):
                                    kb.I('tensor', 'matmul', out=pY[:, fc * 128:(fc + 1) * 128], lhsT=xb_[:, ri, pi, :], rhs=Cc[:, ri, q, :],
                                         start=(pi == 0 and ri == 0), stop=(pi == 3 and ri == 1), R=[d_xb_, d_Cc], W=[d_pY])
                if seg == 0:
                    continue
                kb.I('scalar', 'copy', out=ysb[:], in_=pY[:], R=[d_pY], W=[d_ysb])
                for fc in range(8):
                    kb.I('tensor', 'matmul', out=pU[:, fc, :], lhsT=ysb[:, fc * 128:(fc + 1) * 128], rhs=RJ[:, direction, :], start=True, stop=True,
                         R=[d_ysb, d_RJ], W=[d_pU])
                if direction == 0:
                    for fc in range(8):
                        kb.I('vector', 'scalar_tensor_tensor', out=yfs[:, fc, :], in0=uT[:, fc, :], scalar=dcol[:, fc:fc + 1], in1=pU[:, fc, :],
                             op0=ALU.mult, op1=ALU.add, R=[d_uT, d_dcol, d_pU], W=[d_yfs])
                    kb.dma(yf_d[ti], yfs[:].rearrange("p f t -> p (f t)"), R=[d_yfs], W=[d_yf[ti]])
                    continue
                kb.dma(yfl[:].rearrange("p f t -> p (f t)"), yf_d[ti], R=[d_yf[ti]], W=[d_yfl])
                ys = yfs[:].rearrange("p f t -> p (f t)")
                kb.I('vector', 'tensor_tensor', out=ys, in0=pU[:].rearrange("p f t -> p (f t)"), in1=yfl[:].rearrange("p f t -> p (f t)"), op=ALU.add,
                     R=[d_pU, d_yfl], W=[d_yfs])
                kb.I('gpsimd', 'tensor_tensor', out=g1[:], in0=ys, in1=ys, op=ALU.mult, R=[d_yfs], W=[d_g1])
                kb.I('scalar', 'activation', out=g1[:], in_=g1[:], func=AF.Identity, scale=0.044715, bias=cst[:, 0:1], R=[d_g1, d_cst], W=[d_g1])
                kb.I('vector', 'tensor_tensor', out=g1[:], in0=g1[:], in1=ys, op=ALU.mult, R=[d_g1, d_yfs], W=[d_g1])
                kb.I('scalar', 'activation', out=g1[:], in_=g1[:], func=AF.Sigmoid, scale=1.5957691216, R=[d_g1], W=[d_g1])
                kb.I('vector', 'tensor_tensor', out=gTo[:], in0=g1[:], in1=ys, op=ALU.mult, R=[d_g1, d_yfs], W=[d_gTo])
                kb.dma(gT_d[ti], gTo[:], R=[d_gTo], W=[d_gT[ti]])
        kb.barrier()

    with ExitStack() as es:
        Wg = E.sb([128, 8, 2048], BF16, "sWg", es=es); d_Wg = Dep()
        with ExitStack() as es2:
            cs = Caster(E, es2)
            cs.cast3(Wg, d_Wg, A['s5_w_glu'], 0, 2048)
            kb.barrier()
        d_Wg.const = True
        gl = Ring(E, [128, 8, 128], BF16, 2, "sgl", es=es)
        xs = Ring(E, [128, D], F32, 2, "sx2", es=es)
        sg = Ring(E, [128, D], F32, 1, "ssg", es=es); yv = Ring(E, [128, D], F32, 1, "syv", es=es)
        pU2 = pU[:].rearrange("p f t -> p (f t)")
        for ti in range(2, NTT):
            seg, n_ = seqt[ti]
            g_, d_g = gl.next()
            kb.dma(g_[:].rearrange("p f t -> p (f t)"), gT_d[ti], R=[d_gT[ti]], W=[d_g])
            x, d_x = xs.next()
            load_tile(E, x, d_x, A, seg, n_)
            for half in range(2):
                for k in range(8):
                    kb.I('tensor', 'matmul', out=pY[:, half * 512:(half + 1) * 512], lhsT=g_[:, k, :], rhs=Wg[:, k, half * 512:(half + 1) * 512],
                         start=(k == 0), stop=(k == 7), R=[d_g, d_Wg], W=[d_pY])
                for k in range(8):
                    kb.I('tensor', 'matmul', out=pU2[:, half * 512:(half + 1) * 512], lhsT=g_[:, k, :],
                         rhs=Wg[:, k, 1024 + half * 512:1024 + (half + 1) * 512], start=(k == 0), stop=(k == 7), R=[d_g, d_Wg], W=[d_pU])
            s_, d_s = sg.next(); v_, d_v = yv.next()
            kb.I('scalar', 'activation', out=s_[:], in_=pU2, func=AF.Sigmoid, R=[d_pU], W=[d_s])
            kb.I('vector', 'tensor_tensor', out=v_[:], in0=pY[:], in1=s_[:], op=ALU.mult, R=[d_pY, d_s], W=[d_v])
            post.run(v_[:], d_v, x[:], d_x, GGl, dl[2])
            store_tile(E, x, d_x, A, seg, n_)
        kb.barrier()


def gla_consts(c=-1.0 / 16):
    s = np.arange(128)[:, None]; t = np.arange(128)[None, :]
    UF = (s <= t) * c; LF = (s > t) * c; UB = (s >= t) * c; LB = (s < t) * c
    mF = (t >= s) * 1.0; mB = (t <= s) * 1.0
    return np.stack([UF, LF, UB, LB, mF, mB]).astype(np.float32)


def ml_consts():
    return np.concatenate([gla_consts(-1.0), -np.ones((1, 128, 128), np.float32)]).astype(np.float32)


def hy_consts(L):
    T = L // 128; N = 2 * L; NFQ = T + 1
    pos = np.arange(L, dtype=np.float32); t = pos / np.float32(L)
    bands = np.linspace(1e-4, 15, 16, dtype=np.float32)
    ang = (2.0 * math.pi * t[:, None] * bands[None]).astype(np.float32)
    feats = np.concatenate([t[:, None], np.cos(ang), -np.sin(ang)], axis=-1).astype(np.float32)
    tn = (-t).reshape(T, 128).T.copy()
    tt = np.arange(L, dtype=np.int64)[:, None]; kk = np.arange(NFQ * 128, dtype=np.int64)[None, :]
    ph = ((tt * kk) % N).astype(np.float64) * (2 * math.pi / N)
    valid = (kk <= L)
    c1 = np.cos(ph) * valid; s1 = np.sin(ph) * valid

    def lay1(m):
        return m.reshape(T, 128, NFQ, 128).transpose(2, 1, 0, 3).astype(ml_dtypes.bfloat16).copy()
    alpha = np.where((kk == 0) | (kk == L), 1.0, 2.0) * valid / N
    c2 = (np.cos(ph) * alpha).T; s2 = (np.sin(ph) * alpha).T

    def lay2(m):
        return m.reshape(NFQ, 128, L).astype(ml_dtypes.bfloat16).copy()
    return dict(feT=feats.T.copy(), tn=tn.astype(np.float32), T1c=lay1(c1), T1s=lay1(s1), T2c=lay2(c2), T2s=lay2(s2))


def hy_delta_bc():
    lt = math.log(1e-2)
    deltas = np.abs(np.linspace(lt / 1.5, lt / 0.3, 1024, dtype=np.float32))
    return np.broadcast_to(deltas[None], (128, 1024)).astype(np.float32).copy()


def ml_bd_layout(wq, wk, wv):
    out = np.zeros((3, 16, 128, 128), np.float32)
    for j, w in enumerate((wq, wk, wv)):
        w = w.reshape(16, 32, 4, 4)
        for n in range(32):
            out[j, :, n * 4:(n + 1) * 4, n * 4:(n + 1) * 4] = w[:, n]
    return out


def s5_layouts(b_re, b_im, c_re, c_im, lam_re, lam_im, log_dt, dvec):
    out = {}
    for nm, b in (('s5_Bre', b_re), ('s5_Bim', b_im)):
        m = np.zeros((32, 128, 128), np.float32)
        for q in range(32):
            for two in range(2):
                g = 2 * q + two; gl = g % 8
                m[q, gl * 16:(gl + 1) * 16, two * 64:(two + 1) * 64] = b[g].T
        out[nm] = m
    for nm, c in (('s5_Cre', c_re), ('s5_Cim', c_im)):
        m = np.zeros((32, 128, 128), np.float32)
        for q in range(32):
            for two in range(2):
                g = 2 * q + two; gl = g % 8
                m[q, two * 64:(two + 1) * 64, gl * 16:(gl + 1) * 16] = c[g].T
        out[nm] = m

    def lay(a):
        return a.reshape(2, 32, 2, 64).transpose(0, 2, 3, 1).reshape(2, 128, 32).copy()
    out['s5_lreT'] = lay(lam_re); out['s5_limT'] = lay(lam_im)
    out['s5_ldtT'] = lay(np.broadcast_to(log_dt[:, :, None], (2, 64, 64)))
    out['s5_dcol'] = dvec.reshape(8, 128).T.copy()
    out['s5_J'] = np.eye(128, dtype=np.float32)[::-1].copy()
    return out


def _common_inputs(nc):
    IN = lambda n, s, dt=F32: nc.dram_tensor(n, list(s), dt, kind="ExternalInput").ap()
    A = dict(lat=IN("lat", [LAT, D]), cx=IN("cx", [CTXL, D]), c=IN("c", [D]), cc=IN("cc", [D]),
             mod_w=IN("mod_w", [D, 6 * D]), mod_b=IN("mod_b", [6 * D]), norm_g=IN("norm_g", [4, D]), ident=IN("ident", [128, 128]))
    return IN, A


def build_ffn(do_ctx=True):
    E = Env(); nc = E.nc
    IN, A = _common_inputs(nc)
    w_in = IN("w_in", [D, 2 * FH]); w_out = IN("w_out", [FH, D])
    lat_o = nc.dram_tensor("lat_o", [LAT, D], F32, kind="ExternalOutput").ap()
    cx_o = nc.dram_tensor("cx_o", [CTXL, D], F32, kind="ExternalOutput").ap() if do_ctx else None
    setup_consts(E, A['ident'])
    ffn_layer(E, A, w_in, w_out, lat_o, cx_o, do_ctx)
    E.kb.finish()
    return nc


def ffn_layer(E, A, w_in, w_out, lat_o, cx_o, do_ctx):
    Pl = alloc_mod(E); Pc = alloc_mod(E)
    wo = E.sb([128, 22, D], BF16, "wo")
    nt = NormT(E); post = PostRes(E)
    with ExitStack() as es:
        pcol = E.ps([128, 512], F32, 'pcol', es=es); pm = E.ps([128, 512], F32, 'pm', es=es); dpc = PDep(); dpm = PDep()
        (Gl, Sl, GGl, dl), (Gc, Sc, GGc, dc) = compute_mod2(E, Pl, Pc, A['c'], A['cc'], A['mod_w'], A['mod_b'], A['norm_g'], 1, es, pcol, dpc, pm, dpm)
        E.kb.barrier()
    with ExitStack() as es:
        wsc, d_wsc, wo, d_wo = ffn_weights_prep(E, wo, w_in, w_out, es)
        E.kb.barrier()
    groups = []
    lt = A['lat'].rearrange("(n p) d -> n p d", p=128); lo = lat_o.rearrange("(n p) d -> n p d", p=128)
    for g in range(8):
        groups.append(dict(tiles=[(lt[4 * g + j], lo[4 * g + j]) for j in range(4)], G=Gl, S=Sl, GG=GGl, dGS=dl[:2], dGG=dl[2]))
    if do_ctx:
        ct = A['cx'].rearrange("(n p) d -> n p d", p=128); co = cx_o.rearrange("(n p) d -> n p d", p=128)
        groups.append(dict(tiles=[(ct[j], co[j]) for j in range(2)], G=Gc, S=Sc, GG=GGc, dGS=dc[:2], dGG=dc[2]))
    ffn_run(E, groups, wsc, d_wsc, wo, d_wo, nt, post)
    E.kb.barrier()


GLA_IN = dict(gla_w_in=[D, 3104], gla_w_gate=[2, 16, 512], gla_b_gate=[2, 512], gla_norm_g=[256], gla_w_out=[D, D], gla_cm=[6, 128, 128])
HY_IN = dict(hy_w_in=[D, 3 * D], hy_conv_w=[3, 3 * D], hy_conv_b=[3 * D], hy_f_w1=[33, 64], hy_f_b1=[64], hy_f_w2=[64, 64], hy_f_b2=[64],
             hy_f_w3=[64, 2 * D], hy_f_freq=[64], hy_f_bias=[D], hy_w_out=[D, D], hy_delta_bc=[128, D])
ML_IN = dict(ml_w_in=[D, 4096], ml_conv_w=[3, 2048], ml_conv_b=[2048], ml_bd=[3, 16, 128, 128], ml_w_gates=[2, 6144, 8], ml_b_gates=[2, 8],
             ml_norm_g=[2048], ml_skip=[2048], ml_w_out=[2048, D], ml_cm=[7, 128, 128])
S5_IN = dict(s5_Bre=[32, 128, 128], s5_Bim=[32, 128, 128], s5_Cre=[32, 128, 128], s5_Cim=[32, 128, 128], s5_lreT=[2, 128, 32],
             s5_limT=[2, 128, 32], s5_ldtT=[2, 128, 32], s5_dcol=[128, 8], s5_J=[128, 128], s5_w_glu=[D, 2 * D])


def _hy_table_inputs(IN, A):
    for tag, L in (("l", LAT), ("c", CTXL)):
        T = L // 128; NFQ = T + 1
        A[f'feT_{tag}'] = IN(f"feT_{tag}", [33, L]); A[f'tn_{tag}'] = IN(f"tn_{tag}", [128, T])
        A[f'T1c_{tag}'] = IN(f"T1c_{tag}", [NFQ, 128, T, 128], BF16); A[f'T1s_{tag}'] = IN(f"T1s_{tag}", [NFQ, 128, T, 128], BF16)
        A[f'T2c_{tag}'] = IN(f"T2c_{tag}", [NFQ, 128, L], BF16); A[f'T2s_{tag}'] = IN(f"T2s_{tag}", [NFQ, 128, L], BF16)


def build_mixer(kind):
    E = Env(); nc = E.nc
    IN, A = _common_inputs(nc)
    spec = (GLA_IN, HY_IN, ML_IN, S5_IN)[kind]
    for k, shp in spec.items():
        A[k] = IN(k, shp)
    if kind == 1:
        _hy_table_inputs(IN, A)
    A['lat_o'] = nc.dram_tensor("lat_o", [LAT, D], F32, kind="ExternalOutput").ap()
    if kind != 3:
        A['cx_o'] = nc.dram_tensor("cx_o", [CTXL, D], F32, kind="ExternalOutput").ap()
    setup_consts(E, A['ident'])
    (gla_layer, hyena_layer, mlstm_layer, s5_layer)[kind](E, A)
    E.kb.finish()
    return nc


def _f32(a):
    return np.ascontiguousarray(a, dtype=np.float32)


def mixer_host_inputs(kind, inp):
    if kind == 0:
        d = {k: _f32(inp[k][0]) for k in ('gla_w_in', 'gla_w_gate', 'gla_b_gate', 'gla_norm_g', 'gla_w_out')}
        d['gla_cm'] = gla_consts()
        return d
    if kind == 1:
        d = {k: _f32(inp[k][0]) for k in inp if k.startswith('hy_')}
        d['hy_delta_bc'] = hy_delta_bc()
        for tag, L in (("l", LAT), ("c", CTXL)):
            for k, v in hy_consts(L).items():
                d[f'{k}_{tag}'] = v
        return d
    if kind == 2:
        d = {k: _f32(inp[k][0]) for k in ('ml_w_in', 'ml_conv_w', 'ml_conv_b', 'ml_w_gates', 'ml_b_gates', 'ml_norm_g', 'ml_skip', 'ml_w_out')}
        d['ml_cm'] = ml_consts()
        d['ml_bd'] = ml_bd_layout(_f32(inp['ml_w_q'][0]), _f32(inp['ml_w_k'][0]), _f32(inp['ml_w_v'][0]))
        return d
    d = s5_layouts(_f32(inp['s5_b_re'][0]), _f32(inp['s5_b_im'][0]), _f32(inp['s5_c_re'][0]), _f32(inp['s5_c_im'][0]),
                   _f32(inp['s5_lam_re'][0]), _f32(inp['s5_lam_im'][0]), _f32(inp['s5_log_dt'][0]), _f32(inp['s5_d'][0]))
    d['s5_w_glu'] = _f32(inp['s5_w_glu'][0])
    return d


def kernel_unfused(**inp):
    NC = 8
    x = _f32(inp['x']); c = _f32(inp['c']); ctx = _f32(inp['ctx']); cc = _f32(inp['c_ctx'])
    ident = np.eye(128, dtype=np.float32)
    lat = [x[b] for b in range(NC)]
    cx = [ctx[b] for b in range(NC)]
    ffn_prog = {}
    for i in range(4):
        last = i == 3
        common = dict(mod_w=_f32(inp['mod_w'][i]), mod_b=_f32(inp['mod_b'][i]), norm_g=_f32(inp['norm_g'][i]), cc=cc, ident=ident)
        nc = build_mixer(i)
        extra = mixer_host_inputs(i, inp)
        maps = [dict(common, lat=lat[b], cx=cx[b], c=c[b], **extra) for b in range(NC)]
        res = run_bass_kernel_spmd(nc, maps, core_ids=list(range(NC)))
        lat = [_f32(res.results[b]['lat_o']) for b in range(NC)]
        if not last:
            cx = [_f32(res.results[b]['cx_o']) for b in range(NC)]
        del maps, extra
        key = not last
        if key not in ffn_prog:
            ffn_prog[key] = build_ffn(do_ctx=key)
        maps = [dict(common, lat=lat[b], cx=cx[b], c=c[b], w_in=_f32(inp['ffn_w_in'][i]), w_out=_f32(inp['ffn_w_out'][i])) for b in range(NC)]
        res = run_bass_kernel_spmd(ffn_prog[key], maps, core_ids=list(range(NC)))
        lat = [_f32(res.results[b]['lat_o']) for b in range(NC)]
        if not last:
            cx = [_f32(res.results[b]['cx_o']) for b in range(NC)]
        del maps
    return np.stack(lat, axis=0).astype(np.float32)


def build_fused():
    E = Env(); nc = E.nc
    IN = lambda n, s, dt=F32: nc.dram_tensor(n, list(s), dt, kind="ExternalInput").ap()
    x = IN("x", [LAT, D]); ctx = IN("ctx", [CTXL, D]); c = IN("c", [D]); cc = IN("cc", [D])
    mod_w = IN("mod_w", [4, D, 6 * D]); mod_b = IN("mod_b", [4, 6 * D]); norm_g = IN("norm_g", [4, 4, D])
    ffn_w_in = IN("ffn_w_in", [4, D, 2 * FH]); ffn_w_out = IN("ffn_w_out", [4, FH, D]); ident = IN("ident", [128, 128])
    MIX = {}
    for kind, spec in enumerate((GLA_IN, HY_IN, ML_IN, S5_IN)):
        for k, shp in spec.items():
            MIX[k] = IN(k, shp)
    _hy_table_inputs(IN, MIX)
    out = nc.dram_tensor("out", [LAT, D], F32, kind="ExternalOutput").ap()
    setup_consts(E, ident)
    glob_es = E.es
    cur_lat, cur_cx = x, ctx
    for i in range(4):
        last = i == 3
        base = dict(c=c, cc=cc, mod_w=mod_w[i], mod_b=mod_b[i], norm_g=norm_g[i], ident=ident)
        nl = E.dram(f"lat_m{i}", [LAT, D]); ncx = E.dram(f"cx_m{i}", [CTXL, D]) if not last else None
        A = dict(base, lat=cur_lat, cx=cur_cx, lat_o=nl, **MIX)
        if not last:
            A['cx_o'] = ncx
        with ExitStack() as les:
            E.es = les
            (gla_layer, hyena_layer, mlstm_layer, s5_layer)[i](E, A)
            E.kb.barrier()
        E.es = glob_es
        cur_lat = nl
        if not last:
            cur_cx = ncx
        nl = out if last else E.dram(f"lat_f{i}", [LAT, D])
        ncx = E.dram(f"cx_f{i}", [CTXL, D]) if not last else None
        A = dict(base, lat=cur_lat, cx=cur_cx)
        with ExitStack() as les:
            E.es = les
            ffn_layer(E, A, ffn_w_in[i], ffn_w_out[i], nl, ncx, not last)
            E.kb.barrier()
        E.es = glob_es
        cur_lat = nl
        if not last:
            cur_cx = ncx
    E.kb.finish()
    return nc


def kernel(**inp):
    NC = 8
    x = _f32(inp['x']); c = _f32(inp['c']); ctx = _f32(inp['ctx']); cc = _f32(inp['c_ctx'])
    shared = dict(cc=cc, ident=np.eye(128, dtype=np.float32), mod_w=_f32(inp['mod_w']), mod_b=_f32(inp['mod_b']), norm_g=_f32(inp['norm_g']),
                  ffn_w_in=_f32(inp['ffn_w_in']), ffn_w_out=_f32(inp['ffn_w_out']))
    for kind in range(4):
        shared.update(mixer_host_inputs(kind, inp))
    nc = build_fused()
    maps = [dict(shared, x=x[b], ctx=ctx[b], c=c[b]) for b in range(NC)]
    res = run_bass_kernel_spmd(nc, maps, core_ids=list(range(NC)))
    return np.stack([np.asarray(res.results[b]['out'], dtype=np.float32) for b in range(NC)], axis=0)
```

```python
import ml_dtypes


import numpy as np
import math
import concourse.bass as bass
import concourse.mybir as mybir
from concourse.bass_utils import run_bass_kernel_spmd

F32 = mybir.dt.float32
BF16 = mybir.dt.bfloat16
AF = mybir.ActivationFunctionType
ALU = mybir.AluOpType
AX = mybir.AxisListType
ENGS = ('tensor', 'vector', 'scalar', 'gpsimd', 'sync')
SEM_ROT = 30000
STORE_Q = 'gpsimd'


class Dep:
    __slots__ = ('w', 'r', 'const', 'excl')

    def __init__(self, const=False, excl=False):
        self.w = None
        self.r = {}
        self.const = const
        self.excl = excl


def PDep():
    return Dep(excl=True)


class KB:
    def __init__(self, nc, n_dma_sems=24):
        self.nc = nc
        self.prog = {e: [] for e in ENGS}
        self.cnt = {e: 0 for e in ENGS}
        self.sem = {e: nc.alloc_semaphore(name=f"s_{e}_0") for e in ENGS}
        self.semgen = {e: 0 for e in ENGS}
        self.seen = {e: {} for e in ENGS}
        self.dsem = [nc.alloc_semaphore(name=f"s_dma_{i}") for i in range(n_dma_sems)]
        self.dcnt = [0] * n_dma_sems
        self.drr = 0
        self.ninst = 0
        self.last_ev = {}

    def _wait(self, eng, ev):
        sem, val = ev
        key = id(sem)
        if self.seen[eng].get(key, 0) >= val:
            return
        self.seen[eng][key] = val
        self.prog[eng].append(lambda e, sem=sem, val=val: e.wait_ge(sem, val))

    def _deps(self, eng, R, W):
        evs = []
        for d in R:
            if d.w is not None:
                evs.append(d.w)
        for d in W:
            if d.w is not None:
                evs.append(d.w)
            evs.extend(d.r.values())
        for ev in evs:
            if eng == 'tensor' and ev[2] == 'tensor':
                continue
            self._wait(eng, ev[:2])

    def _record(self, ev, R, W):
        for d in W:
            d.w = ev
            d.r = {}
        for d in R:
            if not d.const:
                d.r[id(ev[0])] = ev

    def op(self, eng, fn, R=(), W=()):
        ex = [d for d in R if d.excl]
        if ex:
            R = [d for d in R if not d.excl]
            W = list(W) + ex
        self._deps(eng, R, W)
        if self.cnt[eng] >= SEM_ROT:
            self.semgen[eng] += 1
            self.sem[eng] = self.nc.alloc_semaphore(name=f"s_{eng}_{self.semgen[eng]}")
            self.cnt[eng] = 0
        self.cnt[eng] += 1
        sem, val = self.sem[eng], self.cnt[eng]
        self.prog[eng].append(lambda e, fn=fn, sem=sem: fn(e).then_inc(sem, 1))
        self._record((sem, val, eng), R, W)
        self.last_ev[eng] = (sem, val)
        self.ninst += 1

    def I(self, eng, name, *args, R=(), W=(), **kw):
        self.op(eng, lambda e, name=name, args=args, kw=kw: getattr(e, name)(*args, **kw), R=R, W=W)

    def dma(self, out, in_, R=(), W=(), eng=None, **kw):
        if eng is None:
            eng = STORE_Q if (str(out.space) == 'DRAM' and str(in_.space) != 'DRAM') else 'sync'
        self._deps(eng, R, W)
        i = self.drr
        self.drr = (self.drr + 1) % len(self.dsem)
        sem = self.dsem[i]
        if self.dcnt[i] > 0:
            self._wait(eng, (sem, self.dcnt[i]))
        self.dcnt[i] += 16
        val = self.dcnt[i]
        self.prog[eng].append(lambda e, sem=sem, out=out, in_=in_, kw=kw: e.dma_start(out=out, in_=in_, **kw).then_inc(sem, 16))
        self._record((sem, val, 'dma'), R, W)
        self.ninst += 1

    def barrier(self):
        for e in ENGS:
            for e2 in ENGS:
                if e2 != e and e2 in self.last_ev:
                    self._wait(e, self.last_ev[e2])
            for i, s in enumerate(self.dsem):
                if self.dcnt[i] > 0:
                    self._wait(e, (s, self.dcnt[i]))

    def finish(self):
        for e2 in ENGS:
            if e2 != 'sync' and e2 in self.last_ev:
                self._wait('sync', self.last_ev[e2])
        for i, s in enumerate(self.dsem):
            if self.dcnt[i] > 0:
                self._wait('sync', (s, self.dcnt[i]))
        with self.nc.Block() as block:
            for e in ENGS:
                lst = self.prog[e]
                if not lst:
                    continue
                getattr(block, e)(lambda engobj, lst=lst: [f(engobj) for f in lst])


from contextlib import ExitStack

D = 1024
LAT = 4096
CTXL = 256
FH = 2816
EPS = 1e-6


class Env:
    def __init__(self, name="k"):
        self.nc = bass.Bass("TRN2", target_bir_lowering=False)
        self.kb = KB(self.nc)
        self.es = ExitStack()
        self.n = 0

    def sb(self, shape, dt=F32, name="t", es=None):
        self.n += 1
        return (es or self.es).enter_context(self.nc.sbuf_tensor(f"{name}_{self.n}", list(shape), dt))

    def ps(self, shape, dt=F32, name="p", es=None):
        self.n += 1
        return (es or self.es).enter_context(self.nc.psum_tensor(f"{name}_{self.n}", list(shape), dt))

    def dram(self, name, shape, dt=F32, kind="Internal"):
        return self.nc.dram_tensor(name, list(shape), dt, kind=kind).ap()


def setup_consts(E, ident_dram):
    kb = E.kb
    E.idf = E.sb([128, 128], F32, "idf"); E.d_idf = Dep(const=True)
    E.idb = E.sb([128, 128], BF16, "idb"); E.d_idb = Dep(const=True)
    E.ones = E.sb([128, 512], F32, "ones"); E.d_ones = Dep(const=True)
    E.onesb = E.sb([128, 512], BF16, "onesb"); E.d_onesb = Dep(const=True)
    E.epsc = E.sb([128, 1], F32, "epsc"); E.d_eps = Dep(const=True)
    kb.dma(E.idf[:], ident_dram, W=[E.d_idf])
    kb.I('vector', 'tensor_copy', out=E.idb[:], in_=E.idf[:], R=[E.d_idf], W=[E.d_idb])
    kb.I('vector', 'memset', E.ones[:], 1.0, W=[E.d_ones])
    kb.I('vector', 'memset', E.onesb[:], 1.0, W=[E.d_onesb])
    kb.I('vector', 'memset', E.epsc[:], EPS, W=[E.d_eps])


def alloc_mod(E):
    P = {}
    for nm, shp in (("sc", [128, 8]), ("gcol", [128, 8]), ("bcol", [128, 2, 8]), ("Gcol", [128, 8]), ("Scol", [128, 8]),
                    ("GG", [128, D])):
        P[nm] = E.sb(shp, F32, nm)
    return P


def compute_mod(E, P, cvec, mod_w, mod_b, norm_g, h, es, pcol, d_pcol, pm, d_pm):
    kb, nc = E.kb, E.nc
    sc = P["sc"]; d_sc = Dep()
    kb.dma(sc[:], cvec.rearrange("(k p) -> p k", p=128), W=[d_sc], allow_slow_non_contiguous=True)
    kb.I('scalar', 'activation', out=sc[:], in_=sc[:], func=AF.Silu, R=[d_sc], W=[d_sc])
    gcol = P["gcol"]; d_gcol = Dep()
    bcol = P["bcol"]; d_bcol = Dep()
    kb.dma(gcol[:], norm_g[2 * h].rearrange("(k p) -> p k", p=128), W=[d_gcol], allow_slow_non_contiguous=True)
    kb.dma(bcol[:], mod_b[3 * h * D:(3 * h + 2) * D].rearrange("(j k p) -> p j k", p=128, k=8), W=[d_bcol],
           allow_slow_non_contiguous=True)
    Gcol = P["Gcol"]; Scol = P["Scol"]; d_G = Dep(); d_S = Dep()
    mwv = mod_w.rearrange("(k p) c -> p k c", p=128)
    wt = [E.sb([128, 8, 256], F32, "mwt", es=es) for _ in range(2)]
    d_wt = [Dep(), Dep()]
    nld = 0
    for j in range(2):
        for half in range(4):
            c0 = (3 * h + j) * D + half * 256
            b = nld % 2; nld += 1
            kb.dma(wt[b][:], mwv[:, :, c0:c0 + 256], W=[d_wt[b]])
            for oc in range(2):
                col = j * 8 + half * 2 + oc
                for k in range(8):
                    kb.I('tensor', 'matmul', out=pcol[:, col:col + 1], lhsT=wt[b][:, k, oc * 128:(oc + 1) * 128], rhs=sc[:, k:k + 1],
                        start=(k == 0), stop=(k == 7), R=[d_wt[b], d_sc], W=[d_pcol])
    kb.I('vector', 'tensor_tensor', out=Scol[:], in0=pcol[:, 0:8], in1=bcol[:, 0, :], op=ALU.add, R=[d_pcol, d_bcol], W=[d_S])
    kb.I('vector', 'scalar_tensor_tensor', out=Gcol[:], in0=pcol[:, 8:16], scalar=1.0, in1=bcol[:, 1, :],
                                                     op0=ALU.add, op1=ALU.add, R=[d_pcol, d_bcol], W=[d_G])
    kb.I('vector', 'tensor_tensor', out=Gcol[:], in0=Gcol[:], in1=gcol[:], op=ALU.mult, R=[d_G, d_gcol], W=[d_G])
    rep = E.sb([128, 8, 128], F32, "rep", es=es); d_rep = Dep()
    for k in range(8):
        kb.I('vector', 'tensor_scalar_mul', out=rep[:, k, :], in0=E.ones[:, 0:128], scalar1=sc[:, k:k + 1], R=[d_sc, E.d_ones], W=[d_rep])
    rows = E.sb([1, 2, D], F32, "rows", es=es); d_rows = Dep()
    kb.dma(rows[0:1, 0, :], mod_b[(3 * h + 2) * D:(3 * h + 3) * D].rearrange("(o c) -> o c", o=1), W=[d_rows])
    kb.dma(rows[0:1, 1, :], norm_g[2 * h + 1].rearrange("(o c) -> o c", o=1), W=[d_rows])
    GG = P["GG"]; d_GG = Dep()
    gb = E.sb([128, D], F32, "gbbc", es=es); d_gb = Dep()
    for half in range(2):
        kb.I('tensor', 'matmul', out=pm[:], lhsT=E.ones[0:1, 0:128],
                                                      rhs=rows[0:1, 1, half * 512:(half + 1) * 512], start=True, stop=True, R=[d_rows, E.d_ones], W=[d_pm])
        kb.I('vector', 'tensor_copy', out=gb[:, half * 512:(half + 1) * 512], in_=pm[:], R=[d_pm], W=[d_gb])
    for q in range(4):
        c0 = (3 * h + 2) * D + q * 256
        b = nld % 2; nld += 1
        kb.dma(wt[b][:], mwv[:, :, c0:c0 + 256], W=[d_wt[b]])
        for k in range(8):
            kb.I('tensor', 'matmul', out=pm[:, 0:256], lhsT=rep[:, k, :], rhs=wt[b][:, k, :],
                                                         start=(k == 0), stop=False, R=[d_wt[b], d_rep], W=[d_pm])
        kb.I('tensor', 'matmul', out=pm[:, 0:256], lhsT=E.ones[0:1, 0:128],
                                                rhs=rows[0:1, 0, q * 256:(q + 1) * 256], start=False, stop=True, R=[d_rows, E.d_ones], W=[d_pm])
        kb.I('vector', 'tensor_tensor', out=GG[:, q * 256:(q + 1) * 256], in0=pm[:, 0:256],
                                                       in1=gb[:, q * 256:(q + 1) * 256], op=ALU.mult, R=[d_pm, d_gb], W=[d_GG])
    return Gcol, Scol, GG, (d_G, d_S, d_GG)


class NormT:
    def __init__(self, E, nbuf=2):
        self.E = E
        self.sq = [E.sb([128, D], F32, "sqj") for _ in range(nbuf)]
        self.ss = [E.sb([128, 2], F32, "ss") for _ in range(nbuf)]
        self.xb = [E.sb([128, D], BF16, "xb") for _ in range(nbuf)]
        self.pT = [E.ps([128, 8, 128], BF16, "pT") for _ in range(1)]
        self.d_sq = [Dep() for _ in range(nbuf)]
        self.d_ss = [Dep() for _ in range(nbuf)]
        self.d_xb = [Dep() for _ in range(nbuf)]
        self.d_pT = [PDep()]
        self.i = 0

    def run(self, xt, d_xt, Gcol, Scol, dGS, out_fn, d_out):
        E, kb = self.E, self.E.kb
        b = self.i % len(self.sq); self.i += 1
        sq, ss, xb, pT = self.sq[b], self.ss[b], self.xb[b], self.pT[0]
        kb.I('scalar', 'activation', out=sq[:], in_=xt, func=AF.Square, accum_out=ss[:, 0:1], R=[d_xt], W=[self.d_sq[b], self.d_ss[b]])
        kb.I('scalar', 'activation', out=ss[:, 1:2], in_=ss[:, 0:1], func=AF.Sqrt, bias=E.epsc[:, 0:1], scale=1.0 / D, R=[self.d_ss[b], E.d_eps], W=[self.d_ss[b]])
        kb.I('vector', 'reciprocal', out=ss[:, 1:2], in_=ss[:, 1:2], R=[self.d_ss[b]], W=[self.d_ss[b]])
        kb.I('vector', 'tensor_scalar_mul', out=xb[:], in0=xt, scalar1=ss[:, 1:2], R=[d_xt, self.d_ss[b]], W=[self.d_xb[b]])
        for k in range(8):
            kb.I('tensor', 'transpose', out=pT[:, k, :], in_=xb[:, k * 128:(k + 1) * 128], identity=E.idb[:], R=[self.d_xb[b], E.d_idb], W=[self.d_pT[0]])
        for k in range(8):
            kb.I('scalar', 'activation', out=out_fn(k), in_=pT[:, k, :], func=AF.Identity,
                                                        bias=Scol[:, k:k + 1], scale=Gcol[:, k:k + 1], R=[self.d_pT[0]] + list(dGS), W=[d_out])


class PostRes:
    def __init__(self, E, nbuf=2):
        self.E = E
        self.sq = [E.sb([128, D], F32, "psq") for _ in range(nbuf)]
        self.ss = [E.sb([128, 2], F32, "pss") for _ in range(nbuf)]
        self.d_sq = [Dep() for _ in range(nbuf)]
        self.d_ss = [Dep() for _ in range(nbuf)]
        self.i = 0

    def run(self, y, d_y, xt, d_xt, GG, d_GG):
        E, kb = self.E, self.E.kb
        b = self.i % len(self.sq); self.i += 1
        sq, ss = self.sq[b], self.ss[b]
        kb.I('scalar', 'activation', out=sq[:], in_=y, func=AF.Square, accum_out=ss[:, 0:1], R=[d_y], W=[self.d_sq[b], self.d_ss[b]])
        kb.I('scalar', 'activation', out=ss[:, 1:2], in_=ss[:, 0:1], func=AF.Sqrt, bias=E.epsc[:, 0:1], scale=1.0 / D, R=[self.d_ss[b], E.d_eps], W=[self.d_ss[b]])
        kb.I('vector', 'reciprocal', out=ss[:, 1:2], in_=ss[:, 1:2], R=[self.d_ss[b]], W=[self.d_ss[b]])
        kb.I('vector', 'scalar_tensor_tensor', out=sq[:], in0=y, scalar=ss[:, 1:2], in1=GG[:],
                                                         op0=ALU.mult, op1=ALU.mult, R=[d_y, self.d_ss[b], d_GG], W=[self.d_sq[b]])
        kb.I('gpsimd', 'tensor_tensor', out=xt, in0=xt, in1=sq[:], op=ALU.add, R=[self.d_sq[b], d_xt], W=[d_xt])


class Ring:
    def __init__(self, E, shape, dt, n=1, name="r", psum=False, es=None):
        mk = E.ps if psum else E.sb
        self.t = [mk(shape, dt, name, es=es) for _ in range(n)]
        self.d = [Dep() for _ in range(n)]
        self.i = -1

    def next(self):
        self.i = (self.i + 1) % len(self.t)
        return self.t[self.i], self.d[self.i]

    def cur(self):
        return self.t[self.i], self.d[self.i]


def compute_mod2(E, Pl, Pc, cl, cc, mod_w, mod_b, norm_g, h, es, pcol, d_pcol, pm, d_pm):
    kb = E.kb
    PP = (Pl, Pc)
    sc2 = E.sb([128, 8, 2], F32, "sc2", es=es); d_sc = Dep()
    kb.dma(sc2[:, :, 0], cl.rearrange("(k p) -> p k", p=128), W=[d_sc], allow_slow_non_contiguous=True)
    kb.dma(sc2[:, :, 1], cc.rearrange("(k p) -> p k", p=128), W=[d_sc], allow_slow_non_contiguous=True)
    kb.I('scalar', 'activation', out=sc2[:], in_=sc2[:], func=AF.Silu, R=[d_sc], W=[d_sc])
    gcol = Pl["gcol"]; d_gcol = Dep(); bcol = Pl["bcol"]; d_bcol = Dep()
    kb.dma(gcol[:], norm_g[2 * h].rearrange("(k p) -> p k", p=128), W=[d_gcol], allow_slow_non_contiguous=True)
    kb.dma(bcol[:], mod_b[3 * h * D:(3 * h + 2) * D].rearrange("(j k p) -> p j k", p=128, k=8), W=[d_bcol],
           allow_slow_non_contiguous=True)
    d_G = [Dep(), Dep()]; d_S = [Dep(), Dep()]; d_GG = [Dep(), Dep()]
    mwv = mod_w.rearrange("(k p) c -> p k c", p=128)
    wt = [E.sb([128, 8, 256], F32, "mwt", es=es) for _ in range(2)]
    d_wt = [Dep(), Dep()]
    nld = 0
    for j in range(2):
        for half in range(4):
            c0 = (3 * h + j) * D + half * 256
            b = nld % 2; nld += 1
            kb.dma(wt[b][:], mwv[:, :, c0:c0 + 256], W=[d_wt[b]])
            for oc in range(2):
                col = j * 8 + half * 2 + oc
                for k in range(8):
                    kb.I('tensor', 'matmul', out=pcol[:, 2 * col:2 * col + 2], lhsT=wt[b][:, k, oc * 128:(oc + 1) * 128], rhs=sc2[:, k, :],
                         start=(k == 0), stop=(k == 7), R=[d_wt[b], d_sc], W=[d_pcol])
    pc3 = pcol[:, 0:32].rearrange("p (c x) -> p c x", x=2)
    for x in range(2):
        P = PP[x]
        kb.I('vector', 'tensor_tensor', out=P["Scol"][:], in0=pc3[:, 0:8, x], in1=bcol[:, 0, :], op=ALU.add, R=[d_pcol, d_bcol], W=[d_S[x]])
        kb.I('vector', 'scalar_tensor_tensor', out=P["Gcol"][:], in0=pc3[:, 8:16, x], scalar=1.0, in1=bcol[:, 1, :], op0=ALU.add, op1=ALU.add,
             R=[d_pcol, d_bcol], W=[d_G[x]])
        kb.I('vector', 'tensor_tensor', out=P["Gcol"][:], in0=P["Gcol"][:], in1=gcol[:], op=ALU.mult, R=[d_G[x], d_gcol], W=[d_G[x]])
    rep = [E.sb([128, 8, 128], F32, "rep", es=es) for _ in range(2)]; d_rep = Dep()
    for x in range(2):
        for k in range(8):
            kb.I('vector', 'tensor_scalar_mul', out=rep[x][:, k, :], in0=E.ones[:, 0:128], scalar1=sc2[:, k, x:x + 1], R=[d_sc, E.d_ones], W=[d_rep])
    rows = E.sb([1, 2, D], F32, "rows", es=es); d_rows = Dep()
    kb.dma(rows[0:1, 0, :], mod_b[(3 * h + 2) * D:(3 * h + 3) * D].rearrange("(o c) -> o c", o=1), W=[d_rows])
    kb.dma(rows[0:1, 1, :], norm_g[2 * h + 1].rearrange("(o c) -> o c", o=1), W=[d_rows])
    gb = E.sb([128, D], F32, "gbbc", es=es); d_gb = Dep()
    for half in range(2):
        kb.I('tensor', 'matmul', out=pm[:], lhsT=E.ones[0:1, 0:128], rhs=rows[0:1, 1, half * 512:(half + 1) * 512], start=True, stop=True,
             R=[d_rows, E.d_ones], W=[d_pm])
        kb.I('vector', 'tensor_copy', out=gb[:, half * 512:(half + 1) * 512], in_=pm[:], R=[d_pm], W=[d_gb])
    pmx = (pm, pcol); d_pmx = (d_pm, d_pcol)
    for q in range(4):
        c0 = (3 * h + 2) * D + q * 256
        b = nld % 2; nld += 1
        kb.dma(wt[b][:], mwv[:, :, c0:c0 + 256], W=[d_wt[b]])
        for x in range(2):
            for k in range(8):
                kb.I('tensor', 'matmul', out=pmx[x][:, 0:256], lhsT=rep[x][:, k, :], rhs=wt[b][:, k, :], start=(k == 0), stop=False,
                     R=[d_wt[b], d_rep], W=[d_pmx[x]])
            kb.I('tensor', 'matmul', out=pmx[x][:, 0:256], lhsT=E.ones[0:1, 0:128], rhs=rows[0:1, 0, q * 256:(q + 1) * 256], start=False, stop=True,
                 R=[d_rows, E.d_ones], W=[d_pmx[x]])
            kb.I('vector', 'tensor_tensor', out=PP[x]["GG"][:, q * 256:(q + 1) * 256], in0=pmx[x][:, 0:256], in1=gb[:, q * 256:(q + 1) * 256],
                 op=ALU.mult, R=[d_pmx[x], d_gb], W=[d_GG[x]])
    return ((Pl["Gcol"], Pl["Scol"], Pl["GG"], (d_G[0], d_S[0], d_GG[0])), (Pc["Gcol"], Pc["Scol"], Pc["GG"], (d_G[1], d_S[1], d_GG[1])))


def ffn_weights_prep(E, wo, w_in, w_out, es):
    kb = E.kb
    E.n += 1
    wsc = E.dram(f"wsc_{E.n}", [11, 128, 8 * 2 * 256], BF16)
    d_wo = Dep()
    stg = [E.sb([128, 2048], F32, "stg", es=es) for _ in range(2)]; d_stg = [Dep(), Dep()]
    stb = [E.sb([128, 8, 2, 256], BF16, "stb", es=es) for _ in range(2)]; d_stb = [Dep(), Dep()]
    wov = w_out.rearrange("(f p) c -> p f c", p=128)
    n = 0
    for f2 in range(11):
        b = n % 2; n += 1
        kb.dma(stg[b][:].rearrange("p (f c) -> p f c", f=2), wov[:, 2 * f2:2 * f2 + 2, :], W=[d_stg[b]])
        kb.I('gpsimd', 'tensor_copy', out=wo[:, 2 * f2:2 * f2 + 2, :],
                                                           in_=stg[b][:].rearrange("p (f c) -> p f c", f=2), R=[d_stg[b]], W=[d_wo])
    wiv = w_in.rearrange("(k p) c -> p k c", p=128)
    d_wsc = Dep()
    for s in range(11):
        bb = s % 2
        for g in range(2):
            b = n % 2; n += 1
            c0 = g * FH + s * 256
            kb.dma(stg[b][:].rearrange("p (k c) -> p k c", k=8), wiv[:, :, c0:c0 + 256], W=[d_stg[b]])
            kb.I('gpsimd' if g == 0 else 'vector', 'tensor_copy', out=stb[bb][:, :, g, :], in_=stg[b][:].rearrange("p (k c) -> p k c", k=8), R=[d_stg[b]], W=[d_stb[bb]])
        kb.dma(wsc[s], stb[bb][:].rearrange("p k g c -> p (k g c)"), R=[d_stb[bb]], W=[d_wsc])
    return wsc, d_wsc, wo, d_wo


def ffn_run(E, groups, wsc, d_wsc, wo, d_wo, nt, post):
    kb = E.kb
    NX = 8
    xs = [E.sb([128, D], F32, "x") for _ in range(NX)]; d_xs = [Dep() for _ in range(NX)]
    hT = [E.sb([128, 8, 512], BF16, "hT") for _ in range(2)]; d_hT = [Dep(), Dep()]
    wbuf = [E.sb([128, 8, 2, 256], BF16, "wi") for _ in range(4)]; d_wb = [Dep() for _ in range(4)]
    act = E.sb([128, 22, 512], BF16, "act"); d_act = Dep()
    sg = [E.sb([128, 512], F32, "sg") for _ in range(2)]; d_sg = [Dep(), Dep()]
    pg = [E.ps([128, 512], F32, "pg") for _ in range(2)]; d_pg = [PDep(), PDep()]
    pu = [E.ps([128, 512], F32, "pu") for _ in range(2)]; d_pu = [PDep(), PDep()]
    py = E.ps([128, D], F32, "py"); d_py = PDep()
    xi = 0; wi = 0
    for gi, g in enumerate(groups):
        tiles = g['tiles']
        ntk = len(tiles) * 128
        hb = gi % 2
        slots = []
        for ti, (src, dst) in enumerate(tiles):
            s = xi % NX; xi += 1
            slots.append(s)
            kb.dma(xs[s][:], src, W=[d_xs[s]])
            nt.run(xs[s][:], d_xs[s], g['G'], g['S'], g['dGS'],
                   lambda k, ti=ti, hb=hb: hT[hb][:, k, ti * 128:(ti + 1) * 128], d_hT[hb])
        for s in range(11):
            wb = wi % 4; wi += 1
            kb.dma(wbuf[wb][:].rearrange("p k g c -> p (k g c)"), wsc[s], R=[d_wsc], W=[d_wb[wb]])
            for fo in range(2):
                f = 2 * s + fo
                pb = f % 2
                for k in range(8):
                    kb.I('tensor', 'matmul', out=pg[pb][:, 0:ntk], lhsT=wbuf[wb][:, k, 0, fo * 128:(fo + 1) * 128], rhs=hT[hb][:, k, 0:ntk],
                        start=(k == 0), stop=(k == 7), R=[d_wb[wb], d_hT[hb]], W=[d_pg[pb]])
                for k in range(8):
                    kb.I('tensor', 'matmul', out=pu[pb][:, 0:ntk], lhsT=wbuf[wb][:, k, 1, fo * 128:(fo + 1) * 128], rhs=hT[hb][:, k, 0:ntk],
                        start=(k == 0), stop=(k == 7), R=[d_wb[wb], d_hT[hb]], W=[d_pu[pb]])
                kb.I('scalar', 'activation', out=sg[pb][:, 0:ntk], in_=pg[pb][:, 0:ntk], func=AF.Silu, R=[d_pg[pb]], W=[d_sg[pb]])
                kb.I('vector', 'tensor_tensor', out=act[:, f, 0:ntk], in0=pu[pb][:, 0:ntk],
                                                                       in1=sg[pb][:, 0:ntk], op=ALU.mult, R=[d_pu[pb], d_sg[pb]], W=[d_act])
        for ti, (src, dst) in enumerate(tiles):
            s = slots[ti]
            for half in range(2):
                for f in range(22):
                    kb.I('tensor', 'matmul', out=py[:, half * 512:(half + 1) * 512], lhsT=act[:, f, ti * 128:(ti + 1) * 128],
                        rhs=wo[:, f, half * 512:(half + 1) * 512], start=(f == 0), stop=(f == 21), R=[d_act, d_wo], W=[d_py])
            post.run(py[:], d_py, xs[s][:], d_xs[s], g['GG'], g['dGG'])
            kb.dma(dst, xs[s][:], R=[d_xs[s]])


class Caster:
    def __init__(self, E, es):
        self.E = E
        self.stg = Ring(E, [128, 2048], F32, 2, "cstg", es=es)
        self.n = 0

    def cast3(self, dst, d_dst, src, c0, C, scale=None, nk_total=8):
        E, kb = self.E, self.E.kb
        sv = src.rearrange("(k p) c -> p k c", p=128)
        nk = max(1, min(nk_total, 2048 // C))
        for k0 in range(0, nk_total, nk):
            kk = min(nk, nk_total - k0)
            st, d_st = self.stg.next()
            v = st[:, 0:kk * C].rearrange("p (k c) -> p k c", k=kk)
            kb.dma(v, sv[:, k0:k0 + kk, c0:c0 + C], W=[d_st])
            if scale is not None:
                kb.I('scalar', 'mul', out=dst[:, k0:k0 + kk, :], in_=v, mul=scale, R=[d_st], W=[d_dst])
            else:
                eng = ('vector', 'gpsimd')[self.n % 2]; self.n += 1
                kb.I(eng, 'tensor_copy', out=dst[:, k0:k0 + kk, :], in_=v, R=[d_st], W=[d_dst])


def gla_layer(E, A):
    kb = E.kb
    DK = 128
    Pl = alloc_mod(E); Pc = alloc_mod(E)
    Wq = E.sb([128, 8, 512], BF16, "Wq"); Wk = E.sb([128, 8, 512], BF16, "Wk")
    Wv = E.sb([128, 8, 1024], BF16, "Wv"); Wr = E.sb([128, 8, 1024], BF16, "Wr")
    Wg = E.sb([128, 8, 32], BF16, "Wg"); Wo = E.sb([128, 8, 1024], BF16, "Wo")
    d_W = Dep()
    wgate = E.sb([16, 2, 512], F32, "wgate"); bgate = E.sb([1, 2, 512], F32, "bgate"); d_gate = Dep()
    cm = E.sb([128, 6, 128], F32, "cmats"); d_cm = Dep()
    gng = E.sb([128, 1024], F32, "gng"); d_gng = Dep()
    onec = E.sb([128, 1], F32, "onec"); d_onec = Dep()
    S32 = E.sb([128, 1024], F32, "S32"); d_S32 = Dep()
    Sbf = E.sb([128, 1024], BF16, "Sbf"); d_Sbf = Dep()
    nt = NormT(E, nbuf=1); post = PostRes(E, nbuf=1)
    xs = Ring(E, [128, D], F32, 2, "x")
    hT = Ring(E, [128, 8, 128], BF16, 2, "hT")
    glr = Ring(E, [16, 128], F32, 2, "glr")
    e1 = Ring(E, [128, 512], F32, 2, "e1"); lsp = Ring(E, [128, 512], F32, 2, "lsp")
    EbT = Ring(E, [128, 512], F32, 2, "EbT"); EnbT = Ring(E, [128, 512], F32, 2, "EnbT"); Eend = Ring(E, [128, 512], F32, 2, "Eend")
    qd = Ring(E, [128, 512], BF16, 2, "qd"); ki = Ring(E, [128, 512], BF16, 2, "ki"); ke = Ring(E, [128, 512], BF16, 2, "ke")
    vsb = Ring(E, [128, 1024], BF16, 1, "vsb"); sT = Ring(E, [128, 512], BF16, 2, "sT")
    osb = Ring(E, [128, 1024], F32, 2, "osb")
    ofl = Ring(E, [128, 1024], F32, 1, "ofl")
    sr = Ring(E, [128, 1024], F32, 1, "sr")
    onb = Ring(E, [128, 1024], BF16, 1, "onb"); onT = Ring(E, [128, 8, 128], BF16, 1, "onT")
    ssh = Ring(E, [128, 8], F32, 2, "ssh")
    pA = E.ps([128, 512], F32, "pA"); d_pA = PDep()
    pB = E.ps([128, 512], F32, "pB"); d_pB = PDep()
    p2a = E.ps([128, 1024], F32, "p2a"); d_p2a = PDep()
    p2b = E.ps([128, 1024], F32, "p2b"); d_p2b = PDep()
    E.n += 1
    ofd = E.dram(f"ofd_{E.n}", [34, 128, 1024], F32)
    d_ofd = [Dep() for _ in range(34)]
    with ExitStack() as es:
        (Gl, Sl, GGl, dl), (Gc, Sc, GGc, dc) = compute_mod2(E, Pl, Pc, A['c'], A['cc'], A['mod_w'], A['mod_b'], A['norm_g'], 0, es, pA, d_pA, pB, d_pB)
        kb.barrier()
    with ExitStack() as es:
        cs = Caster(E, es)
        w = A['gla_w_in']
        cs.cast3(Wq, d_W, w, 0, 512, scale=DK ** -0.5)
        cs.cast3(Wk, d_W, w, 512, 512)
        cs.cast3(Wv, d_W, w, 1024, 1024)
        cs.cast3(Wr, d_W, w, 2048, 1024)
        cs.cast3(Wg, d_W, w, 3072, 32)
        cs.cast3(Wo, d_W, A['gla_w_out'], 0, 1024)
        kb.dma(wgate[:], A['gla_w_gate'].rearrange("z r k -> r z k"), W=[d_gate])
        kb.dma(bgate[:], A['gla_b_gate'].rearrange("(o z) k -> o z k", o=1), W=[d_gate])
        kb.dma(cm[:], A['gla_cm'].rearrange("m p c -> p m c"), W=[d_cm])
        kb.I('vector', 'memset', onec[:], 1.0, W=[d_onec])
        kb.I('vector', 'memset', S32[:], 0.0, W=[d_S32])
        row = E.sb([1, 256], F32, "gnrow", es=es); d_row = Dep()
        kb.dma(row[:], A['gla_norm_g'].rearrange("(o c) -> o c", o=1), W=[d_row])
        kb.I('tensor', 'matmul', out=pA[:, 0:256], lhsT=E.ones[0:1, 0:128], rhs=row[0:1, :], start=True, stop=True,
             R=[d_row, E.d_ones], W=[d_pA])
        for h in range(4):
            kb.I('vector', 'tensor_copy', out=gng[:, h * 256:(h + 1) * 256], in_=pA[:, 0:256], R=[d_pA], W=[d_gng])
        kb.barrier()
    d_onec.const = True; d_cm.const = True; d_W.const = True; d_gate.const = True; d_gng.const = True
    for dd in list(dl) + list(dc):
        dd.const = True
    cxv = A['cx'].rearrange("(n p) d -> n p d", p=128); cxo = A['cx_o'].rearrange("(n p) d -> n p d", p=128)
    ltv = A['lat'].rearrange("(n p) d -> n p d", p=128); lto = A['lat_o'].rearrange("(n p) d -> n p d", p=128)
    seq = [(cxv[j], cxo[j], 1) for j in range(2)] + [(ltv[j], lto[j], 0) for j in range(32)]
    for direction in (0, 1):
        order = list(range(34)) if direction == 0 else [1, 0] + list(range(33, 1, -1))
        U = cm[:, 0 + 2 * direction, :]; L = cm[:, 1 + 2 * direction, :]; mask = cm[:, 4 + direction, :]
        ecol = 127 if direction == 0 else 0
        kb.I('vector', 'memset', S32[:], 0.0, W=[d_S32])
        kb.I('vector', 'memset', Sbf[:], 0.0, W=[d_Sbf])
        for ti in order:
            src, dst, isc = seq[ti]
            G, S_, GG, dm = (Gc, Sc, GGc, dc) if isc else (Gl, Sl, GGl, dl)
            x, d_x = xs.next()
            kb.dma(x[:], src, W=[d_x])
            h_, d_h = hT.next()
            nt.run(x[:], d_x, G, S_, dm[:2], lambda k: h_[:, k, :], d_h)
            g_, d_g = glr.next()
            for k in range(8):
                kb.I('tensor', 'matmul', out=pA[0:16, 0:128], lhsT=Wg[:, k, direction * 16:(direction + 1) * 16], rhs=h_[:, k, :],
                     start=(k == 0), stop=(k == 7), R=[d_W, d_h], W=[d_pA])
            kb.I('vector', 'tensor_copy', out=g_[:], in_=pA[0:16, 0:128], R=[d_pA], W=[d_g])
            kb.I('tensor', 'matmul', out=pB[:], lhsT=g_[:], rhs=wgate[:, direction, :], start=True, stop=False,
                 R=[d_g, d_gate], W=[d_pB])
            kb.I('tensor', 'matmul', out=pB[:], lhsT=E.ones[0:1, 0:128], rhs=bgate[0:1, direction, :], start=False, stop=True,
                 R=[d_gate, E.d_ones], W=[d_pB])
            e_, d_e = e1.next(); l_, d_l = lsp.next()
            kb.I('scalar', 'activation', out=e_[:], in_=pB[:], func=AF.Exp, scale=-1.0, R=[d_pB], W=[d_e])
            kb.I('scalar', 'activation', out=l_[:], in_=e_[:], func=AF.Ln, bias=onec[:, 0:1], scale=1.0, R=[d_e, d_onec], W=[d_l])
            for h in range(4):
                kb.I('tensor', 'matmul', out=pA[:, h * 128:(h + 1) * 128], lhsT=l_[:, h * 128:(h + 1) * 128], rhs=U,
                     start=True, stop=True, R=[d_l, d_cm], W=[d_pA])
            eb, d_eb = EbT.next(); enb, d_enb = EnbT.next(); een, d_een = Eend.next()
            kb.I('scalar', 'activation', out=eb[:], in_=pA[:], func=AF.Exp, R=[d_pA], W=[d_eb])
            kb.I('scalar', 'activation', out=enb[:], in_=pA[:], func=AF.Exp, scale=-1.0, R=[d_pA], W=[d_enb])
            kb.I('tensor', 'matmul', out=pB[:], lhsT=L, rhs=l_[:], start=True, stop=True, R=[d_l, d_cm], W=[d_pB])
            kb.I('scalar', 'activation', out=een[:], in_=pB[:], func=AF.Exp, R=[d_pB], W=[d_een])
            for h in range(4):
                for k in range(8):
                    kb.I('tensor', 'matmul', out=pA[:, h * 128:(h + 1) * 128], lhsT=Wq[:, k, h * 128:(h + 1) * 128], rhs=h_[:, k, :],
                         start=(k == 0), stop=(k == 7), R=[d_W, d_h], W=[d_pA])
            q_, d_q = qd.next()
            kb.I('vector', 'tensor_tensor', out=q_[:], in0=pA[:], in1=eb[:], op=ALU.mult, R=[d_pA, d_eb], W=[d_q])
            for h in range(4):
                for k in range(8):
                    kb.I('tensor', 'matmul', out=pB[:, h * 128:(h + 1) * 128], lhsT=Wk[:, k, h * 128:(h + 1) * 128], rhs=h_[:, k, :],
                         start=(k == 0), stop=(k == 7), R=[d_W, d_h], W=[d_pB])
            k_, d_k = ki.next()
            kb.I('vector', 'tensor_tensor', out=k_[:], in0=pB[:], in1=enb[:], op=ALU.mult, R=[d_pB, d_enb], W=[d_k])
            for k in range(8):
                kb.I('tensor', 'matmul', out=pA[:], lhsT=h_[:, k, :], rhs=Wk[:, k, :], start=(k == 0), stop=(k == 7),
                     R=[d_W, d_h], W=[d_pA])
            ke_, d_ke = ke.next()
            kb.I('vector', 'tensor_tensor', out=ke_[:], in0=pA[:], in1=een[:], op=ALU.mult, R=[d_pA, d_een], W=[d_ke])
            for half in range(2):
                for k in range(8):
                    kb.I('tensor', 'matmul', out=p2a[:, half * 512:(half + 1) * 512], lhsT=h_[:, k, :],
                         rhs=Wv[:, k, half * 512:(half + 1) * 512], start=(k == 0), stop=(k == 7), R=[d_W, d_h], W=[d_p2a])
            v_, d_v = vsb.next()
            kb.I('scalar', 'copy', out=v_[:], in_=p2a[:], R=[d_p2a], W=[d_v])
            for h in range(4):
                kb.I('tensor', 'matmul', out=pB[:, h * 128:(h + 1) * 128], lhsT=k_[:, h * 128:(h + 1) * 128],
                     rhs=q_[:, h * 128:(h + 1) * 128], start=True, stop=True, R=[d_k, d_q], W=[d_pB])
            s_, d_s = sT.next()
            for h in range(4):
                kb.I('vector', 'tensor_tensor', out=s_[:, h * 128:(h + 1) * 128], in0=pB[:, h * 128:(h + 1) * 128], in1=mask,
                     op=ALU.mult, R=[d_pB, d_cm], W=[d_s])
            for h in range(4):
                kb.I('tensor', 'matmul', out=p2b[:, h * 256:(h + 1) * 256], lhsT=s_[:, h * 128:(h + 1) * 128],
                     rhs=v_[:, h * 256:(h + 1) * 256], start=True, stop=False, R=[d_s, d_v], W=[d_p2b])
                kb.I('tensor', 'matmul', out=p2b[:, h * 256:(h + 1) * 256], lhsT=q_[:, h * 128:(h + 1) * 128],
                     rhs=Sbf[:, h * 256:(h + 1) * 256], start=False, stop=True, R=[d_q, d_Sbf], W=[d_p2b])
            for h in range(4):
                kb.I('tensor', 'matmul', out=p2a[:, h * 256:(h + 1) * 256], lhsT=ke_[:, h * 128:(h + 1) * 128],
                     rhs=v_[:, h * 256:(h + 1) * 256], start=True, stop=True, R=[d_ke, d_v], W=[d_p2a])
            for h in range(4):
                kb.I('vector', 'scalar_tensor_tensor', out=S32[:, h * 256:(h + 1) * 256], in0=S32[:, h * 256:(h + 1) * 256],
                     scalar=eb[:, h * 128 + ecol:h * 128 + ecol + 1], in1=p2a[:, h * 256:(h + 1) * 256],
                     op0=ALU.mult, op1=ALU.add, R=[d_S32, d_eb, d_p2a], W=[d_S32])
            kb.I('scalar', 'copy', out=Sbf[:], in_=S32[:], R=[d_S32], W=[d_Sbf])
            o_, d_o = osb.next()
            if direction == 0:
                kb.I('scalar', 'copy', out=o_[:], in_=p2b[:], R=[d_p2b], W=[d_o])
                kb.dma(ofd[ti], o_[:], R=[d_o], W=[d_ofd[ti]])
                continue
            f_, d_f = ofl.next()
            kb.dma(f_[:], ofd[ti], R=[d_ofd[ti]], W=[d_f])
            kb.I('vector', 'tensor_tensor', out=o_[:], in0=p2b[:], in1=f_[:], op=ALU.add, R=[d_p2b, d_f], W=[d_o])
            ss_, d_ss = ssh.next()
            for h in range(4):
                kb.I('scalar', 'activation', out=f_[:, h * 256:(h + 1) * 256], in_=o_[:, h * 256:(h + 1) * 256], func=AF.Square,
                     accum_out=ss_[:, h:h + 1], R=[d_o], W=[d_f, d_ss])
            kb.I('scalar', 'activation', out=ss_[:, 4:8], in_=ss_[:, 0:4], func=AF.Sqrt, bias=E.epsc[:, 0:1], scale=1.0 / 256,
                 R=[d_ss, E.d_eps], W=[d_ss])
            kb.I('vector', 'reciprocal', out=ss_[:, 4:8], in_=ss_[:, 4:8], R=[d_ss], W=[d_ss])
            for half in range(2):
                for k in range(8):
                    kb.I('tensor', 'matmul', out=p2a[:, half * 512:(half + 1) * 512], lhsT=h_[:, k, :],
                         rhs=Wr[:, k, half * 512:(half + 1) * 512], start=(k == 0), stop=(k == 7), R=[d_W, d_h], W=[d_p2a])
            r_, d_r = sr.next()
            kb.I('scalar', 'activation', out=r_[:], in_=p2a[:], func=AF.Silu, R=[d_p2a], W=[d_r])
            kb.I('gpsimd', 'tensor_tensor', out=r_[:], in0=r_[:], in1=gng[:], op=ALU.mult, R=[d_r, d_gng], W=[d_r])
            ob_, d_ob = onb.next()
            for h in range(4):
                kb.I('vector', 'scalar_tensor_tensor', out=ob_[:, h * 256:(h + 1) * 256], in0=o_[:, h * 256:(h + 1) * 256],
                     scalar=ss_[:, 4 + h:5 + h], in1=r_[:, h * 256:(h + 1) * 256], op0=ALU.mult, op1=ALU.mult,
                     R=[d_o, d_ss, d_r], W=[d_ob])
            oT_, d_oT = onT.next()
            for k in range(8):
                kb.I('tensor', 'transpose', out=nt.pT[0][:, k, :], in_=ob_[:, k * 128:(k + 1) * 128], identity=E.idb[:],
                     R=[d_ob, E.d_idb], W=[nt.d_pT[0]])
            kb.I('scalar', 'copy', out=oT_[:], in_=nt.pT[0][:], R=[nt.d_pT[0]], W=[d_oT])
            for half in range(2):
                for k in range(8):
                    kb.I('tensor', 'matmul', out=p2b[:, half * 512:(half + 1) * 512], lhsT=oT_[:, k, :],
                         rhs=Wo[:, k, half * 512:(half + 1) * 512], start=(k == 0), stop=(k == 7), R=[d_W, d_oT], W=[d_p2b])
            post.run(p2b[:], d_p2b, x[:], d_x, GG, dm[2])
            kb.dma(dst, x[:], R=[d_x])


TWO_PI = 6.283185307179586


def bcast_rows(E, dst, d_dst, src_row_ap, ncols, pm, d_pm, stg, d_stg):
    kb = E.kb
    for c0 in range(0, ncols, 512):
        cc = min(512, ncols - c0)
        kb.dma(stg[0:1, 0:cc], src_row_ap[c0:c0 + cc].rearrange("(o c) -> o c", o=1), W=[d_stg])
        kb.I('tensor', 'matmul', out=pm[:, 0:cc], lhsT=E.ones[0:1, 0:128], rhs=stg[0:1, 0:cc], start=True, stop=True,
             R=[d_stg, E.d_ones], W=[d_pm])
        kb.I('vector', 'tensor_copy', out=dst[:, c0:c0 + cc], in_=pm[:, 0:cc], R=[d_pm], W=[d_dst])


def hyena_seq(E, A, L, tag, src, dst, G, S_, GG, dm, nt, post, PS):
    kb = E.kb
    T = L // 128
    NFQ = T + 1
    TB = min(512, L); NTB = L // TB
    pA, d_pA, pB, d_pB, p2a, d_p2a, p2b, d_p2b = PS
    E.n += 1
    u = E.n
    hs_d = E.dram(f"hs_{u}", [T, 128, D], BF16); hd_d = E.dram(f"hd_{u}", [T, 128, D], BF16)
    w_d = E.dram(f"w_{u}", [T, 128, D], BF16); x0_d = E.dram(f"x0_{u}", [8, 128, L], BF16)
    pq_d = E.dram(f"pq_{u}", [NFQ, 128, 2 * D], BF16); yh_d = E.dram(f"yh_{u}", [NFQ, 128, 2 * D], BF16)
    yx_d = E.dram(f"yx_{u}", [8, 128, L], BF16)
    d_hs = Dep(); d_hd = Dep(); d_w = Dep(); d_x0 = Dep(); d_pq = Dep(); d_yh = Dep(); d_yx = Dep()
    srcv = src.rearrange("(n p) d -> n p d", p=128); dstv = dst.rearrange("(n p) d -> n p d", p=128)
    T1c, T1s, T2c, T2s = A[f'T1c_{tag}'], A[f'T1s_{tag}'], A[f'T2c_{tag}'], A[f'T2s_{tag}']

    with ExitStack() as es:
        fe = E.sb([33, L], F32, "fe", es=es); d_fe = Dep()
        w1 = E.sb([33, 64], F32, "w1", es=es); w2 = E.sb([64, 64], F32, "w2", es=es); w3 = E.sb([64, 2 * D], F32, "w3", es=es)
        cols = E.sb([64, 8], F32, "fcols", es=es); d_fw = Dep()
        z1 = E.sb([64, L], F32, "z1", es=es); z2 = E.sb([64, L], F32, "z2", es=es); d_z1 = Dep(); d_z2 = Dep()
        dbc = E.sb([128, D], F32, "dbc", es=es); d_dbc = Dep()
        tn = E.sb([128, T], F32, "tn", es=es); d_tn = Dep()
        dec = Ring(E, [128, D], F32, 2, "dec", es=es)
        hf = Ring(E, [128, D], F32, 2, "hf", es=es); hb = Ring(E, [128, D], F32, 2, "hb", es=es)
        hso = Ring(E, [128, D], BF16, 2, "hso", es=es); hdo = Ring(E, [128, D], BF16, 2, "hdo", es=es)
        kb.dma(fe[:], A[f'feT_{tag}'], W=[d_fe])
        kb.dma(w1[:], A['hy_f_w1'], W=[d_fw]); kb.dma(w2[:], A['hy_f_w2'], W=[d_fw]); kb.dma(w3[:], A['hy_f_w3'], W=[d_fw])
        kb.dma(cols[:, 0:1], A['hy_f_freq'].rearrange("(p o) -> p o", o=1), W=[d_fw])
        kb.dma(cols[:, 1:2], A['hy_f_b1'].rearrange("(p o) -> p o", o=1), W=[d_fw])
        kb.dma(cols[:, 2:3], A['hy_f_b2'].rearrange("(p o) -> p o", o=1), W=[d_fw])
        kb.dma(dbc[:], A['hy_delta_bc'], W=[d_dbc])
        kb.dma(tn[:], A[f'tn_{tag}'], W=[d_tn])
        three = E.sb([64, 512], F32, "three", es=es); d_three = Dep(const=True)
        sq_ = E.sb([64, 512], F32, "sq_", es=es); d_sq_ = Dep()
        s3 = E.sb([64, 512], F32, "s3", es=es); d_s3 = Dep()
        kb.I('vector', 'memset', three[:], 3.0, W=[d_three])
        kb.I('scalar', 'mul', out=cols[:, 3:4], in_=cols[:, 0:1], mul=1.0 / 3.0, R=[d_fw], W=[d_fw])
        kb.I('vector', 'tensor_tensor', out=cols[:, 4:5], in0=cols[:, 1:2], in1=cols[:, 3:4], op=ALU.mult, R=[d_fw], W=[d_fw])
        kb.I('vector', 'tensor_tensor', out=cols[:, 5:6], in0=cols[:, 2:3], in1=cols[:, 3:4], op=ALU.mult, R=[d_fw], W=[d_fw])
        for (zin, d_zin, wm, zo, d_zo, bc, kk) in ((fe, d_fe, w1, z1, d_z1, 4, 33), (z1, d_z1, w2, z2, d_z2, 5, 64)):
            for c0 in range(0, L, 512):
                cw = min(512, L - c0)
                kb.I('tensor', 'matmul', out=pA[0:64, 0:cw], lhsT=wm[0:kk, :], rhs=zin[0:kk, c0:c0 + cw], start=True, stop=True,
                     R=[d_zin, d_fw], W=[d_pA])
                kb.I('scalar', 'activation', out=s3[:, 0:cw], in_=pA[0:64, 0:cw], func=AF.Sin, scale=cols[:, 3:4],
                     bias=cols[:, bc:bc + 1], R=[d_pA, d_fw], W=[d_s3])
                kb.I('vector', 'tensor_tensor', out=sq_[:, 0:cw], in0=s3[:, 0:cw], in1=s3[:, 0:cw], op=ALU.mult, R=[d_s3], W=[d_sq_])
                kb.I('vector', 'scalar_tensor_tensor', out=sq_[:, 0:cw], in0=sq_[:, 0:cw], scalar=-4.0, in1=three[:, 0:cw],
                     op0=ALU.mult, op1=ALU.add, R=[d_sq_, d_three], W=[d_sq_])
                kb.I('vector', 'tensor_tensor', out=zo[:, c0:c0 + cw], in0=sq_[:, 0:cw], in1=s3[:, 0:cw], op=ALU.mult,
                     R=[d_sq_, d_s3], W=[d_zo])
        for t in range(T):
            for (pp, d_pp, c0) in ((p2a, d_p2a, 0), (p2b, d_p2b, D)):
                for half in range(2):
                    kb.I('tensor', 'matmul', out=pp[:, half * 512:(half + 1) * 512], lhsT=z2[:, t * 128:(t + 1) * 128],
                         rhs=w3[:, c0 + half * 512:c0 + (half + 1) * 512], start=True, stop=True, R=[d_z2, d_fw], W=[d_pp])
            de, d_de = dec.next()
            kb.I('scalar', 'activation', out=de[:], in_=dbc[:], func=AF.Exp, scale=tn[:, t:t + 1], R=[d_dbc, d_tn], W=[d_de])
            f_, d_f = hf.next(); b_, d_b = hb.next()
            kb.I('vector', 'tensor_tensor', out=f_[:], in0=p2a[:], in1=de[:], op=ALU.mult, R=[d_p2a, d_de], W=[d_f])
            kb.I('vector', 'tensor_tensor', out=b_[:], in0=p2b[:], in1=de[:], op=ALU.mult, R=[d_p2b, d_de], W=[d_b])
            so, d_so = hso.next(); do_, d_do = hdo.next()
            kb.I('gpsimd', 'tensor_tensor', out=so[:], in0=f_[:], in1=b_[:], op=ALU.add, R=[d_f, d_b], W=[d_so])
            kb.I('gpsimd', 'tensor_tensor', out=do_[:], in0=f_[:], in1=b_[:], op=ALU.subtract, R=[d_f, d_b], W=[d_do])
            kb.dma(hs_d[t], so[:], R=[d_so], W=[d_hs])
            kb.dma(hd_d[t], do_[:], R=[d_do], W=[d_hd])
        kb.barrier()

    with ExitStack() as esA:
        hTa = E.sb([128, 8, L + 2], BF16, "hTa", es=esA); d_hTa = Dep()
        xs = Ring(E, [128, D], F32, 2, "hx", es=esA)
        kb.I('vector', 'memset', hTa[:, :, 0:1], 0.0, W=[d_hTa])
        kb.I('vector', 'memset', hTa[:, :, L + 1:L + 2], 0.0, W=[d_hTa])
        for t in range(T):
            x, d_x = xs.next()
            kb.dma(x[:], srcv[t], W=[d_x])
            nt.run(x[:], d_x, G, S_, dm[:2], lambda k, t=t: hTa[:, k, 1 + t * 128:1 + (t + 1) * 128], d_hTa)
        wv = A['hy_w_in'].rearrange("(k p) c -> p k c", p=128)
        for sub in (0, 1, 2, 3):
            with ExitStack() as es:
                parts = (1, 2) if sub < 2 else (0,)
                half = sub % 2
                Wt = {pp: [E.sb([128, 8, 512], BF16, "Wt", es=es) for _ in range(3)] for pp in parts}
                d_Wt = Dep()
                cwb = E.sb([128, 3, 512], F32, "cwb", es=es); d_cwb = Dep()
                stg = Ring(E, [128, 512], F32, 2, "wstg", es=es)
                rstg = E.sb([1, 512], F32, "rstg", es=es); d_rstg = Dep()
                for pp in parts:
                    c0 = pp * D + half * 512
                    for j in range(3):
                        bcast_rows(E, cwb[:, j, :], d_cwb, A['hy_conv_w'][j, c0:c0 + 512], 512, pA, d_pA, rstg, d_rstg)
                    for k in range(8):
                        st, d_st = stg.next()
                        kb.dma(st[:], wv[:, k, c0:c0 + 512], W=[d_st])
                        for j in range(3):
                            kb.I(('vector', 'gpsimd', 'vector')[j], 'tensor_tensor', out=Wt[pp][j][:, k, :], in0=st[:], in1=cwb[:, j, :],
                                 op=ALU.mult, R=[d_st, d_cwb], W=[d_Wt])
                if sub < 2:
                    brow = E.sb([1, 2, 512], F32, "brow", es=es); d_brow = Dep()
                    for pp in (1, 2):
                        kb.dma(brow[0:1, pp - 1, :], A['hy_conv_b'][pp * D + half * 512:pp * D + half * 512 + 512].rearrange("(o c) -> o c", o=1),
                               W=[d_brow])
                    x1s = Ring(E, [128, 512], F32, 2, "x1s", es=es); wo_ = Ring(E, [128, 512], BF16, 2, "wo_", es=es)
                    for t in range(T):
                        for (pp, pt, d_pt) in ((1, pA, d_pA), (2, pB, d_pB)):
                            for j in range(3):
                                for k in range(8):
                                    kb.I('tensor', 'matmul', out=pt[:], lhsT=hTa[:, k, t * 128 + j:t * 128 + j + 128], rhs=Wt[pp][j][:, k, :],
                                         start=(j == 0 and k == 0), stop=False, R=[d_hTa, d_Wt], W=[d_pt])
                            kb.I('tensor', 'matmul', out=pt[:], lhsT=E.ones[0:1, 0:128], rhs=brow[0:1, pp - 1, :], start=False, stop=True,
                                 R=[d_brow, E.d_ones], W=[d_pt])
                        x1, d_x1 = x1s.next()
                        kb.I('scalar', 'copy', out=x1[:], in_=pA[:], R=[d_pA], W=[d_x1])
                        w_, d_w_ = wo_.next()
                        kb.I('vector', 'tensor_tensor', out=w_[:], in0=pB[:], in1=x1[:], op=ALU.mult, R=[d_pB, d_x1], W=[d_w_])
                        kb.dma(w_d[t][:, half * 512:(half + 1) * 512], w_[:], R=[d_w_], W=[d_w])
                else:
                    bcol = E.sb([128, 4], F32, "bcol0", es=es); d_bcol = Dep()
                    kb.dma(bcol[:], A['hy_conv_b'][half * 512:(half + 1) * 512].rearrange("(k p) -> p k", p=128), W=[d_bcol],
                           allow_slow_non_contiguous=True)
                    xo = Ring(E, [128, TB], BF16, 2, "xo", es=es)
                    pr = [(pA, d_pA), (pB, d_pB)]
                    n = 0
                    for ci in range(4):
                        cc = half * 4 + ci
                        for tb in range(NTB):
                            pt, d_pt = pr[n % 2]; n += 1
                            for j in range(3):
                                for k in range(8):
                                    kb.I('tensor', 'matmul', out=pt[:, 0:TB], lhsT=Wt[0][j][:, k, ci * 128:(ci + 1) * 128],
                                         rhs=hTa[:, k, tb * TB + j:tb * TB + j + TB], start=(j == 0 and k == 0), stop=(j == 2 and k == 7),
                                         R=[d_hTa, d_Wt], W=[d_pt])
                            o_, d_o = xo.next()
                            kb.I('scalar', 'activation', out=o_[:], in_=pt[:, 0:TB], func=AF.Identity, bias=bcol[:, ci:ci + 1], scale=1.0,
                                 R=[d_pt, d_bcol], W=[d_o])
                            kb.dma(x0_d[cc][:, tb * TB:(tb + 1) * TB], o_[:], R=[d_o], W=[d_x0])
                kb.barrier()

    for sub in (0, 1):
        with ExitStack() as es:
            nres = 2 if sub == 0 else 1
            res = [E.sb([128, T, D], BF16, "res", es=es) for _ in range(nres)]; d_res = Dep()
            t1 = Ring(E, [128, 2, T, 128], BF16, (3 if sub == 1 else 2), "t1", es=es)
            ob = Ring(E, [128, 2 * D], BF16, 2, "ob", es=es)
            srcs = (hs_d, hd_d) if sub == 0 else (w_d,)
            dsrc = (d_hs, d_hd) if sub == 0 else (d_w,)
            for i in range(nres):
                for t in range(T):
                    kb.dma(res[i][:, t, :], srcs[i][t], R=[dsrc[i]], W=[d_res])
            if sub == 0:
                fb = E.sb([128, D], F32, "fb", es=es); d_fb = Dep()
                rstg = E.sb([1, 512], F32, "rstg2", es=es); d_rstg = Dep()
                bcast_rows(E, fb, d_fb, A['hy_f_bias'], D, pA, d_pA, rstg, d_rstg)
            else:
                pq = Ring(E, [128, 2 * D], BF16, 2, "pq", es=es)
                tmp = Ring(E, [128, 512], F32, 2, "ytmp", es=es); tmp2 = Ring(E, [128, 512], F32, 2, "ytmp2", es=es)
            for fq in range(NFQ):
                tt, d_tt = t1.next()
                kb.dma(tt[:, 0], T1c[fq], W=[d_tt])
                kb.dma(tt[:, 1], T1s[fq], W=[d_tt])
                o_, d_o = ob.next()
                if sub == 1:
                    q_, d_q = pq.next()
                    kb.dma(q_[:], pq_d[fq], R=[d_pq], W=[d_q])
                for half in range(2):
                    hsl = slice(half * 512, (half + 1) * 512)
                    pA, d_pA, pB, d_pB = (PS[0], PS[1], PS[2], PS[3]) if half == 0 else (PS[4][:, 0:512], PS[5], PS[6][:, 0:512], PS[7])
                    for tr, (pt, d_pt) in enumerate(((pA, d_pA), (pB, d_pB))):
                        r = res[tr] if sub == 0 else res[0]
                        for tc in range(T):
                            kb.I('tensor', 'matmul', out=pt[:], lhsT=tt[:, tr, tc, :], rhs=r[:, tc, hsl], start=(tc == 0), stop=(tc == T - 1),
                                 R=[d_tt, d_res], W=[d_pt])
                    if sub == 0:
                        kb.I('vector', 'tensor_tensor', out=o_[:, hsl], in0=pA[:], in1=fb[:, hsl], op=ALU.add, R=[d_pA, d_fb], W=[d_o])
                        kb.I('scalar', 'copy', out=o_[:, D + half * 512:D + (half + 1) * 512], in_=pB[:], R=[d_pB], W=[d_o])
                    else:
                        P_ = q_[:, hsl]; Q_ = q_[:, D + half * 512:D + (half + 1) * 512]
                        a_, d_a = tmp.next(); b_, d_b = tmp2.next()
                        kb.I('vector', 'tensor_tensor', out=a_[:], in0=pA[:], in1=P_, op=ALU.mult, R=[d_pA, d_q], W=[d_a])
                        kb.I('vector', 'tensor_tensor', out=b_[:], in0=pB[:], in1=Q_, op=ALU.mult, R=[d_pB, d_q], W=[d_b])
                        kb.I('gpsimd', 'tensor_tensor', out=o_[:, hsl], in0=a_[:], in1=b_[:], op=ALU.subtract, R=[d_a, d_b], W=[d_o])
                        a_, d_a = tmp.next(); b_, d_b = tmp2.next()
                        kb.I('vector', 'tensor_tensor', out=a_[:], in0=pA[:], in1=Q_, op=ALU.mult, R=[d_pA, d_q], W=[d_a])
                        kb.I('vector', 'tensor_tensor', out=b_[:], in0=pB[:], in1=P_, op=ALU.mult, R=[d_pB, d_q], W=[d_b])
                        kb.I('gpsimd', 'tensor_tensor', out=o_[:, D + half * 512:D + (half + 1) * 512], in0=a_[:], in1=b_[:], op=ALU.add,
                             R=[d_a, d_b], W=[d_o])
                if sub == 0:
                    kb.dma(pq_d[fq], o_[:], R=[d_o], W=[d_pq])
                else:
                    kb.dma(yh_d[fq], o_[:], R=[d_o], W=[d_yh])
            kb.barrier()

    pA, d_pA, pB, d_pB, p2a, d_p2a, p2b, d_p2b = PS
    with ExitStack() as es:
        yh = E.sb([128, NFQ, 2 * D], BF16, "yh", es=es); d_yhs = Dep()
        for fq in range(NFQ):
            kb.dma(yh[:, fq, :], yh_d[fq], R=[d_yh], W=[d_yhs])
        t2 = Ring(E, [128, 2, TB], BF16, 4, "t2", es=es)
        x0t = Ring(E, [128, TB], BF16, 2, "x0t", es=es); yo = Ring(E, [128, TB], BF16, 2, "yo", es=es)
        acc = [(pA, d_pA), (pB, d_pB), (p2a, d_p2a), (p2b, d_p2b)]
        for ccg in range(2):
            for tb in range(NTB):
                for fq in range(NFQ):
                    tt, d_tt = t2.next()
                    kb.dma(tt[:, 0, :], T2c[fq][:, tb * TB:(tb + 1) * TB], W=[d_tt])
                    kb.dma(tt[:, 1, :], T2s[fq][:, tb * TB:(tb + 1) * TB], W=[d_tt])
                    for ci in range(4):
                        cc = ccg * 4 + ci
                        pt, d_pt = acc[ci]
                        kb.I('tensor', 'matmul', out=pt[:, 0:TB], lhsT=yh[:, fq, cc * 128:(cc + 1) * 128], rhs=tt[:, 0, :],
                             start=(fq == 0), stop=False, R=[d_yhs, d_tt], W=[d_pt])
                        kb.I('tensor', 'matmul', out=pt[:, 0:TB], lhsT=yh[:, fq, D + cc * 128:D + (cc + 1) * 128], rhs=tt[:, 1, :],
                             start=False, stop=(fq == NFQ - 1), R=[d_yhs, d_tt], W=[d_pt])
                for ci in range(4):
                    cc = ccg * 4 + ci
                    pt, d_pt = acc[ci]
                    x_, d_x_ = x0t.next()
                    kb.dma(x_[:], x0_d[cc][:, tb * TB:(tb + 1) * TB], R=[d_x0], W=[d_x_])
                    o_, d_o = yo.next()
                    kb.I('vector', 'tensor_tensor', out=o_[:], in0=pt[:, 0:TB], in1=x_[:], op=ALU.mult, R=[d_pt, d_x_], W=[d_o])
                    kb.dma(yx_d[cc][:, tb * TB:(tb + 1) * TB], o_[:], R=[d_o], W=[d_yx])
        kb.barrier()

    with ExitStack() as es:
        Wo = E.sb([128, 8, D], BF16, "hWo", es=es); d_Wo = Dep()
        cs = Caster(E, es)
        cs.cast3(Wo, d_Wo, A['hy_w_out'], 0, D)
        yT = Ring(E, [128, 8, 128], BF16, 2, "yT", es=es)
        xs = Ring(E, [128, D], F32, 2, "dx", es=es)
        for t in range(T):
            y_, d_y = yT.next()
            kb.dma(y_[:], yx_d.rearrange("c p l -> p c l")[:, :, t * 128:(t + 1) * 128], R=[d_yx], W=[d_y])
            x, d_x = xs.next()
            kb.dma(x[:], srcv[t], W=[d_x])
            for half in range(2):
                for k in range(8):
                    kb.I('tensor', 'matmul', out=p2a[:, half * 512:(half + 1) * 512], lhsT=y_[:, k, :],
                         rhs=Wo[:, k, half * 512:(half + 1) * 512], start=(k == 0), stop=(k == 7), R=[d_y, d_Wo], W=[d_p2a])
            post.run(p2a[:], d_p2a, x[:], d_x, GG, dm[2])
            kb.dma(dstv[t], x[:], R=[d_x])
        kb.barrier()


def hyena_layer(E, A):
    kb = E.kb
    Pl = alloc_mod(E); Pc = alloc_mod(E)
    nt = NormT(E, nbuf=2); post = PostRes(E, nbuf=2)
    pA = E.ps([128, 512], F32, "pA"); d_pA = PDep()
    pB = E.ps([128, 512], F32, "pB"); d_pB = PDep()
    p2a = E.ps([128, 1024], F32, "p2a"); d_p2a = PDep()
    p2b = E.ps([128, 1024], F32, "p2b"); d_p2b = PDep()
    PS = (pA, d_pA, pB, d_pB, p2a, d_p2a, p2b, d_p2b)
    with ExitStack() as es:
        (Gl, Sl, GGl, dl), (Gc, Sc, GGc, dc) = compute_mod2(E, Pl, Pc, A['c'], A['cc'], A['mod_w'], A['mod_b'], A['norm_g'], 0, es, pA, d_pA, pB, d_pB)
        kb.barrier()
    hyena_seq(E, A, CTXL, "c", A['cx'], A['cx_o'], Gc, Sc, GGc, dc, nt, post, PS)
    hyena_seq(E, A, LAT, "l", A['lat'], A['lat_o'], Gl, Sl, GGl, dl, nt, post, PS)


def col_major_rows(ap2d, n):
    v = ap2d.rearrange("(r w) d -> w r d", w=64)
    return v[2 * n], v[2 * n + 1]


def load_tile(E, x, d_x, A, seg, n):
    kb = E.kb
    if seg == 0:
        kb.dma(x[:], A['cx'].rearrange("(n p) d -> n p d", p=128)[n], W=[d_x])
    else:
        a, b = col_major_rows(A['lat'], n)
        kb.dma(x[0:64, :], a, W=[d_x])
        kb.dma(x[64:128, :], b, W=[d_x])


def store_tile(E, x, d_x, A, seg, n):
    kb = E.kb
    if seg == 0:
        kb.dma(A['cx_o'].rearrange("(n p) d -> n p d", p=128)[n], x[:], R=[d_x])
    else:
        a, b = col_major_rows(A['lat_o'], n)
        kb.dma(a, x[0:64, :], R=[d_x])
        kb.dma(b, x[64:128, :], R=[d_x])


def mlstm_layer(E, A, passes=(1, 2)):
    kb = E.kb
    LT = CTXL + LAT
    NTT = LT // 128
    Pl = alloc_mod(E); Pc = alloc_mod(E)
    nt = NormT(E, nbuf=1); post = PostRes(E, nbuf=1)
    pA = E.ps([128, 512], F32, "pA"); d_pA = PDep()
    pB = E.ps([128, 512], F32, "pB"); d_pB = PDep()
    pC = E.ps([128, 512], F32, "pC"); d_pC = PDep()
    p4 = E.ps([128, 2048], F32, "p4"); d_p4 = PDep()
    gates = E.sb([16, LT], F32, "gates"); d_gates = Dep()
    E.n += 1
    u = E.n
    qT_d = E.dram(f"mqT_{u}", [16, 128, LT], BF16); kT_d = E.dram(f"mkT_{u}", [16, 128, LT], BF16)
    xcT_d = E.dram(f"mxc_{u}", [16, 128, LT], BF16); szT_d = E.dram(f"msz_{u}", [16, 128, LT], BF16)
    ktok_d = E.dram(f"mkt_{u}", [NTT, 128, 2048], BF16); vtok_d = E.dram(f"mvt_{u}", [NTT, 128, 2048], BF16)
    hf_d = E.dram(f"mhf_{u}", [NTT, 128, 2048], F32)
    d_qT = Dep(); d_kT = Dep(); d_xcT = Dep(); d_szT = Dep(); d_ktok = Dep(); d_vtok = Dep()
    d_hf = [Dep() for _ in range(NTT)]
    with ExitStack() as es:
        (Gl, Sl, GGl, dl), (Gc, Sc, GGc, dc) = compute_mod2(E, Pl, Pc, A['c'], A['cc'], A['mod_w'], A['mod_b'], A['norm_g'], 0, es, pA, d_pA, pB, d_pB)
        kb.barrier()
    for dd in list(dl) + list(dc):
        dd.const = True

    with ExitStack() as es:
        hTa = E.sb([128, 8, LAT + 2], BF16, "mhTa", es=es); d_hTa = Dep()
        bd = E.sb([128, 3, 16, 128], BF16, "bd", es=es); d_bd = Dep()
        wg = E.sb([128, 48, 16], BF16, "wg", es=es); d_wg = Dep()
        cwc = E.sb([128, 4, 16], F32, "cwc", es=es); d_cwc = Dep()
        xs = Ring(E, [128, D], F32, 2, "mx", es=es)
        stg = Ring(E, [128, 2048], F32, 2, "mstg", es=es)
        wxb = Ring(E, [128, 8, 128], BF16, 2, "wxb", es=es); wzb = Ring(E, [128, 8, 128], BF16, 2, "wzb", es=es)
        xm32 = E.sb([128, LAT + 2], F32, "xm32", es=es); d_xm32 = Dep()
        tcv = E.sb([128, LAT], F32, "tcv", es=es); d_tcv = Dep()
        xmb = E.sb([128, LAT], BF16, "xmb", es=es); d_xmb = Dep()
        xcb = E.sb([128, LAT], BF16, "xcb", es=es); d_xcb = Dep()
        ob = Ring(E, [128, 512], BF16, 4, "mob", es=es)
        for j in range(3):
            for c0 in range(0, 16, 8):
                st, d_st = stg.next()
                kb.dma(st[:, 0:1024].rearrange("p (f c) -> p f c", f=8), A['ml_bd'][j, c0:c0 + 8].rearrange("f p c -> p f c"), W=[d_st])
                kb.I('vector', 'tensor_copy', out=bd[:, j, c0:c0 + 8, :], in_=st[:, 0:1024].rearrange("p (f c) -> p f c", f=8),
                     R=[d_st], W=[d_bd])
        st, d_st = stg.next()
        for z in range(2):
            for c0 in range(0, 48, 8):
                kb.dma(st[:, 0:768].rearrange("p (c z g) -> p c z g", c=48, z=2)[:, c0:c0 + 8, z, :],
                       A['ml_w_gates'][z].rearrange("(c p) g -> p c g", p=128)[:, c0:c0 + 8, :], W=[d_st], allow_slow_non_contiguous=True)
        kb.I('vector', 'tensor_copy', out=wg[:], in_=st[:, 0:768].rearrange("p (c zg) -> p c zg", c=48), R=[d_st], W=[d_wg])
        st, d_st = stg.next()
        kb.dma(st[0:48, 0:128], A['ml_conv_w'].rearrange("j (f p) -> (j f) p", p=128), W=[d_st])
        kb.dma(st[48:64, 0:128], A['ml_conv_b'].rearrange("(f p) -> f p", p=128), W=[d_st])
        kb.I('tensor', 'matmul', out=pA[:, 0:64], lhsT=st[0:64, 0:128], rhs=E.idf[0:64, 0:64], start=True, stop=True, R=[d_st, E.d_idf], W=[d_pA])
        kb.I('vector', 'tensor_copy', out=cwc[:].rearrange("p j f -> p (j f)"), in_=pA[:, 0:64], R=[d_pA], W=[d_cwc])
        wv = A['ml_w_in'].rearrange("(k p) c -> p k c", p=128)
        STOP = 99
        if STOP == 0:
            kb.barrier(); return
        for seg, L, off, tb0 in ((0, CTXL, 0, 0), (1, LAT, CTXL, 2)):
            T = L // 128; B = min(512, L); NB = L // B
            G, S_, dm = (Gc, Sc, dc) if seg == 0 else (Gl, Sl, dl)
            kb.I('vector', 'memset', hTa[:, :, 0:1], 0.0, W=[d_hTa])
            kb.I('vector', 'memset', hTa[:, :, L + 1:L + 2], 0.0, W=[d_hTa])
            for t in range(T):
                x, d_x = xs.next()
                load_tile(E, x, d_x, A, seg, t)
                nt.run(x[:], d_x, G, S_, dm[:2], lambda k, t=t: hTa[:, k, 1 + t * 128:1 + (t + 1) * 128], d_hTa)
            if STOP == 1:
                kb.barrier(); return
            kb.I('vector', 'memset', xm32[:, 0:1], 0.0, W=[d_xm32])
            kb.I('vector', 'memset', xm32[:, L + 1:L + 2], 0.0, W=[d_xm32])
            for fc in range(16):
                st, d_st = stg.next()
                wx_, d_wx = wxb.next(); wz_, d_wz = wzb.next()
                kb.dma(st[:, 0:1024].rearrange("p (k c) -> p k c", k=8), wv[:, :, fc * 128:(fc + 1) * 128], W=[d_st])
                kb.dma(st[:, 1024:2048].rearrange("p (k c) -> p k c", k=8), wv[:, :, 2048 + fc * 128:2048 + (fc + 1) * 128], W=[d_st])
                kb.I('vector', 'tensor_copy', out=wx_[:], in_=st[:, 0:1024].rearrange("p (k c) -> p k c", k=8), R=[d_st], W=[d_wx])
                kb.I('gpsimd', 'tensor_copy', out=wz_[:], in_=st[:, 1024:2048].rearrange("p (k c) -> p k c", k=8), R=[d_st], W=[d_wz])
                for blk in range(NB):
                    bs = slice(blk * B, (blk + 1) * B)
                    for k in range(8):
                        kb.I('tensor', 'matmul', out=pA[:, 0:B], lhsT=wx_[:, k, :], rhs=hTa[:, k, 1 + blk * B:1 + (blk + 1) * B],
                             start=(k == 0), stop=(k == 7), R=[d_wx, d_hTa], W=[d_pA])
                    kb.I('scalar', 'copy', out=xm32[:, 1 + blk * B:1 + (blk + 1) * B], in_=pA[:, 0:B], R=[d_pA], W=[d_xm32])
                    kb.I('vector', 'tensor_copy', out=xmb[:, bs], in_=pA[:, 0:B], R=[d_pA], W=[d_xmb])
                    for k in range(8):
                        kb.I('tensor', 'matmul', out=pB[:, 0:B], lhsT=wz_[:, k, :], rhs=hTa[:, k, 1 + blk * B:1 + (blk + 1) * B],
                             start=(k == 0), stop=(k == 7), R=[d_wz, d_hTa], W=[d_pB])
                    o_, d_o = ob.next()
                    kb.I('scalar', 'activation', out=o_[:, 0:B], in_=pB[:, 0:B], func=AF.Silu, R=[d_pB], W=[d_o])
                    kb.dma(szT_d[fc][:, off + blk * B:off + (blk + 1) * B], o_[:, 0:B], R=[d_o], W=[d_szT])
                if STOP == 2:
                    kb.barrier(); return
                kb.I('vector', 'tensor_scalar_mul', out=tcv[:, 0:L], in0=xm32[:, 1:L + 1], scalar1=cwc[:, 1, fc:fc + 1],
                     R=[d_xm32, d_cwc], W=[d_tcv])
                kb.I('vector', 'scalar_tensor_tensor', out=tcv[:, 0:L], in0=xm32[:, 0:L], scalar=cwc[:, 0, fc:fc + 1], in1=tcv[:, 0:L],
                     op0=ALU.mult, op1=ALU.add, R=[d_xm32, d_cwc, d_tcv], W=[d_tcv])
                kb.I('vector', 'scalar_tensor_tensor', out=tcv[:, 0:L], in0=xm32[:, 2:L + 2], scalar=cwc[:, 2, fc:fc + 1], in1=tcv[:, 0:L],
                     op0=ALU.mult, op1=ALU.add, R=[d_xm32, d_cwc, d_tcv], W=[d_tcv])
                kb.I('scalar', 'activation', out=xcb[:, 0:L], in_=tcv[:, 0:L], func=AF.Silu, bias=cwc[:, 3, fc:fc + 1], scale=1.0,
                     R=[d_tcv, d_cwc], W=[d_xcb])
                kb.dma(xcT_d[fc][:, off:off + L], xcb[:, 0:L], R=[d_xcb], W=[d_xcT])
                if STOP == 3:
                    kb.barrier(); return
                for blk in range(NB):
                    bs = slice(blk * B, (blk + 1) * B)
                    outs = []
                    for j, (srcb, d_src, dd, d_dd) in enumerate(((xcb, d_xcb, qT_d, d_qT), (xcb, d_xcb, kT_d, d_kT), (xmb, d_xmb, None, None))):
                        pt, d_pt = (pA, d_pA) if j % 2 == 0 else (pB, d_pB)
                        kb.I('tensor', 'matmul', out=pt[:, 0:B], lhsT=bd[:, j, fc, :], rhs=srcb[:, bs], start=True, stop=True,
                             R=[d_bd, d_src], W=[d_pt])
                        o_, d_o = ob.next()
                        kb.I(('vector', 'scalar', 'vector')[j], 'tensor_copy' if j != 1 else 'copy', out=o_[:, 0:B], in_=pt[:, 0:B],
                             R=[d_pt], W=[d_o])
                        if dd is not None:
                            kb.dma(dd[fc][:, off + blk * B:off + (blk + 1) * B], o_[:, 0:B], R=[d_o], W=[d_dd])
                        outs.append((o_, d_o))
                    for j, (o_, d_o) in enumerate(outs):
                        kb.I('tensor', 'matmul', out=pC[0:16, 0:B], lhsT=wg[:, 16 * j + fc, :], rhs=o_[:, 0:B], start=(j == 0), stop=(j == 2),
                             R=[d_wg, d_o], W=[d_pC])
                    gsl = gates[:, off + blk * B:off + (blk + 1) * B]
                    if fc == 0:
                        kb.I('vector', 'tensor_copy', out=gsl, in_=pC[0:16, 0:B], R=[d_pC], W=[d_gates])
                    else:
                        kb.I('vector', 'tensor_tensor', out=gsl, in0=pC[0:16, 0:B], in1=gsl, op=ALU.add, R=[d_pC, d_gates], W=[d_gates])
                if STOP == 4:
                    kb.barrier(); return
                for t0 in range(0, T, 4):
                    nn = min(4, T - t0)
                    for (srcb, d_src, j, dd, d_dd, pt, d_pt) in ((xcb, d_xcb, 1, ktok_d, d_ktok, pA, d_pA), (xmb, d_xmb, 2, vtok_d, d_vtok, pB, d_pB)):
                        for q in range(nn):
                            kb.I('tensor', 'matmul', out=pt[:, q * 128:(q + 1) * 128], lhsT=srcb[:, (t0 + q) * 128:(t0 + q + 1) * 128],
                                 rhs=bd[:, j, fc, :], start=True, stop=True, R=[d_src, d_bd], W=[d_pt])
                        o_, d_o = ob.next()
                        kb.I('vector' if j == 1 else 'scalar', 'tensor_copy' if j == 1 else 'copy', out=o_[:, 0:nn * 128], in_=pt[:, 0:nn * 128],
                             R=[d_pt], W=[d_o])
                        kb.dma(dd[tb0 + t0:tb0 + t0 + nn].rearrange("t p c -> p t c")[:, :, fc * 128:(fc + 1) * 128],
                               o_[:, 0:nn * 128].rearrange("p (t c) -> p t c", t=nn), R=[d_o], W=[d_dd])
            if STOP == 5:
                kb.barrier(); return
        kb.barrier()

    if 2 not in passes:
        return
    with ExitStack() as es:
        cm = E.sb([128, 7, 128], F32, "mcm", es=es); d_cm = Dep(const=True)
        kb.dma(cm[:], A['ml_cm'].rearrange("m p c -> p m c"), W=[d_cm])
        bgr = E.sb([1, 16], F32, "bgr", es=es); d_bgr = Dep(const=True)
        kb.dma(bgr[:], A['ml_b_gates'].rearrange("(o z) g -> o (z g)", o=1), W=[d_bgr])
        onec = E.sb([128, 1], F32, "monec", es=es); d_onec = Dep(const=True)
        kb.I('vector', 'memset', onec[:], 1.0, W=[d_onec])
        c22 = E.sb([128, 4], F32, "c22", es=es); d_c22 = Dep(const=True)
        kb.I('vector', 'memset', c22[:], 512.0 ** 0.5, W=[d_c22])
        Wo = E.sb([128, 16, D], BF16, "mWo", es=es); d_Wo = Dep()
        ngsk = E.sb([128, 2, 16], F32, "ngsk", es=es); d_ngsk = Dep()
        NGB = E.sb([128, 16, 128], F32, "NGB", es=es); SKB = E.sb([128, 16, 128], F32, "SKB", es=es); d_NS = Dep()
        nstg = E.sb([32, 128], F32, "nstg", es=es); d_nstg = Dep()
        kb.dma(nstg[0:16, :], A['ml_norm_g'].rearrange("(f p) -> f p", p=128), W=[d_nstg])
        kb.dma(nstg[16:32, :], A['ml_skip'].rearrange("(f p) -> f p", p=128), W=[d_nstg])
        kb.I('tensor', 'matmul', out=pA[:, 0:32], lhsT=nstg[0:32, :], rhs=E.idf[0:32, 0:32], start=True, stop=True, R=[d_nstg, E.d_idf], W=[d_pA])
        kb.I('vector', 'tensor_copy', out=ngsk[:].rearrange("p j f -> p (j f)"), in_=pA[:, 0:32], R=[d_pA], W=[d_ngsk])
        for f in range(16):
            kb.I('vector', 'tensor_scalar_mul', out=NGB[:, f, :], in0=E.ones[:, 0:128], scalar1=ngsk[:, 0, f:f + 1], R=[d_ngsk, E.d_ones], W=[d_NS])
            kb.I('vector', 'tensor_scalar_mul', out=SKB[:, f, :], in0=E.ones[:, 0:128], scalar1=ngsk[:, 1, f:f + 1], R=[d_ngsk, E.d_ones], W=[d_NS])
        with ExitStack() as es2:
            cs = Caster(E, es2)
            cs.cast3(Wo, d_Wo, A['ml_w_out'], 0, D, nk_total=16)
            kb.barrier()
        d_Wo.const = True; d_NS.const = True
        C32 = E.sb([128, 16, 512], F32, "C32", es=es); d_C32 = Dep()
        Cbf = E.sb([128, 16, 512], BF16, "Cbf", es=es); d_Cbf = Dep()
        n32 = E.sb([128, 16], F32, "n32", es=es); nbf = E.sb([128, 16], BF16, "nbf", es=es); d_n32 = Dep(); d_nbf = Dep()
        qT = Ring(E, [128, 16, 128], BF16, 1, "mqT", es=es); kT = Ring(E, [128, 16, 128], BF16, 1, "mkT", es=es)
        ktk = Ring(E, [128, 2048], BF16, 1, "mktk", es=es); vtk = Ring(E, [128, 2048], BF16, 1, "mvtk", es=es)
        qd = Ring(E, [128, 16, 128], BF16, 1, "mqd", es=es); kw = Ring(E, [128, 2048], BF16, 1, "mkw", es=es)
        sm = Ring(E, [128, 64], F32, 1, "msm", es=es)
        R1 = Ring(E, [128, 512], F32, 1, "mR1", es=es); EB = Ring(E, [128, 512], F32, 1, "mEB", es=es); Dm = Ring(E, [128, 512], F32, 1, "mDm", es=es)
        sT = Ring(E, [128, 512], BF16, 1, "msT", es=es)
        hdir = Ring(E, [128, 2048], F32, 1, "mhdir", es=es); hfl = Ring(E, [128, 2048], F32, 1, "mhfl", es=es)
        hnb = Ring(E, [128, 2048], BF16, 1, "mhnb", es=es); fin = Ring(E, [128, 16, 128], BF16, 1, "mfin", es=es)
        xcl = Ring(E, [128, 16, 128], BF16, 1, "mxcl", es=es); szl = Ring(E, [128, 16, 128], BF16, 1, "mszl", es=es)
        xs = Ring(E, [128, D], F32, 1, "mx2", es=es)
        seqt = [(0, 0), (0, 1)] + [(1, n) for n in range(32)]
        for direction in (0, 1):
            order = list(range(NTT)) if direction == 0 else [1, 0] + list(range(NTT - 1, 1, -1))
            U = cm[:, 0 + 2 * direction, :]; Lm = cm[:, 1 + 2 * direction, :]; mask = cm[:, 4 + direction, :]; negones = cm[:, 6, :]
            kb.I('vector', 'memset', C32[:], 0.0, W=[d_C32]); kb.I('vector', 'memset', Cbf[:], 0.0, W=[d_Cbf])
            kb.I('vector', 'memset', n32[:], 0.0, W=[d_n32]); kb.I('vector', 'memset', nbf[:], 0.0, W=[d_nbf])
            zb = direction * 8
            for ti in order:
                seg, n = seqt[ti]
                tsl = slice(ti * 128, (ti + 1) * 128)
                q_, d_q = qT.next(); k_, d_k = kT.next(); kt_, d_kt = ktk.next(); vt_, d_vt = vtk.next()
                for f0 in (0, 8):
                    kb.dma(q_[:, f0:f0 + 8, :], qT_d.rearrange("f p l -> p f l")[:, f0:f0 + 8, tsl], R=[d_qT], W=[d_q])
                for f0 in (0, 8):
                    kb.dma(k_[:, f0:f0 + 8, :], kT_d.rearrange("f p l -> p f l")[:, f0:f0 + 8, tsl], R=[d_kT], W=[d_k])
                kb.dma(kt_[:], ktok_d[ti], R=[d_ktok], W=[d_kt])
                kb.dma(vt_[:], vtok_d[ti], R=[d_vtok], W=[d_vt])
                s_, d_s = sm.next()
                kb.I('tensor', 'matmul', out=pC[:, 0:16], lhsT=gates[0:16, tsl], rhs=E.idf[0:16, 0:16], start=True, stop=False,
                     R=[d_gates, E.d_idf], W=[d_pC])
                kb.I('tensor', 'matmul', out=pC[:, 0:16], lhsT=E.ones[0:1, 0:128], rhs=bgr[0:1, :], start=False, stop=True,
                     R=[d_bgr, E.d_ones], W=[d_pC])
                kb.I('vector', 'tensor_copy', out=s_[:, 0:4], in_=pC[:, zb:zb + 4], R=[d_pC], W=[d_s])
                kb.I('scalar', 'activation', out=s_[:, 4:8], in_=pC[:, zb + 4:zb + 8], func=AF.Exp, scale=-1.0, R=[d_pC], W=[d_s])
                kb.I('scalar', 'activation', out=s_[:, 8:12], in_=s_[:, 4:8], func=AF.Ln, bias=onec[:, 0:1], scale=1.0, R=[d_s, d_onec], W=[d_s])
                lsp = s_[:, 8:12]
                kb.I('tensor', 'matmul', out=pC[:, 16:20], lhsT=U, rhs=lsp, start=True, stop=True, R=[d_s, d_cm], W=[d_pC])
                kb.I('tensor', 'matmul', out=pC[:, 20:24], lhsT=Lm, rhs=lsp, start=True, stop=True, R=[d_s, d_cm], W=[d_pC])
                kb.I('tensor', 'matmul', out=pC[:, 24:28], lhsT=negones, rhs=lsp, start=True, stop=True, R=[d_s, d_cm], W=[d_pC])
                kb.I('vector', 'tensor_tensor', out=s_[:, 12:16], in0=s_[:, 0:4], in1=pC[:, 16:20], op=ALU.subtract, R=[d_s, d_pC], W=[d_s])
                kb.I('vector', 'tensor_tensor', out=s_[:, 16:20], in0=pC[:, 20:24], in1=s_[:, 0:4], op=ALU.add, R=[d_s, d_pC], W=[d_s])
                kb.I('scalar', 'activation', out=s_[:, 16:20], in_=s_[:, 16:20], func=AF.Exp, R=[d_s], W=[d_s])
                kb.I('scalar', 'activation', out=s_[:, 20:24], in_=pC[:, 24:28], func=AF.Exp, R=[d_pC], W=[d_s])
                r1, d_r1 = R1.next()
                for h in range(4):
                    kb.I('vector', 'tensor_scalar_mul', out=r1[:, h * 128:(h + 1) * 128], in0=U, scalar1=s_[:, 8 + h:9 + h], R=[d_s, d_cm], W=[d_r1])
                kb.I('tensor', 'matmul', out=pB[:], lhsT=E.ones[:, 0:128], rhs=r1[:], start=True, stop=True, R=[d_r1, E.d_ones], W=[d_pB])
                eb, d_eb = EB.next(); dmt, d_dm = Dm.next()
                kb.I('scalar', 'activation', out=eb[:], in_=pB[:], func=AF.Exp, R=[d_pB], W=[d_eb])
                for h in range(4):
                    kb.I('scalar', 'activation', out=dmt[:, h * 128:(h + 1) * 128], in_=pB[:, h * 128:(h + 1) * 128], func=AF.Exp,
                         bias=s_[:, 12 + h:13 + h], scale=1.0, R=[d_pB, d_s], W=[d_dm])
                    kb.I('gpsimd', 'tensor_tensor', out=dmt[:, h * 128:(h + 1) * 128], in0=dmt[:, h * 128:(h + 1) * 128], in1=mask, op=ALU.mult,
                         R=[d_dm, d_cm], W=[d_dm])
                for h in range(4):
                    for c in range(4):
                        kb.I('tensor', 'matmul', out=pA[:, h * 128:(h + 1) * 128], lhsT=k_[:, 4 * h + c, :], rhs=q_[:, 4 * h + c, :],
                             start=(c == 0), stop=(c == 3), R=[d_k, d_q], W=[d_pA])
                st_, d_st_ = sT.next()
                kb.I('vector', 'tensor_tensor', out=st_[:], in0=pA[:], in1=dmt[:], op=ALU.mult, R=[d_pA, d_dm], W=[d_st_])
                qd_, d_qd = qd.next()
                for f in range(16):
                    h = f // 4
                    kb.I('vector' if f % 2 == 0 else 'gpsimd', 'tensor_tensor', out=qd_[:, f, :], in0=q_[:, f, :], in1=eb[:, h * 128:(h + 1) * 128],
                         op=ALU.mult, R=[d_q, d_eb], W=[d_qd])
                kw_, d_kw = kw.next()
                for h in range(4):
                    kb.I('vector', 'tensor_scalar_mul', out=kw_[:, h * 512:(h + 1) * 512], in0=kt_[:, h * 512:(h + 1) * 512],
                         scalar1=s_[:, 16 + h:17 + h], R=[d_kt, d_s], W=[d_kw])
                for h in range(4):
                    kb.I('tensor', 'matmul', out=p4[:, h * 512:(h + 1) * 512], lhsT=st_[:, h * 128:(h + 1) * 128], rhs=vt_[:, h * 512:(h + 1) * 512],
                         start=True, stop=False, R=[d_st_, d_vt], W=[d_p4])
                    for c in range(4):
                        kb.I('tensor', 'matmul', out=p4[:, h * 512:(h + 1) * 512], lhsT=qd_[:, 4 * h + c, :], rhs=Cbf[:, 4 * h + c, :],
                             start=False, stop=(c == 3), R=[d_qd, d_Cbf], W=[d_p4])
                    kb.I('tensor', 'matmul', out=pC[:, 32 + h:33 + h], lhsT=st_[:, h * 128:(h + 1) * 128], rhs=E.onesb[:, 0:1],
                         start=True, stop=False, R=[d_st_, E.d_onesb], W=[d_pC])
                    for c in range(4):
                        kb.I('tensor', 'matmul', out=pC[:, 32 + h:33 + h], lhsT=qd_[:, 4 * h + c, :], rhs=nbf[:, 4 * h + c:4 * h + c + 1],
                             start=False, stop=(c == 3), R=[d_qd, d_nbf], W=[d_pC])
                kb.I('scalar', 'activation', out=s_[:, 24:28], in_=pC[:, 32:36], func=AF.Abs, R=[d_pC], W=[d_s])
                kb.I('vector', 'tensor_tensor', out=s_[:, 24:28], in0=s_[:, 24:28], in1=c22[:], op=ALU.max, R=[d_s, d_c22], W=[d_s])
                kb.I('vector', 'reciprocal', out=s_[:, 24:28], in_=s_[:, 24:28], R=[d_s], W=[d_s])
                hd_, d_hd = hdir.next()
                for h in range(4):
                    if h % 2 == 0:
                        kb.I('vector', 'tensor_scalar_mul', out=hd_[:, h * 512:(h + 1) * 512], in0=p4[:, h * 512:(h + 1) * 512],
                             scalar1=s_[:, 24 + h:25 + h], R=[d_p4, d_s], W=[d_hd])
                    else:
                        kb.I('scalar', 'mul', out=hd_[:, h * 512:(h + 1) * 512], in_=p4[:, h * 512:(h + 1) * 512], mul=s_[:, 24 + h:25 + h],
                             R=[d_p4, d_s], W=[d_hd])
                for h in range(4):
                    for c in range(4):
                        f = 4 * h + c
                        kb.I('tensor', 'matmul', out=p4[:, c * 512:(c + 1) * 512], lhsT=kw_[:, f * 128:(f + 1) * 128], rhs=vt_[:, h * 512:(h + 1) * 512],
                             start=True, stop=True, R=[d_kw, d_vt], W=[d_p4])
                        kb.I('tensor', 'matmul', out=pC[:, 48 + f:49 + f], lhsT=kw_[:, f * 128:(f + 1) * 128], rhs=E.onesb[:, 0:1],
                             start=True, stop=True, R=[d_kw, E.d_onesb], W=[d_pC])
                    for c in range(4):
                        f = 4 * h + c
                        kb.I('vector', 'scalar_tensor_tensor', out=C32[:, f, :], in0=C32[:, f, :], scalar=s_[:, 20 + h:21 + h],
                             in1=p4[:, c * 512:(c + 1) * 512], op0=ALU.mult, op1=ALU.add, R=[d_C32, d_s, d_p4], W=[d_C32])
                    kb.I('scalar', 'copy', out=Cbf[:, 4 * h:4 * h + 4, :], in_=C32[:, 4 * h:4 * h + 4, :], R=[d_C32], W=[d_Cbf])
                    kb.I('vector', 'scalar_tensor_tensor', out=n32[:, 4 * h:4 * h + 4], in0=n32[:, 4 * h:4 * h + 4], scalar=s_[:, 20 + h:21 + h],
                         in1=pC[:, 48 + 4 * h:52 + 4 * h], op0=ALU.mult, op1=ALU.add, R=[d_n32, d_s, d_pC], W=[d_n32])
                kb.I('vector', 'tensor_copy', out=nbf[:], in_=n32[:], R=[d_n32], W=[d_nbf])
                if direction == 0:
                    kb.dma(hf_d[ti], hd_[:], R=[d_hd], W=[d_hf[ti]])
                    continue
                hf_, d_hf_ = hfl.next()
                kb.dma(hf_[:], hf_d[ti], R=[d_hf[ti]], W=[d_hf_])
                kb.I('gpsimd', 'tensor_tensor', out=hd_[:], in0=hd_[:], in1=hf_[:], op=ALU.add, R=[d_hd, d_hf_], W=[d_hd])
                hn_, d_hn = hnb.next()
                for h in range(4):
                    hs = slice(h * 512, (h + 1) * 512)
                    kb.I('scalar', 'activation', out=hf_[:, hs], in_=hd_[:, hs], func=AF.Identity, accum_out=s_[:, 28 + h:29 + h],
                         R=[d_hd], W=[d_hf_, d_s])
                kb.I('scalar', 'mul', out=s_[:, 28:32], in_=s_[:, 28:32], mul=-1.0 / 512, R=[d_s], W=[d_s])
                for h in range(4):
                    hs = slice(h * 512, (h + 1) * 512)
                    kb.I('scalar', 'activation', out=hd_[:, hs], in_=hd_[:, hs], func=AF.Identity, bias=s_[:, 28 + h:29 + h], scale=1.0,
                         R=[d_hd, d_s], W=[d_hd])
                    kb.I('scalar', 'activation', out=hf_[:, hs], in_=hd_[:, hs], func=AF.Square, accum_out=s_[:, 32 + h:33 + h],
                         R=[d_hd], W=[d_hf_, d_s])
                kb.I('scalar', 'activation', out=s_[:, 36:40], in_=s_[:, 32:36], func=AF.Sqrt, bias=E.epsc[:, 0:1], scale=1.0 / 512,
                     R=[d_s, E.d_eps], W=[d_s])
                kb.I('vector', 'reciprocal', out=s_[:, 36:40], in_=s_[:, 36:40], R=[d_s], W=[d_s])
                for h in range(4):
                    hs = slice(h * 512, (h + 1) * 512)
                    kb.I('vector', 'tensor_scalar_mul', out=hn_[:, hs], in0=hd_[:, hs], scalar1=s_[:, 36 + h:37 + h], R=[d_hd, d_s], W=[d_hn])
                xc_, d_xc = xcl.next(); sz_, d_sz = szl.next()
                for f0 in (0, 8):
                    kb.dma(xc_[:, f0:f0 + 8, :], xcT_d.rearrange("f p l -> p f l")[:, f0:f0 + 8, tsl], R=[d_xcT], W=[d_xc])
                for f0 in (0, 8):
                    kb.dma(sz_[:, f0:f0 + 8, :], szT_d.rearrange("f p l -> p f l")[:, f0:f0 + 8, tsl], R=[d_szT], W=[d_sz])
                ff_ = hf_[:].rearrange("p (f c) -> p f c", f=16); d_ff = d_hf_; fi_, d_fi = fin.next()
                for rnd in range(2):
                    for k in range(8):
                        f = rnd * 8 + k
                        kb.I('tensor', 'transpose', out=nt.pT[0][:, k, :], in_=hn_[:, f * 128:(f + 1) * 128], identity=E.idb[:],
                             R=[d_hn, E.d_idb], W=[nt.d_pT[0]])
                    kb.I('vector', 'tensor_tensor', out=ff_[:, rnd * 8:(rnd + 1) * 8, :], in0=nt.pT[0][:], in1=NGB[:, rnd * 8:(rnd + 1) * 8, :],
                         op=ALU.mult, R=[nt.d_pT[0], d_NS], W=[d_ff])
                kb.I('gpsimd', 'tensor_tensor', out=fi_[:], in0=xc_[:], in1=SKB[:], op=ALU.mult, R=[d_xc, d_NS], W=[d_fi])
                kb.I('vector', 'tensor_tensor', out=ff_[:], in0=ff_[:], in1=fi_[:], op=ALU.add, R=[d_ff, d_fi], W=[d_ff])
                kb.I('vector', 'tensor_tensor', out=fi_[:], in0=ff_[:], in1=sz_[:], op=ALU.mult, R=[d_ff, d_sz], W=[d_fi])
                x, d_x = xs.next()
                load_tile(E, x, d_x, A, seg, n)
                for half in range(2):
                    for f in range(16):
                        kb.I('tensor', 'matmul', out=p4[:, half * 512:(half + 1) * 512], lhsT=fi_[:, f, :], rhs=Wo[:, f, half * 512:(half + 1) * 512],
                             start=(f == 0), stop=(f == 15), R=[d_fi, d_Wo], W=[d_p4])
                post.run(p4[:, 0:1024], d_p4, x[:], d_x, GGc if seg == 0 else GGl, (dc if seg == 0 else dl)[2])
                store_tile(E, x, d_x, A, seg, n)
        kb.barrier()


def cmul(kb, outr, outi, ar, ai, br, bi, t1, t2, R, d_out, d_t):
    kb.I('vector', 'tensor_tensor', out=t1, in0=ar, in1=br, op=ALU.mult, R=R, W=[d_t])
    kb.I('vector', 'tensor_tensor', out=t2, in0=ai, in1=bi, op=ALU.mult, R=R, W=[d_t])
    kb.I('gpsimd', 'tensor_tensor', out=outr, in0=t1, in1=t2, op=ALU.subtract, R=[d_t], W=[d_out])
    kb.I('vector', 'tensor_tensor', out=t1, in0=ar, in1=bi, op=ALU.mult, R=R, W=[d_t])
    kb.I('vector', 'tensor_tensor', out=t2, in0=ai, in1=br, op=ALU.mult, R=R, W=[d_t])
    kb.I('gpsimd', 'tensor_tensor', out=outi, in0=t1, in1=t2, op=ALU.add, R=[d_t], W=[d_out])


def s5_layer(E, A):
    kb = E.kb
    NTT = 34
    PI = math.pi
    Pl = alloc_mod(E); Pc = alloc_mod(E)
    post = PostRes(E, nbuf=1)
    pU = E.ps([128, 8, 128], F32, "pU"); d_pU = PDep()
    pBr = E.ps([128, 512], F32, "pBr"); d_pBr = PDep()
    pBi = E.ps([128, 512], F32, "pBi"); d_pBi = PDep()
    pBr2 = E.ps([128, 512], F32, "pBr2"); d_pBr2 = PDep()
    pBi2 = E.ps([128, 512], F32, "pBi2"); d_pBi2 = PDep()
    pY = E.ps([128, 1024], F32, "pY"); d_pY = PDep()
    E.n += 1
    u = E.n
    yf_d = E.dram(f"syf_{u}", [NTT, 128, 1024], F32); gT_d = E.dram(f"sgT_{u}", [NTT, 128, 1024], BF16)
    d_yf = [Dep() for _ in range(NTT)]; d_gT = [Dep() for _ in range(NTT)]
    with ExitStack() as es:
        (Gl, Sl, GGl, dl), (Gc, Sc, GGc, dc) = compute_mod2(E, Pl, Pc, A['c'], A['cc'], A['mod_w'], A['mod_b'], A['norm_g'], 0, es, pBr, d_pBr, pBi, d_pBi)
        kb.barrier()
    for dd in list(dl) + list(dc):
        dd.const = True
    seqt = [(0, 0), (0, 1)] + [(1, n) for n in range(32)]

    with ExitStack() as es:
        Bb = E.sb([128, 2, 32, 128], BF16, "sBb", es=es); d_Bb = Dep()
        Cc = E.sb([128, 2, 32, 128], BF16, "sCc", es=es); d_Cc = Dep()
        RJ = E.sb([128, 2, 128], F32, "sRJ", es=es); RJb = E.sb([128, 2, 128], BF16, "sRJb", es=es); d_RJ = Dep()
        dcol = E.sb([128, 8], F32, "sdcol", es=es); d_dcol = Dep()
        cst = E.sb([128, 4], F32, "scst", es=es); d_cst = Dep()
        on128 = E.ones[:, 0:128]
        kb.dma(RJ[:, 0, :], A['ident'], W=[d_RJ]); kb.dma(RJ[:, 1, :], A['s5_J'], W=[d_RJ])
        kb.I('vector', 'tensor_copy', out=RJb[:], in_=RJ[:], R=[d_RJ], W=[d_RJ])
        kb.dma(dcol[:], A['s5_dcol'], W=[d_dcol])
        kb.I('vector', 'memset', cst[:, 0:1], 1.0, W=[d_cst]); kb.I('vector', 'memset', cst[:, 1:2], PI / 2, W=[d_cst])
        with ExitStack() as es2:
            stg = Ring(E, [128, 2048], F32, 2, "sstg", es=es2)
            for ri, nm in enumerate(('s5_Bre', 's5_Bim')):
                for q0 in range(0, 32, 16):
                    st, d_st = stg.next()
                    kb.dma(st[:].rearrange("p (q c) -> p q c", q=16), A[nm][q0:q0 + 16].rearrange("q p c -> p q c"), W=[d_st])
                    kb.I('vector', 'tensor_copy', out=Bb[:, ri, q0:q0 + 16, :], in_=st[:].rearrange("p (q c) -> p q c", q=16), R=[d_st], W=[d_Bb])
            for ri, nm in enumerate(('s5_Cre', 's5_Cim')):
                for q0 in range(0, 32, 16):
                    st, d_st = stg.next()
                    kb.dma(st[:].rearrange("p (q c) -> p q c", q=16), A[nm][q0:q0 + 16].rearrange("q p c -> p q c"), W=[d_st])
                    kb.I('scalar', 'mul', out=Cc[:, ri, q0:q0 + 16, :], in_=st[:].rearrange("p (q c) -> p q c", q=16), mul=(1.0 if ri == 0 else -1.0),
                         R=[d_st], W=[d_Cc])
            kb.barrier()
        d_Bb.const = True; d_Cc.const = True; d_RJ.const = True; d_dcol.const = True; d_cst.const = True
        LP = E.sb([128, 2, 32, 128], F32, "sLP", es=es); LN = E.sb([128, 2, 32, 128], F32, "sLN", es=es); d_LP = Dep(); d_LN = Dep()
        X = E.sb([128, 2, 32, 128], F32, "sX", es=es); d_X = [[Dep() for _ in range(8)] for _ in range(2)]
        pr = E.sb([128, 24, 32], F32, "spr", es=es); d_pr = Dep()
        SET = []
        for _s in range(2):
            SET.append(dict(
                Br=E.sb([128, 512], BF16, "sBr", es=es), Bi=E.sb([128, 512], BF16, "sBi", es=es),
                A1=E.sb([128, 512], F32, "sA1", es=es), A2=E.sb([128, 512], F32, "sA2", es=es),
                P1=E.sb([128, 512], F32, "sP1", es=es), P2=E.sb([128, 512], F32, "sP2", es=es),
                Zr=E.sb([128, 512], F32, "sZr", es=es), Zi=E.sb([128, 512], F32, "sZi", es=es),
                d=dict((k, Dep()) for k in ('Br', 'Bi', 'A1', 'A2', 'P1', 'P2', 'Zr', 'Zi'))))
        wk = [SET[0]['A1'], SET[0]['A2']]; d_wk = [SET[0]['d']['A1'], SET[0]['d']['A2']]
        Xb = Ring(E, [128, 2, 4, 128], BF16, 2, "sXb", es=es)
        rmask = E.sb([128, 512], F32, "srmask", es=es); d_rmask = Dep()
        kb.I('vector', 'memset', rmask[:], 1.0, W=[d_rmask])
        kb.I('vector', 'memset', rmask[:].rearrange("p (q t) -> p q t", q=4)[:, :, 0:1], 0.0, W=[d_rmask])
        d_rmask.const = True
        xs = Ring(E, [128, D], F32, 1, "sx", es=es)
        ss = E.sb([128, 2], F32, "sss", es=es); d_ss = Dep()
        xb = E.sb([128, D], BF16, "sxb", es=es); d_xb = Dep()
        uT = E.sb([128, 8, 128], BF16, "suT", es=es); d_uT = Dep()
        ysb = E.sb([128, D], F32, "sysb", es=es); d_ysb = Dep()
        yfs = E.sb([128, 8, 128], F32, "syfs", es=es); d_yfs = Dep()
        yfl = E.sb([128, 8, 128], F32, "syfl", es=es); d_yfl = Dep()
        g1 = E.sb([128, D], F32, "sg1", es=es); d_g1 = Dep()
        sq = g1; d_sq = d_g1
        gTo = xb; d_gTo = d_xb

        def P(i):
            return pr[:, i, :]

        for direction in (0, 1):
            kb.dma(P(0), A['s5_lreT'][direction], W=[d_pr]); kb.dma(P(1), A['s5_limT'][direction], W=[d_pr])
            kb.dma(P(2), A['s5_ldtT'][direction], W=[d_pr])
            I = lambda eng, name, **kw: kb.I(eng, name, R=[d_pr, d_cst], W=[d_pr], **kw)
            I('scalar', 'activation', out=P(2), in_=P(2), func=AF.Exp)
            I('vector', 'tensor_tensor', out=P(3), in0=P(0), in1=P(2), op=ALU.mult)
            I('vector', 'tensor_tensor', out=P(4), in0=P(1), in1=P(2), op=ALU.mult)
            I('scalar', 'activation', out=P(5), in_=P(3), func=AF.Exp)
            I('scalar', 'activation', out=P(6), in_=P(3), func=AF.Exp, scale=-1.0)
            I('vector', 'tensor_scalar', out=P(7), in0=P(4), scalar1=PI, scalar2=1.0, op0=ALU.is_ge, op1=ALU.mult)
            for m in (3, 5, 7):
                I('vector', 'tensor_scalar', out=P(8), in0=P(4), scalar1=m * PI, scalar2=1.0, op0=ALU.is_ge, op1=ALU.mult)
                I('vector', 'tensor_tensor', out=P(7), in0=P(7), in1=P(8), op=ALU.add)
            I('vector', 'scalar_tensor_tensor', out=P(8), in0=P(7), scalar=-6.28125, in1=P(4), op0=ALU.mult, op1=ALU.add)
            I('vector', 'scalar_tensor_tensor', out=P(8), in0=P(7), scalar=-(2 * PI - 6.28125), in1=P(8), op0=ALU.mult, op1=ALU.add)
            I('scalar', 'activation', out=P(9), in_=P(8), func=AF.Sin)
            I('scalar', 'activation', out=P(10), in_=P(8), func=AF.Abs)
            I('scalar', 'activation', out=P(10), in_=P(10), func=AF.Sin, scale=-1.0, bias=cst[:, 1:2])
            I('vector', 'tensor_tensor', out=P(11), in0=P(5), in1=P(10), op=ALU.mult)
            I('vector', 'tensor_tensor', out=P(12), in0=P(5), in1=P(9), op=ALU.mult)
            I('vector', 'tensor_tensor', out=P(13), in0=P(6), in1=P(10), op=ALU.mult)
            I('vector', 'scalar_tensor_tensor', out=P(14), in0=P(6), scalar=-1.0, in1=P(9), op0=ALU.mult, op1=ALU.mult)
            I('vector', 'tensor_scalar', out=P(15), in0=P(11), scalar1=-1.0, scalar2=1.0, op0=ALU.add, op1=ALU.mult)
            I('vector', 'tensor_tensor', out=P(16), in0=P(0), in1=P(0), op=ALU.mult)
            I('vector', 'tensor_tensor', out=P(17), in0=P(1), in1=P(1), op=ALU.mult)
            I('vector', 'tensor_tensor', out=P(16), in0=P(16), in1=P(17), op=ALU.add)
            I('vector', 'reciprocal', out=P(16), in_=P(16))
            I('vector', 'tensor_tensor', out=P(17), in0=P(15), in1=P(0), op=ALU.mult)
            I('vector', 'tensor_tensor', out=P(18), in0=P(12), in1=P(1), op=ALU.mult)
            I('vector', 'tensor_tensor', out=P(17), in0=P(17), in1=P(18), op=ALU.add)
            I('vector', 'tensor_tensor', out=P(17), in0=P(17), in1=P(16), op=ALU.mult)
            I('vector', 'tensor_tensor', out=P(18), in0=P(12), in1=P(0), op=ALU.mult)
            I('vector', 'tensor_tensor', out=P(19), in0=P(15), in1=P(1), op=ALU.mult)
            I('vector', 'tensor_tensor', out=P(18), in0=P(18), in1=P(19), op=ALU.subtract)
            I('vector', 'tensor_tensor', out=P(18), in0=P(18), in1=P(16), op=ALU.mult)
            cmul(kb, P(19), P(20), P(17), P(18), P(13), P(14), P(21), P(22), [d_pr], d_pr, d_pr)
            for (tbl, d_tbl, b_r, b_i, m_r, m_i) in ((LP, d_LP, 11, 12, 11, 12), (LN, d_LN, 19, 20, 13, 14)):
                kb.I('vector', 'tensor_copy', out=tbl[:, 0, :, 0], in_=P(b_r), R=[d_pr], W=[d_tbl])
                kb.I('vector', 'tensor_copy', out=tbl[:, 1, :, 0], in_=P(b_i), R=[d_pr], W=[d_tbl])
                kb.I('vector', 'tensor_copy', out=P(15), in_=P(m_r), R=[d_pr], W=[d_pr])
                kb.I('vector', 'tensor_copy', out=P(16), in_=P(m_i), R=[d_pr], W=[d_pr])
                n = 1
                while n < 128:
                    shp = [128, 32, n]
                    t1 = wk[0][:, 0:32 * n].rearrange("p (q t) -> p q t", q=32) if 32 * n <= 512 else g1[:, 0:32 * n // 4].rearrange("p (q t) -> p q t", q=32) if False else None
                    t1 = sq[:, 0:1024].rearrange("p (q t) -> p q t", q=32)[:, :, 0:min(n, 32)] if n <= 32 else None
                    for c0 in range(0, n, 32):
                        cn = min(32, n - c0)
                        ta = sq[:].rearrange("p (q t) -> p q t", q=32)[:, :, 0:cn]
                        tb_ = ysb[:].rearrange("p (q t) -> p q t", q=32)[:, :, 0:cn]
                        src_r = tbl[:, 0, :, c0:c0 + cn]; src_i = tbl[:, 1, :, c0:c0 + cn]
                        pwr = pr[:, 15, :].unsqueeze(2).to_broadcast([128, 32, cn]); pwi = pr[:, 16, :].unsqueeze(2).to_broadcast([128, 32, cn])
                        kb.I('vector', 'tensor_tensor', out=ta, in0=src_r, in1=pwr, op=ALU.mult, R=[d_tbl, d_pr], W=[d_sq])
                        kb.I('vector', 'tensor_tensor', out=tb_, in0=src_i, in1=pwi, op=ALU.mult, R=[d_tbl, d_pr], W=[d_ysb])
                        kb.I('gpsimd', 'tensor_tensor', out=tbl[:, 0, :, n + c0:n + c0 + cn], in0=ta, in1=tb_, op=ALU.subtract, R=[d_sq, d_ysb], W=[d_tbl])
                        kb.I('vector', 'tensor_tensor', out=ta, in0=src_r, in1=pwi, op=ALU.mult, R=[d_tbl, d_pr], W=[d_sq])
                        kb.I('vector', 'tensor_tensor', out=tb_, in0=src_i, in1=pwr, op=ALU.mult, R=[d_tbl, d_pr], W=[d_ysb])
                        kb.I('gpsimd', 'tensor_tensor', out=tbl[:, 1, :, n + c0:n + c0 + cn], in0=ta, in1=tb_, op=ALU.add, R=[d_sq, d_ysb], W=[d_tbl])
                    cmul(kb, P(21), P(22), P(15), P(16), P(15), P(16), P(23), P(8), [d_pr], d_pr, d_pr)
                    kb.I('vector', 'tensor_copy', out=P(15), in_=P(21), R=[d_pr], W=[d_pr])
                    kb.I('vector', 'tensor_copy', out=P(16), in_=P(22), R=[d_pr], W=[d_pr])
                    n *= 2
            for f in range(8):
                kb.I('vector', 'memset', X[:, :, 4 * f:4 * f + 4, 127:128], 0.0, W=[d_X[0][f], d_X[1][f]])
            order = list(range(NTT)) if direction == 0 else [1, 0] + list(range(NTT - 1, 1, -1))
            for ti in order:
                seg, n_ = seqt[ti]
                G, S_, dm = (Gc, Sc, dc) if seg == 0 else (Gl, Sl, dl)
                x, d_x = xs.next()
                load_tile(E, x, d_x, A, seg, n_)
                kb.I('scalar', 'activation', out=sq[:], in_=x[:], func=AF.Square, accum_out=ss[:, 0:1], R=[d_x], W=[d_sq, d_ss])
                kb.I('scalar', 'activation', out=ss[:, 1:2], in_=ss[:, 0:1], func=AF.Sqrt, bias=E.epsc[:, 0:1], scale=1.0 / D, R=[d_ss, E.d_eps], W=[d_ss])
                kb.I('vector', 'reciprocal', out=ss[:, 1:2], in_=ss[:, 1:2], R=[d_ss], W=[d_ss])
                kb.I('vector', 'tensor_scalar_mul', out=xb[:], in0=x[:], scalar1=ss[:, 1:2], R=[d_x, d_ss], W=[d_xb])
                for k in range(8):
                    kb.I('tensor', 'matmul', out=pU[:, k, :], lhsT=xb[:, k * 128:(k + 1) * 128], rhs=RJb[:, direction, :], start=True, stop=True,
                         R=[d_xb, d_RJ], W=[d_pU])
                for k in range(8):
                    kb.I('scalar', 'activation', out=uT[:, k, :], in_=pU[:, k, :], func=AF.Identity, bias=S_[:, k:k + 1], scale=G[:, k:k + 1],
                         R=[d_pU] + list(dm[:2]), W=[d_uT])
                v4 = lambda t: t[:].rearrange("p (q t) -> p q t", q=4)
                PSS = ((pBr, d_pBr, pBi, d_pBi), (pBr2, d_pBr2, pBi2, d_pBi2))
                for j in range(4):
                    for s_ in range(2):
                        fc = 2 * j + s_; S = SET[s_]; dS = S['d']; pr_, d_pr_, pi_, d_pi_ = PSS[s_]
                        for pi in range(4):
                            q = 4 * fc + pi
                            kb.I('tensor', 'matmul', out=pr_[:, pi * 128:(pi + 1) * 128], lhsT=Bb[:, 0, q, :], rhs=uT[:, fc, :], start=True, stop=True,
                                 R=[d_Bb, d_uT], W=[d_pr_])
                            kb.I('tensor', 'matmul', out=pi_[:, pi * 128:(pi + 1) * 128], lhsT=Bb[:, 1, q, :], rhs=uT[:, fc, :], start=True, stop=True,
                                 R=[d_Bb, d_uT], W=[d_pi_])
                        kb.I('scalar', 'copy', out=S['Br'][:], in_=pr_[:], R=[d_pr_], W=[dS['Br']])
                        kb.I('scalar', 'copy', out=S['Bi'][:], in_=pi_[:], R=[d_pi_], W=[dS['Bi']])
                    for s_ in range(2):
                        fc = 2 * j + s_; S = SET[s_]; dS = S['d']
                        lnr = LN[:, 0, 4 * fc:4 * fc + 4, :]; lni = LN[:, 1, 4 * fc:4 * fc + 4, :]
                        kb.I('vector', 'tensor_tensor', out=v4(S['A1']), in0=v4(S['Br']), in1=lnr, op=ALU.mult, R=[dS['Br'], d_LN], W=[dS['A1']])
                        kb.I('vector', 'tensor_tensor', out=v4(S['A2']), in0=v4(S['Bi']), in1=lni, op=ALU.mult, R=[dS['Bi'], d_LN], W=[dS['A2']])
                        kb.I('vector', 'tensor_tensor', out=S['A1'][:], in0=S['A1'][:], in1=S['A2'][:], op=ALU.subtract, R=[dS['A2']], W=[dS['A1']])
                        kb.I('gpsimd', 'tensor_tensor', out=v4(S['P1']), in0=v4(S['Br']), in1=lni, op=ALU.mult, R=[dS['Br'], d_LN], W=[dS['P1']])
                        kb.I('gpsimd', 'tensor_tensor', out=v4(S['P2']), in0=v4(S['Bi']), in1=lnr, op=ALU.mult, R=[dS['Bi'], d_LN], W=[dS['P2']])
                        kb.I('gpsimd', 'tensor_tensor', out=S['P1'][:], in0=S['P1'][:], in1=S['P2'][:], op=ALU.add, R=[dS['P2']], W=[dS['P1']])
                    for s_ in range(2):
                        fc = 2 * j + s_; S = SET[s_]; dS = S['d']
                        kb.I('vector', 'tensor_tensor', out=v4(S['A1'])[:, :, 0], in0=v4(S['A1'])[:, :, 0], in1=X[:, 0, 4 * fc:4 * fc + 4, 127], op=ALU.add,
                             R=[d_X[0][fc]], W=[dS['A1']])
                        kb.I('gpsimd', 'tensor_tensor', out=v4(S['P1'])[:, :, 0], in0=v4(S['P1'])[:, :, 0], in1=X[:, 1, 4 * fc:4 * fc + 4, 127], op=ALU.add,
                             R=[d_X[1][fc]], W=[dS['P1']])
                    for s_ in range(2):
                        S = SET[s_]; dS = S['d']
                        kb.I('vector', 'tensor_tensor_scan', out=S['Zr'][:], data0=rmask[:], data1=S['A1'][:], initial=0.0, op0=ALU.mult, op1=ALU.add,
                             R=[dS['A1'], d_rmask], W=[dS['Zr']])
                        kb.I('vector', 'tensor_tensor_scan', out=S['Zi'][:], data0=rmask[:], data1=S['P1'][:], initial=0.0, op0=ALU.mult, op1=ALU.add,
                             R=[dS['P1'], d_rmask], W=[dS['Zi']])
                    for s_ in range(2):
                        fc = 2 * j + s_; S = SET[s_]; dS = S['d']
                        lpr = LP[:, 0, 4 * fc:4 * fc + 4, :]; lpi = LP[:, 1, 4 * fc:4 * fc + 4, :]
                        kb.I('vector', 'tensor_tensor', out=v4(S['A1']), in0=v4(S['Zr']), in1=lpr, op=ALU.mult, R=[dS['Zr'], d_LP], W=[dS['A1']])
                        kb.I('vector', 'tensor_tensor', out=v4(S['A2']), in0=v4(S['Zi']), in1=lpi, op=ALU.mult, R=[dS['Zi'], d_LP], W=[dS['A2']])
                        kb.I('vector', 'tensor_tensor', out=X[:, 0, 4 * fc:4 * fc + 4, :], in0=v4(S['A1']), in1=v4(S['A2']), op=ALU.subtract,
                             R=[dS['A1'], dS['A2']], W=[d_X[0][fc]])
                        kb.I('gpsimd', 'tensor_tensor', out=v4(S['P1']), in0=v4(S['Zr']), in1=lpi, op=ALU.mult, R=[dS['Zr'], d_LP], W=[dS['P1']])
                        kb.I('gpsimd', 'tensor_tensor', out=v4(S['P2']), in0=v4(S['Zi']), in1=lpr, op=ALU.mult, R=[dS['Zi'], d_LP], W=[dS['P2']])
                        kb.I('gpsimd', 'tensor_tensor', out=X[:, 1, 4 * fc:4 * fc + 4, :], in0=v4(S['P1']), in1=v4(S['P2']), op=ALU.add,
                             R=[dS['P1'], dS['P2']], W=[d_X[1][fc]])
                    if seg == 1:
                        for s_ in range(2):
                            fc = 2 * j + s_
                            xb_, d_xb_ = Xb.next()
                            for ri in range(2):
                                kb.I('scalar', 'copy', out=xb_[:, ri], in_=X[:, ri, 4 * fc:4 * fc + 4, :], R=[d_X[ri][fc]], W=[d_xb_])
                            for pi in range(4):
                                q = 4 * fc + pi
                                for ri in range(2):
                                    kb.I('tensor', 'matmul', out=pY[:, fc * 128:(fc + 1) * 128], lhsT=xb_[:, ri, pi, :], rhs=Cc[:, ri, q, :],
                                         start=(pi == 0 and ri == 0), stop=(pi == 3 and ri == 1), R=[d_xb_, d_Cc], W=[d_pY])
                if seg == 0:
                    continue
                kb.I('scalar', 'copy', out=ysb[:], in_=pY[:], R=[d_pY], W=[d_ysb])
                for fc in range(8):
                    kb.I('tensor', 'matmul', out=pU[:, fc, :], lhsT=ysb[:, fc * 128:(fc + 1) * 128], rhs=RJ[:, direction, :], start=True, stop=True,
                         R=[d_ysb, d_RJ], W=[d_pU])
                if direction == 0:
                    for fc in range(8):
                        kb.I('vector', 'scalar_tensor_tensor', out=yfs[:, fc, :], in0=uT[:, fc, :], scalar=dcol[:, fc:fc + 1], in1=pU[:, fc, :],
                             op0=ALU.mult, op1=ALU.add, R=[d_uT, d_dcol, d_pU], W=[d_yfs])
                    kb.dma(yf_d[ti], yfs[:].rearrange("p f t -> p (f t)"), R=[d_yfs], W=[d_yf[ti]])
                    continue
                kb.dma(yfl[:].rearrange("p f t -> p (f t)"), yf_d[ti], R=[d_yf[ti]], W=[d_yfl])
                ys = yfs[:].rearrange("p f t -> p (f t)")
                kb.I('vector', 'tensor_tensor', out=ys, in0=pU[:].rearrange("p f t -> p (f t)"), in1=yfl[:].rearrange("p f t -> p (f t)"), op=ALU.add,
                     R=[d_pU, d_yfl], W=[d_yfs])
                kb.I('gpsimd', 'tensor_tensor', out=g1[:], in0=ys, in1=ys, op=ALU.mult, R=[d_yfs], W=[d_g1])
                kb.I('scalar', 'activation', out=g1[:], in_=g1[:], func=AF.Identity, scale=0.044715, bias=cst[:, 0:1], R=[d_g1, d_cst], W=[d_g1])
                kb.I('vector', 'tensor_tensor', out=g1[:], in0=g1[:], in1=ys, op=ALU.mult, R=[d_g1, d_yfs], W=[d_g1])
                kb.I('scalar', 'activation', out=g1[:], in_=g1[:], func=AF.Sigmoid, scale=1.5957691216, R=[d_g1], W=[d_g1])
                kb.I('vector', 'tensor_tensor', out=gTo[:], in0=g1[:], in1=ys, op=ALU.mult, R=[d_g1, d_yfs], W=[d_gTo])
                kb.dma(gT_d[ti], gTo[:], R=[d_gTo], W=[d_gT[ti]])
        kb.barrier()

    with ExitStack() as es:
        Wg = E.sb([128, 8, 2048], BF16, "sWg", es=es); d_Wg = Dep()
        with ExitStack() as es2:
            cs = Caster(E, es2)
            cs.cast3(Wg, d_Wg, A['s5_w_glu'], 0, 2048)
            kb.barrier()
        d_Wg.const = True
        gl = Ring(E, [128, 8, 128], BF16, 2, "sgl", es=es)
        xs = Ring(E, [128, D], F32, 2, "sx2", es=es)
        sg = Ring(E, [128, D], F32, 1, "ssg", es=es); yv = Ring(E, [128, D], F32, 1, "syv", es=es)
        pU2 = pU[:].rearrange("p f t -> p (f t)")
        for ti in range(2, NTT):
            seg, n_ = seqt[ti]
            g_, d_g = gl.next()
            kb.dma(g_[:].rearrange("p f t -> p (f t)"), gT_d[ti], R=[d_gT[ti]], W=[d_g])
            x, d_x = xs.next()
            load_tile(E, x, d_x, A, seg, n_)
            for half in range(2):
                for k in range(8):
                    kb.I('tensor', 'matmul', out=pY[:, half * 512:(half + 1) * 512], lhsT=g_[:, k, :], rhs=Wg[:, k, half * 512:(half + 1) * 512],
                         start=(k == 0), stop=(k == 7), R=[d_g, d_Wg], W=[d_pY])
                for k in range(8):
                    kb.I('tensor', 'matmul', out=pU2[:, half * 512:(half + 1) * 512], lhsT=g_[:, k, :],
                         rhs=Wg[:, k, 1024 + half * 512:1024 + (half + 1) * 512], start=(k == 0), stop=(k == 7), R=[d_g, d_Wg], W=[d_pU])
            s_, d_s = sg.next(); v_, d_v = yv.next()
            kb.I('scalar', 'activation', out=s_[:], in_=pU2, func=AF.Sigmoid, R=[d_pU], W=[d_s])
            kb.I('vector', 'tensor_tensor', out=v_[:], in0=pY[:], in1=s_[:], op=ALU.mult, R=[d_pY, d_s], W=[d_v])
            post.run(v_[:], d_v, x[:], d_x, GGl, dl[2])
            store_tile(E, x, d_x, A, seg, n_)
        kb.barrier()


def gla_consts(c=-1.0 / 16):
    s = np.arange(128)[:, None]; t = np.arange(128)[None, :]
    UF = (s <= t) * c; LF = (s > t) * c; UB = (s >= t) * c; LB = (s < t) * c
    mF = (t >= s) * 1.0; mB = (t <= s) * 1.0
    return np.stack([UF, LF, UB, LB, mF, mB]).astype(np.float32)


def ml_consts():
    return np.concatenate([gla_consts(-1.0), -np.ones((1, 128, 128), np.float32)]).astype(np.float32)


def hy_consts(L):
    T = L // 128; N = 2 * L; NFQ = T + 1
    pos = np.arange(L, dtype=np.float32); t = pos / np.float32(L)
    bands = np.linspace(1e-4, 15, 16, dtype=np.float32)
    ang = (2.0 * math.pi * t[:, None] * bands[None]).astype(np.float32)
    feats = np.concatenate([t[:, None], np.cos(ang), -np.sin(ang)], axis=-1).astype(np.float32)
    tn = (-t).reshape(T, 128).T.copy()
    tt = np.arange(L, dtype=np.int64)[:, None]; kk = np.arange(NFQ * 128, dtype=np.int64)[None, :]
    ph = ((tt * kk) % N).astype(np.float64) * (2 * math.pi / N)
    valid = (kk <= L)
    c1 = np.cos(ph) * valid; s1 = np.sin(ph) * valid

    def lay1(m):
        return m.reshape(T, 128, NFQ, 128).transpose(2, 1, 0, 3).astype(ml_dtypes.bfloat16).copy()
    alpha = np.where((kk == 0) | (kk == L), 1.0, 2.0) * valid / N
    c2 = (np.cos(ph) * alpha).T; s2 = (np.sin(ph) * alpha).T

    def lay2(m):
        return m.reshape(NFQ, 128, L).astype(ml_dtypes.bfloat16).copy()
    return dict(feT=feats.T.copy(), tn=tn.astype(np.float32), T1c=lay1(c1), T1s=lay1(s1), T2c=lay2(c2), T2s=lay2(s2))


def hy_delta_bc():
    lt = math.log(1e-2)
    deltas = np.abs(np.linspace(lt / 1.5, lt / 0.3, 1024, dtype=np.float32))
    return np.broadcast_to(deltas[None], (128, 1024)).astype(np.float32).copy()


def ml_bd_layout(wq, wk, wv):
    out = np.zeros((3, 16, 128, 128), np.float32)
    for j, w in enumerate((wq, wk, wv)):
        w = w.reshape(16, 32, 4, 4)
        for n in range(32):
            out[j, :, n * 4:(n + 1) * 4, n * 4:(n + 1) * 4] = w[:, n]
    return out


def s5_layouts(b_re, b_im, c_re, c_im, lam_re, lam_im, log_dt, dvec):
    out = {}
    for nm, b in (('s5_Bre', b_re), ('s5_Bim', b_im)):
        m = np.zeros((32, 128, 128), np.float32)
        for q in range(32):
            for two in range(2):
                g = 2 * q + two; gl = g % 8
                m[q, gl * 16:(gl + 1) * 16, two * 64:(two + 1) * 64] = b[g].T
        out[nm] = m
    for nm, c in (('s5_Cre', c_re), ('s5_Cim', c_im)):
        m = np.zeros((32, 128, 128), np.float32)
        for q in range(32):
            for two in range(2):
                g = 2 * q + two; gl = g % 8
                m[q, two * 64:(two + 1) * 64, gl * 16:(gl + 1) * 16] = c[g].T
        out[nm] = m

    def lay(a):
        return a.reshape(2, 32, 2, 64).transpose(0, 2, 3, 1).reshape(2, 128, 32).copy()
    out['s5_lreT'] = lay(lam_re); out['s5_limT'] = lay(lam_im)
    out['s5_ldtT'] = lay(np.broadcast_to(log_dt[:, :, None], (2, 64, 64)))
    out['s5_dcol'] = dvec.reshape(8, 128).T.copy()
    out['s5_J'] = np.eye(128, dtype=np.float32)[::-1].copy()
    return out


def _common_inputs(nc):
    IN = lambda n, s, dt=F32: nc.dram_tensor(n, list(s), dt, kind="ExternalInput").ap()
    A = dict(lat=IN("lat", [LAT, D]), cx=IN("cx", [CTXL, D]), c=IN("c", [D]), cc=IN("cc", [D]),
             mod_w=IN("mod_w", [D, 6 * D]), mod_b=IN("mod_b", [6 * D]), norm_g=IN("norm_g", [4, D]), ident=IN("ident", [128, 128]))
    return IN, A


def build_ffn(do_ctx=True):
    E = Env(); nc = E.nc
    IN, A = _common_inputs(nc)
    w_in = IN("w_in", [D, 2 * FH]); w_out = IN("w_out", [FH, D])
    lat_o = nc.dram_tensor("lat_o", [LAT, D], F32, kind="ExternalOutput").ap()
    cx_o = nc.dram_tensor("cx_o", [CTXL, D], F32, kind="ExternalOutput").ap() if do_ctx else None
    setup_consts(E, A['ident'])
    ffn_layer(E, A, w_in, w_out, lat_o, cx_o, do_ctx)
    E.kb.finish()
    return nc


def ffn_layer(E, A, w_in, w_out, lat_o, cx_o, do_ctx):
    Pl = alloc_mod(E); Pc = alloc_mod(E)
    wo = E.sb([128, 22, D], BF16, "wo")
    nt = NormT(E); post = PostRes(E)
    with ExitStack() as es:
        pcol = E.ps([128, 512], F32, 'pcol', es=es); pm = E.ps([128, 512], F32, 'pm', es=es); dpc = PDep(); dpm = PDep()
        (Gl, Sl, GGl, dl), (Gc, Sc, GGc, dc) = compute_mod2(E, Pl, Pc, A['c'], A['cc'], A['mod_w'], A['mod_b'], A['norm_g'], 1, es, pcol, dpc, pm, dpm)
        E.kb.barrier()
    with ExitStack() as es:
        wsc, d_wsc, wo, d_wo = ffn_weights_prep(E, wo, w_in, w_out, es)
        E.kb.barrier()
    groups = []
    lt = A['lat'].rearrange("(n p) d -> n p d", p=128); lo = lat_o.rearrange("(n p) d -> n p d", p=128)
    for g in range(8):
        groups.append(dict(tiles=[(lt[4 * g + j], lo[4 * g + j]) for j in range(4)], G=Gl, S=Sl, GG=GGl, dGS=dl[:2], dGG=dl[2]))
    if do_ctx:
        ct = A['cx'].rearrange("(n p) d -> n p d", p=128); co = cx_o.rearrange("(n p) d -> n p d", p=128)
        groups.append(dict(tiles=[(ct[j], co[j]) for j in range(2)], G=Gc, S=Sc, GG=GGc, dGS=dc[:2], dGG=dc[2]))
    ffn_run(E, groups, wsc, d_wsc, wo, d_wo, nt, post)
    E.kb.barrier()


GLA_IN = dict(gla_w_in=[D, 3104], gla_w_gate=[2, 16, 512], gla_b_gate=[2, 512], gla_norm_g=[256], gla_w_out=[D, D], gla_cm=[6, 128, 128])
HY_IN = dict(hy_w_in=[D, 3 * D], hy_conv_w=[3, 3 * D], hy_conv_b=[3 * D], hy_f_w1=[33, 64], hy_f_b1=[64], hy_f_w2=[64, 64], hy_f_b2=[64],
             hy_f_w3=[64, 2 * D], hy_f_freq=[64], hy_f_bias=[D], hy_w_out=[D, D], hy_delta_bc=[128, D])
ML_IN = dict(ml_w_in=[D, 4096], ml_conv_w=[3, 2048], ml_conv_b=[2048], ml_bd=[3, 16, 128, 128], ml_w_gates=[2, 6144, 8], ml_b_gates=[2, 8],
             ml_norm_g=[2048], ml_skip=[2048], ml_w_out=[2048, D], ml_cm=[7, 128, 128])
S5_IN = dict(s5_Bre=[32, 128, 128], s5_Bim=[32, 128, 128], s5_Cre=[32, 128, 128], s5_Cim=[32, 128, 128], s5_lreT=[2, 128, 32],
             s5_limT=[2, 128, 32], s5_ldtT=[2, 128, 32], s5_dcol=[128, 8], s5_J=[128, 128], s5_w_glu=[D, 2 * D])


def _hy_table_inputs(IN, A):
    for tag, L in (("l", LAT), ("c", CTXL)):
        T = L // 128; NFQ = T + 1
        A[f'feT_{tag}'] = IN(f"feT_{tag}", [33, L]); A[f'tn_{tag}'] = IN(f"tn_{tag}", [128, T])
        A[f'T1c_{tag}'] = IN(f"T1c_{tag}", [NFQ, 128, T, 128], BF16); A[f'T1s_{tag}'] = IN(f"T1s_{tag}", [NFQ, 128, T, 128], BF16)
        A[f'T2c_{tag}'] = IN(f"T2c_{tag}", [NFQ, 128, L], BF16); A[f'T2s_{tag}'] = IN(f"T2s_{tag}", [NFQ, 128, L], BF16)


def build_mixer(kind):
    E = Env(); nc = E.nc
    IN, A = _common_inputs(nc)
    spec = (GLA_IN, HY_IN, ML_IN, S5_IN)[kind]
    for k, shp in spec.items():
        A[k] = IN(k, shp)
    if kind == 1:
        _hy_table_inputs(IN, A)
    A['lat_o'] = nc.dram_tensor("lat_o", [LAT, D], F32, kind="ExternalOutput").ap()
    if kind != 3:
        A['cx_o'] = nc.dram_tensor("cx_o", [CTXL, D], F32, kind="ExternalOutput").ap()
    setup_consts(E, A['ident'])
    (gla_layer, hyena_layer, mlstm_layer, s5_layer)[kind](E, A)
    E.kb.finish()
    return nc


def _f32(a):
    return np.ascontiguousarray(a, dtype=np.float32)


def mixer_host_inputs(kind, inp):
    if kind == 0:
        d = {k: _f32(inp[k][0]) for k in ('gla_w_in', 'gla_w_gate', 'gla_b_gate', 'gla_norm_g', 'gla_w_out')}
        d['gla_cm'] = gla_consts()
        return d
    if kind == 1:
        d = {k: _f32(inp[k][0]) for k in inp if k.startswith('hy_')}
        d['hy_delta_bc'] = hy_delta_bc()
        for tag, L in (("l", LAT), ("c", CTXL)):
            for k, v in hy_consts(L).items():
                d[f'{k}_{tag}'] = v
        return d
    if kind == 2:
        d = {k: _f32(inp[k][0]) for k in ('ml_w_in', 'ml_conv_w', 'ml_conv_b', 'ml_w_gates', 'ml_b_gates', 'ml_norm_g', 'ml_skip', 'ml_w_out')}
        d['ml_cm'] = ml_consts()
        d['ml_bd'] = ml_bd_layout(_f32(inp['ml_w_q'][0]), _f32(inp['ml_w_k'][0]), _f32(inp['ml_w_v'][0]))
        return d
    d = s5_layouts(_f32(inp['s5_b_re'][0]), _f32(inp['s5_b_im'][0]), _f32(inp['s5_c_re'][0]), _f32(inp['s5_c_im'][0]),
                   _f32(inp['s5_lam_re'][0]), _f32(inp['s5_lam_im'][0]), _f32(inp['s5_log_dt'][0]), _f32(inp['s5_d'][0]))
    d['s5_w_glu'] = _f32(inp['s5_w_glu'][0])
    return d


def kernel_unfused(**inp):
    NC = 8
    x = _f32(inp['x']); c = _f32(inp['c']); ctx = _f32(inp['ctx']); cc = _f32(inp['c_ctx'])
    ident = np.eye(128, dtype=np.float32)
    lat = [x[b] for b in range(NC)]
    cx = [ctx[b] for b in range(NC)]
    ffn_prog = {}
    for i in range(4):
        last = i == 3
        common = dict(mod_w=_f32(inp['mod_w'][i]), mod_b=_f32(inp['mod_b'][i]), norm_g=_f32(inp['norm_g'][i]), cc=cc, ident=ident)
        nc = build_mixer(i)
        extra = mixer_host_inputs(i, inp)
        maps = [dict(common, lat=lat[b], cx=cx[b], c=c[b], **extra) for b in range(NC)]
        res = run_bass_kernel_spmd(nc, maps, core_ids=list(range(NC)))
        lat = [_f32(res.results[b]['lat_o']) for b in range(NC)]
        if not last:
            cx = [_f32(res.results[b]['cx_o']) for b in range(NC)]
        del maps, extra
        key = not last
        if key not in ffn_prog:
            ffn_prog[key] = build_ffn(do_ctx=key)
        maps = [dict(common, lat=lat[b], cx=cx[b], c=c[b], w_in=_f32(inp['ffn_w_in'][i]), w_out=_f32(inp['ffn_w_out'][i])) for b in range(NC)]
        res = run_bass_kernel_spmd(ffn_prog[key], maps, core_ids=list(range(NC)))
        lat = [_f32(res.results[b]['lat_o']) for b in range(NC)]
        if not last:
            cx = [_f32(res.results[b]['cx_o']) for b in range(NC)]
        del maps
    return np.stack(lat, axis=0).astype(np.float32)


def build_fused():
    E = Env(); nc = E.nc
    IN = lambda n, s, dt=F32: nc.dram_tensor(n, list(s), dt, kind="ExternalInput").ap()
    x = IN("x", [LAT, D]); ctx = IN("ctx", [CTXL, D]); c = IN("c", [D]); cc = IN("cc", [D])
    mod_w = IN("mod_w", [4, D, 6 * D]); mod_b = IN("mod_b", [4, 6 * D]); norm_g = IN("norm_g", [4, 4, D])
    ffn_w_in = IN("ffn_w_in", [4, D, 2 * FH]); ffn_w_out = IN("ffn_w_out", [4, FH, D]); ident = IN("ident", [128, 128])
    MIX = {}
    for kind, spec in enumerate((GLA_IN, HY_IN, ML_IN, S5_IN)):
        for k, shp in spec.items():
            MIX[k] = IN(k, shp)
    _hy_table_inputs(IN, MIX)
    out = nc.dram_tensor("out", [LAT, D], F32, kind="ExternalOutput").ap()
    setup_consts(E, ident)
    glob_es = E.es
    cur_lat, cur_cx = x, ctx
    for i in range(4):
        last = i == 3
        base = dict(c=c, cc=cc, mod_w=mod_w[i], mod_b=mod_b[i], norm_g=norm_g[i], ident=ident)
        nl = E.dram(f"lat_m{i}", [LAT, D]); ncx = E.dram(f"cx_m{i}", [CTXL, D]) if not last else None
        A = dict(base, lat=cur_lat, cx=cur_cx, lat_o=nl, **MIX)
        if not last:
            A['cx_o'] = ncx
        with ExitStack() as les:
            E.es = les
            (gla_layer, hyena_layer, mlstm_layer, s5_layer)[i](E, A)
            E.kb.barrier()
        E.es = glob_es
        cur_lat = nl
        if not last:
            cur_cx = ncx
        nl = out if last else E.dram(f"lat_f{i}", [LAT, D])
        ncx = E.dram(f"cx_f{i}", [CTXL, D]) if not last else None
        A = dict(base, lat=cur_lat, cx=cur_cx)
        with ExitStack() as les:
            E.es = les
            ffn_layer(E, A, ffn_w_in[i], ffn_w_out[i], nl, ncx, not last)
            E.kb.barrier()
        E.es = glob_es
        cur_lat = nl
        if not last:
            cur_cx = ncx
    E.kb.finish()
    return nc


def kernel(**inp):
    NC = 8
    x = _f32(inp['x']); c = _f32(inp['c']); ctx = _f32(inp['ctx']); cc = _f32(inp['c_ctx'])
    shared = dict(cc=cc, ident=np.eye(128, dtype=np.float32), mod_w=_f32(inp['mod_w']), mod_b=_f32(inp['mod_b']), norm_g=_f32(inp['norm_g']),
                  ffn_w_in=_f32(inp['ffn_w_in']), ffn_w_out=_f32(inp['ffn_w_out']))
    for kind in range(4):
        shared.update(mixer_host_inputs(kind, inp))
    nc = build_fused()
    maps = [dict(shared, x=x[b], ctx=ctx[b], c=c[b]) for b in range(NC)]
    res = run_bass_kernel_spmd(nc, maps, core_ids=list(range(NC)))
    return np.stack([np.asarray(res.results[b]['out'], dtype=np.float32) for b in range(NC)], axis=0)
```
